# Optimizing a Trainium2 kernel written in Bass

```python
import jax, jax.numpy as jnp
from jax import lax
import numpy as np

D_MODEL = 1024
BATCH = 32
SEQ = 2048
DEPTH = 1

CHUNK = 64
N_META = 16
EPS = 1e-6
DN_HEADS = 8
DN_HEAD_DIM = 128
DN_QK = DN_HEADS * DN_HEAD_DIM
DN_V = DN_HEADS * DN_HEAD_DIM
CONV_WIDTH = 4
GLA_HEADS = 4
GLA_KEY_DIM = 128
GLA_VAL_DIM = 256
GLA_QK = GLA_HEADS * GLA_KEY_DIM
GLA_V = GLA_HEADS * GLA_VAL_DIM
GLA_RANK = 16
GLA_TAU = 16.0
N_GROUPS = 8
EXPERTS_PER_GROUP = 8
N_EXPERTS = N_GROUPS * EXPERTS_PER_GROUP
TOP_K = 2
D_EXPERT = 512
MOE_BLOCK = 256
IN_SPLITS = (DN_QK, DN_QK, DN_V, DN_V, DN_HEADS, DN_HEADS,
             GLA_QK, GLA_QK, GLA_V, GLA_V, GLA_RANK, D_MODEL, D_MODEL)
D_IN = sum(IN_SPLITS)

kernel_name = 'streaming_hybrid_deltanet_gla_hmoe'


def rmsnorm(x, g):
    xf = x.astype(jnp.float32)
    y = xf * lax.rsqrt(jnp.mean(jnp.square(xf), axis=-1, keepdims=True) + EPS)
    return (y * g.astype(jnp.float32)).astype(x.dtype)


def l2norm(x):
    return x * lax.rsqrt(jnp.sum(jnp.square(x), axis=-1, keepdims=True) + EPS)


def causal_conv(x, w):
    k_w, L = w.shape[0], x.shape[1]
    xp = jnp.pad(x, ((0, 0), (k_w - 1, 0), (0, 0)))
    return sum(xp[:, j:j + L] * w[j] for j in range(k_w))


def chunkify(x, pad):
    x = jnp.pad(x, ((0, 0), (pad, 0)) + ((0, 0),) * (x.ndim - 2))
    b, lp = x.shape[:2]
    x = x.reshape((b, lp // CHUNK, CHUNK) + x.shape[2:])
    return x.transpose((1, 0, 3, 2) + tuple(range(4, x.ndim)))


def unchunkify(o, pad):
    n, b, h, c, dv = o.shape
    return o.transpose(1, 0, 3, 2, 4).reshape(b, n * c, h, dv)[:, pad:]


def gated_delta_rule(q, k, v, g, beta):
    L = q.shape[1]
    pad = (-L) % CHUNK
    dk, dv = q.shape[-1], v.shape[-1]
    q, k, v = [chunkify(t, pad) for t in (q * dk ** -0.5, k, v)]
    g, beta = chunkify(g, pad), chunkify(beta, pad)
    gc = jnp.cumsum(g, axis=-1)
    causal = jnp.tril(jnp.ones((CHUNK, CHUNK), bool))
    strict = jnp.tril(jnp.ones((CHUNK, CHUNK), bool), -1)
    diff = gc[..., :, None] - gc[..., None, :]
    decay = jnp.where(causal, jnp.exp(jnp.where(causal, diff, 0.0)), 0.0)
    kk = jnp.einsum('nbhid,nbhjd->nbhij', k, k)
    a_mat = jnp.where(strict, beta[..., None] * kk * decay, 0.0) + jnp.eye(CHUNK, dtype=jnp.float32)
    rhs = jnp.concatenate([v * beta[..., None], k * (beta * jnp.exp(gc))[..., None]], axis=-1)
    sol = lax.linalg.triangular_solve(a_mat, rhs, left_side=True, lower=True, unit_diagonal=True)
    u, w = sol[..., :dv], sol[..., dv:]
    qk = jnp.where(causal, jnp.einsum('nbhid,nbhjd->nbhij', q, k) * decay, 0.0)
    q_dec = q * jnp.exp(gc)[..., None]
    g_last = gc[..., -1]
    k_dec = k * jnp.exp(g_last[..., None] - gc)[..., None]

    def step(S, xs):
        qk_c, q_c, k_c, u_c, w_c, gl = xs
        v_new = u_c - jnp.einsum('bhcd,bhde->bhce', w_c, S)
        o = jnp.einsum('bhcd,bhde->bhce', q_c, S) + jnp.einsum('bhij,bhje->bhie', qk_c, v_new)
        S = S * jnp.exp(gl)[..., None, None] + jnp.einsum('bhcd,bhce->bhde', k_c, v_new)
        return S, o

    S0 = jnp.zeros(q.shape[1:3] + (dk, dv), jnp.float32)
    _, o = lax.scan(step, S0, (qk, q_dec, k_dec, u, w, g_last))
    return unchunkify(o, pad)


def gla_attention(q, k, v, log_a):
    L = q.shape[1]
    pad = (-L) % CHUNK
    dk, dv = q.shape[-1], v.shape[-1]
    q, k, v, log_a = [chunkify(t, pad) for t in (q * dk ** -0.5, k, v, log_a)]
    b = jnp.cumsum(log_a, axis=-2)
    b_mid = b[..., CHUNK // 2 - 1:CHUNK // 2, :]
    causal = jnp.tril(jnp.ones((CHUNK, CHUNK), bool))
    att = jnp.einsum('nbhid,nbhjd->nbhij', q * jnp.exp(b - b_mid), k * jnp.exp(b_mid - b))
    o_intra = jnp.einsum('nbhij,nbhje->nbhie', jnp.where(causal, att, 0.0), v)
    q_dec = q * jnp.exp(b)
    b_last = b[..., -1, :]
    k_dec = k * jnp.exp(b_last[..., None, :] - b)

    def step(S, xs):
        q_c, k_c, v_c, bl = xs
        o = jnp.einsum('bhcd,bhde->bhce', q_c, S)
        S = S * jnp.exp(bl)[..., :, None] + jnp.einsum('bhcd,bhce->bhde', k_c, v_c)
        return S, o

    S0 = jnp.zeros(q.shape[1:3] + (dk, dv), jnp.float32)
    _, o_inter = lax.scan(step, S0, (q_dec, k_dec, v, b_last))
    return unchunkify(o_inter + o_intra, pad)


def hybrid_mixer(h, w_in, conv_dn, a_log, dt_bias, norm_head_dn, w_proj_dn,
                 w_alpha, b_alpha, norm_head_gla, w_proj_gla, w_out):
    f32 = jnp.float32
    bsz, L, _ = h.shape
    proj = h @ w_in
    splits = np.cumsum(IN_SPLITS)[:-1].tolist()
    (q_dn, k_dn, v_dn, z_dn, a_dn, b_dn, q_gla, k_gla, v_gla, r_gla, lr_gla,
     gate_dn, gate_gla) = jnp.split(proj, splits, axis=-1)
    qkv = jax.nn.silu(causal_conv(jnp.concatenate([q_dn, k_dn, v_dn], -1), conv_dn)).astype(f32)
    q_dn, k_dn, v_dn = jnp.split(qkv, [DN_QK, 2 * DN_QK], axis=-1)
    q_dn = l2norm(q_dn.reshape(bsz, L, DN_HEADS, DN_HEAD_DIM))
    k_dn = l2norm(k_dn.reshape(bsz, L, DN_HEADS, DN_HEAD_DIM))
    v_dn = v_dn.reshape(bsz, L, DN_HEADS, DN_V // DN_HEADS)
    g = -jnp.exp(a_log.astype(f32)) * jax.nn.softplus(a_dn.astype(f32) + dt_bias.astype(f32))
    beta = jax.nn.sigmoid(b_dn.astype(f32))
    o_dn = gated_delta_rule(q_dn, k_dn, v_dn, g, beta)
    o_dn = rmsnorm(o_dn, norm_head_dn) * jax.nn.silu(z_dn.astype(f32).reshape(bsz, L, DN_HEADS, -1))
    y_dn = o_dn.reshape(bsz, L, DN_V).astype(h.dtype) @ w_proj_dn
    log_alpha = jax.nn.log_sigmoid((lr_gla @ w_alpha).astype(f32) + b_alpha.astype(f32)) / GLA_TAU
    o_gla = gla_attention(q_gla.astype(f32).reshape(bsz, L, GLA_HEADS, GLA_KEY_DIM),
                          k_gla.astype(f32).reshape(bsz, L, GLA_HEADS, GLA_KEY_DIM),
                          v_gla.astype(f32).reshape(bsz, L, GLA_HEADS, GLA_VAL_DIM),
                          log_alpha.reshape(bsz, L, GLA_HEADS, GLA_KEY_DIM))
    o_gla = rmsnorm(o_gla, norm_head_gla) * jax.nn.silu(r_gla.astype(f32).reshape(bsz, L, GLA_HEADS, GLA_VAL_DIM))
    y_gla = o_gla.reshape(bsz, L, GLA_V).astype(h.dtype) @ w_proj_gla
    merged = jax.nn.sigmoid(gate_dn) * y_dn + jax.nn.sigmoid(gate_gla) * y_gla
    return merged @ w_out


def hier_moe(h, w_group, b_group, w_router, b_router, w1, w3, w2):
    f32 = jnp.float32
    bsz, L, d = h.shape
    T = bsz * L
    xf = h.reshape(T, d)
    xr = xf.astype(f32)
    g_logits = xr @ w_group.astype(f32) + b_group.astype(f32)
    g_sel = jnp.argmax(g_logits, axis=-1)
    g_w = jnp.take_along_axis(jax.nn.softmax(g_logits, -1), g_sel[:, None], axis=1)[:, 0]
    e_logits = (xr @ w_router.astype(f32) + b_router.astype(f32)).reshape(T, N_GROUPS, EXPERTS_PER_GROUP)
    e_in = jnp.take_along_axis(e_logits, g_sel[:, None, None], axis=1)[:, 0]
    top_v, top_i = lax.top_k(e_in, TOP_K)
    gate = jax.nn.softmax(top_v, axis=-1) * g_w[:, None]
    expert_id = g_sel[:, None] * EXPERTS_PER_GROUP + top_i
    M = T * TOP_K
    flat_e = expert_id.reshape(M).astype(jnp.int32)
    flat_tok = jnp.repeat(jnp.arange(T, dtype=jnp.int32), TOP_K)
    order = jnp.argsort(flat_e)
    se, stok, sw = flat_e[order], flat_tok[order], gate.reshape(M)[order]
    sizes = jnp.bincount(flat_e, length=N_EXPERTS)
    starts = jnp.cumsum(sizes) - sizes
    padded = (sizes + MOE_BLOCK - 1) // MOE_BLOCK * MOE_BLOCK
    pends = jnp.cumsum(padded)
    pstarts = pends - padded
    slot = pstarts[se] + (jnp.arange(M, dtype=jnp.int32) - starts[se])
    n_blocks = M // MOE_BLOCK + N_EXPERTS
    P = n_blocks * MOE_BLOCK
    slot_tok = jnp.full((P,), T, jnp.int32).at[slot].set(stok)
    x_ext = jnp.concatenate([xf, jnp.zeros((1, d), xf.dtype)], axis=0)
    xb = x_ext[slot_tok].reshape(n_blocks, MOE_BLOCK, d)
    block_expert = jnp.clip(jnp.searchsorted(pends, jnp.arange(n_blocks) * MOE_BLOCK, side='right'),
                            0, N_EXPERTS - 1)

    def expert_block(args):
        xblk, e = args
        hid = jax.nn.silu(xblk @ w1[e]) * (xblk @ w3[e])
        return hid @ w2[e]

    yb = lax.map(expert_block, (xb, block_expert)).reshape(P, d)
    y = jnp.zeros((T, d), yb.dtype).at[stok].add(yb[slot] * sw[:, None].astype(yb.dtype))
    return y.reshape(bsz, L, d).astype(h.dtype)


def setup_inputs(seed: int = 0) -> dict:
    key = jax.random.key(seed)
    ks = jax.random.split(key, 24)

    def nrm(k, shape, scale):
        return jax.random.normal(k, shape, jnp.float32) * scale

    def gain(k, shape):
        return 1.0 + 0.05 * jax.random.normal(k, shape, jnp.float32)

    dt = jnp.exp(jax.random.uniform(ks[5], (DEPTH, DN_HEADS), jnp.float32,
                                    minval=float(np.log(1e-3)), maxval=float(np.log(1e-1))))
    return {
        'x': nrm(ks[0], (BATCH, SEQ, D_MODEL), 1.0),
        'meta_tokens': nrm(ks[1], (N_META, D_MODEL), 1.0),
        'norm_mix': gain(ks[2], (DEPTH, D_MODEL)),
        'w_in': nrm(ks[3], (DEPTH, D_MODEL, D_IN), D_MODEL ** -0.5),
        'conv_dn': nrm(ks[4], (DEPTH, CONV_WIDTH, DN_QK * 2 + DN_V), CONV_WIDTH ** -0.5),
        'a_log': jnp.log(jax.random.uniform(ks[6], (DEPTH, DN_HEADS), jnp.float32, minval=1.0, maxval=16.0)),
        'dt_bias': dt + jnp.log(-jnp.expm1(-dt)),
        'norm_head_dn': gain(ks[7], (DEPTH, DN_HEAD_DIM)),
        'w_proj_dn': nrm(ks[8], (DEPTH, DN_V, D_MODEL), DN_V ** -0.5),
        'w_alpha': nrm(ks[9], (DEPTH, GLA_RANK, GLA_QK), GLA_RANK ** -0.5),
        'b_alpha': nrm(ks[10], (DEPTH, GLA_QK), 0.1),
        'norm_head_gla': gain(ks[11], (DEPTH, GLA_VAL_DIM)),
        'w_proj_gla': nrm(ks[12], (DEPTH, GLA_V, D_MODEL), GLA_V ** -0.5),
        'w_out': nrm(ks[13], (DEPTH, D_MODEL, D_MODEL), D_MODEL ** -0.5),
        'norm_ffn': gain(ks[14], (DEPTH, D_MODEL)),
        'w_group': nrm(ks[15], (DEPTH, D_MODEL, N_GROUPS), D_MODEL ** -0.5),
        'b_group': nrm(ks[16], (DEPTH, N_GROUPS), 0.01),
        'w_router': nrm(ks[17], (DEPTH, D_MODEL, N_EXPERTS), D_MODEL ** -0.5),
        'b_router': nrm(ks[18], (DEPTH, N_EXPERTS), 0.01),
        'w1': nrm(ks[19], (DEPTH, N_EXPERTS, D_MODEL, D_EXPERT), D_MODEL ** -0.5),
        'w3': nrm(ks[20], (DEPTH, N_EXPERTS, D_MODEL, D_EXPERT), D_MODEL ** -0.5),
        'w2': nrm(ks[21], (DEPTH, N_EXPERTS, D_EXPERT, D_MODEL), D_EXPERT ** -0.5),
        'norm_final': gain(ks[22], (D_MODEL,)),
    }


def reference(x, meta_tokens, norm_mix, w_in, conv_dn, a_log, dt_bias, norm_head_dn, w_proj_dn,
              w_alpha, b_alpha, norm_head_gla, w_proj_gla, w_out, norm_ffn, w_group, b_group,
              w_router, b_router, w1, w3, w2, norm_final):
    bsz = x.shape[0]
    meta = jnp.broadcast_to(meta_tokens[None].astype(x.dtype), (bsz, N_META, D_MODEL))
    h = jnp.concatenate([meta, x], axis=1)
    for i in range(DEPTH):
        h = h + hybrid_mixer(rmsnorm(h, norm_mix[i]), w_in[i], conv_dn[i], a_log[i], dt_bias[i],
                             norm_head_dn[i], w_proj_dn[i], w_alpha[i], b_alpha[i],
                             norm_head_gla[i], w_proj_gla[i], w_out[i])
        h = h + hier_moe(rmsnorm(h, norm_ffn[i]), w_group[i], b_group[i], w_router[i], b_router[i],
                         w1[i], w3[i], w2[i])
    return rmsnorm(h, norm_final)[:, N_META:]
```

```python
import numpy as np
import concourse.bass as bass
import concourse.mybir as mybir
from concourse.bass_utils import run_bass_kernel_spmd

F32 = mybir.dt.float32
BF16 = mybir.dt.bfloat16
I32 = mybir.dt.int32
ALU = mybir.AluOpType
AF = mybir.ActivationFunctionType
AX = mybir.AxisListType

D = 1024
D_IN = 9248
EPS = 1e-6
N_EXP = 64


INSTR_NAMES = {"matmul", "transpose", "activation", "copy", "mul", "sqrt", "square", "add", "tensor_tensor", "tensor_scalar",
               "scalar_tensor_tensor", "tensor_copy", "tensor_reduce", "reduce_sum", "reduce_max", "reciprocal", "max", "memset",
               "tensor_tensor_scan", "select", "iota", "tensor_add", "tensor_sub", "tensor_mul"}


class Deferred:
    __slots__ = ("eng", "name", "args", "kw")

    def __init__(self, eng, name, args, kw):
        self.eng, self.name, self.args, self.kw = eng, name, args, kw

    def emit(self):
        return getattr(self.eng, self.name)(*self.args, **self.kw)

    def free_size(self):
        ap = self.kw.get("out", self.args[0] if self.args else None)
        if self.name in ("matmul",):
            ap = self.kw.get("rhs", ap)
        elif self.name == "transpose":
            ap = self.kw.get("in_", ap)
        try:
            n = int(ap.free_size())
        except Exception:
            n = 128
        fp32 = False
        try:
            fp32 = (self.name in ("matmul", "transpose")) and self.kw.get("lhsT", self.kw.get("in_")).dtype == F32
        except Exception:
            pass
        return n, fp32


class EngProxy:
    def __init__(self, eng):
        object.__setattr__(self, "_eng", eng)

    def __getattr__(self, name):
        real = getattr(self._eng, name)
        if name in INSTR_NAMES:
            eng = self._eng
            return lambda *a, **k: Deferred(eng, name, a, k)
        return real


class NCProxy:
    def __init__(self, nc):
        object.__setattr__(self, "_nc", nc)
        for nm in ("tensor", "scalar", "vector", "gpsimd", "sync"):
            object.__setattr__(self, nm, EngProxy(getattr(nc, nm)))

    def __getattr__(self, name):
        return getattr(self._nc, name)


class Res:
    __slots__ = ("name", "w", "r", "dsem", "dn", "dbase", "fw", "excl")

    def __init__(self, fw, name):
        self.fw = fw
        self.name = name
        self.w = None
        self.r = {}
        self.dsem = None
        self.dn = 0
        self.excl = False
        self.dbase = 0


class Eng:
    def __init__(self, fw, idx, name, eng, sem):
        self.fw, self.idx, self.name, self.eng, self.sem = fw, idx, name, eng, sem
        self.n = 0
        self.seen = {}
        self.seen_d = {}

    def _wait_eng(self, idx, cnt):
        if idx == self.idx and self.name == "pe":
            return
        if self.seen.get(idx, 0) < cnt:
            self.eng.wait_ge(self.fw.engs[idx].sem, cnt)
            self.seen[idx] = cnt

    def _wait_dma(self, R):
        if R.dn > R.dbase and self.seen_d.get(R, 0) < R.dn:
            self.eng.wait_ge(R.dsem, 16 * R.dn)
            self.seen_d[R] = R.dn

    def _deps(self, reads, writes, dma_part=False):
        for R in reads:
            if R.w is not None:
                self._wait_eng(*R.w)
            if not dma_part:
                self._wait_dma(R)
        for R in writes:
            if R.w is not None:
                self._wait_eng(*R.w)
            for i, c in R.r.items():
                self._wait_eng(i, c)
            if not dma_part:
                self._wait_dma(R)

    def op(self, ins_fn, reads=(), writes=()):
        if self.fw.recording:
            self.fw.recs.append(("op", self, ins_fn(), tuple(reads), tuple(writes)))
            return None
        if isinstance(ins_fn, Deferred):
            d_ = ins_fn
            ins_fn = d_.emit
        if any(R.excl for R in reads):
            writes = list(writes) + [R for R in reads if R.excl and R not in writes]
            reads = [R for R in reads if not R.excl]
        self._deps(reads, writes)
        ins = ins_fn()
        self.n += 1
        ins.then_inc(self.sem, 1)
        for R in reads:
            R.r[self.idx] = self.n
        for R in writes:
            R.w = (self.idx, self.n)
            R.r = {}
        return ins

    def dma(self, out, in_, R, mode, part=False, extra_reads=(), extra_writes=(), **kw):
        if self.fw.recording:
            self.fw.recs.append(("dma", self, (out, in_, kw), R, mode, part, tuple(extra_reads), tuple(extra_writes)))
            return None
        if mode == "w":
            self._deps(extra_reads, (R,), dma_part=part)
        else:
            self._deps((R,) + tuple(extra_reads), (), dma_part=part)
        self.fw.give_dsem(R)
        ins = self.eng.dma_start(out=out, in_=in_, **kw)
        ins.then_inc(R.dsem, 16)
        R.dn += 1
        return ins

    def wait_dma(self, R, tokens=()):
        if self.fw.recording:
            self.fw.recs.append(("waitdma", self, R, tuple(tokens)))
            return
        self._wait_dma(R)

    def indirect_dma(self, R, mode, extra_reads=(), **kw):
        if self.fw.recording:
            self.fw.recs.append(("idma", self, kw, R, mode, tuple(extra_reads)))
            return None
        if mode == "w":
            self._deps(extra_reads, (R,))
        else:
            self._deps((R,) + tuple(extra_reads), ())
        self.fw.give_dsem(R)
        ins = self.eng.indirect_dma_start(**kw)
        ins.then_inc(R.dsem, 16)
        R.dn += 1
        return ins


class FW:
    def __init__(self, nc):
        self.nc = nc
        self._stack = []
        self.sem_i = 0
        self.engs = []
        for i, (nm, e) in enumerate([("pe", nc.tensor), ("act", nc.scalar), ("dve", nc.vector),
                                     ("pool", nc.gpsimd), ("sp", nc.sync)]):
            self.engs.append(Eng(self, i, nm, e, self.new_sem("e_" + nm)))
        self.pe, self.act, self.dve, self.pool, self.sp = self.engs
        self.all_res = []
        self.free_dsems = []
        self.scopes = []
        self.recording = True
        self.recs = []

    def give_dsem(self, R):
        if R.dsem is None:
            if self.free_dsems:
                R.dsem, R.dn = self.free_dsems.pop()
                R.dbase = R.dn
            else:
                R.dsem = self.new_sem("d")

    def new_sem(self, name):
        self.sem_i += 1
        return self.nc.alloc_semaphore(name=f"{name}_{self.sem_i}")

    def res(self, name):
        R = Res(self, name)
        self.all_res.append(R)
        return R

    def sbuf(self, name, shape, dtype):
        cm = self.nc.sbuf_tensor(name, list(shape), dtype)
        t = cm.__enter__()
        self._stack.append(cm)
        return t, self.res(name)

    def psum(self, name, shape, dtype):
        cm = self.nc.psum_tensor(name, list(shape), dtype)
        t = cm.__enter__()
        self._stack.append(cm)
        R = self.res(name)
        R.excl = True
        return t, R

    def drain_all(self, eng):
        for R in self.all_res:
            eng._wait_dma(R)
        for e in self.engs:
            if e.n and e is not eng:
                eng._wait_eng(e.idx, e.n)


    def _cost(self, rec):
        kind, eng = rec[0], rec[1]
        if kind == "op":
            d = rec[2]
            n, fp32 = d.free_size() if isinstance(d, Deferred) else (128, False)
            if eng.name == "pe":
                return (0.06 + n * 0.00037) * (4 if fp32 and d.name == "matmul" else 1), 0.0
            if eng.name == "act":
                return 0.22 + n * 0.00075, 0.0
            if eng.name == "dve":
                return 0.12 + n * (0.0066 if d.name == "reciprocal" else 0.00105), 0.0
            if d.kw.get("op", None) == ALU.pow:
                return 0.4 + n * 0.125, 0.0
            return 0.25 + n * 0.0019, 0.0
        if kind == "dma":
            try:
                nbytes = int(rec[2][0].nbytes())
            except Exception:
                nbytes = 65536
            return 0.15, 2.0 + nbytes / 120e3
        if kind == "idma":
            return 0.3, 3.0 + 4.0
        return 0.02, 0.0

    def flush(self):
        recs, self.recs = self.recs, []
        self.recording = False
        n = len(recs)
        if n == 0:
            self.recording = True
            return
        import heapq
        preds = [None] * n
        lastw, readers = {}, {}
        for i, rec in enumerate(recs):
            kind = rec[0]
            if kind == "op":
                rd, wr = list(rec[3]), list(rec[4])
            elif kind == "dma":
                R, mode = rec[3], rec[4]
                rd, wr = (list(rec[6]), [R]) if mode == "w" else ([R] + list(rec[6]), [])
                wr = wr + list(rec[7])
            elif kind == "idma":
                R, mode = rec[3], rec[4]
                rd, wr = (list(rec[5]), [R]) if mode == "w" else ([R] + list(rec[5]), [])
            else:
                rd, wr = [], [rec[2]] + list(rec[3])
            wr = wr + [R for R in rd if R.excl and R not in wr]
            rd = [R for R in rd if not R.excl]
            p = set()
            for R in rd:
                if R in lastw:
                    p.add(lastw[R])
            for R in wr:
                if R in lastw:
                    p.add(lastw[R])
                p.update(readers.get(R, ()))
            for R in rd:
                readers.setdefault(R, []).append(i)
            for R in wr:
                lastw[R] = i
                readers[R] = []
            p.discard(i)
            preds[i] = p
        succs = [[] for _ in range(n)]
        indeg = [0] * n
        for i, p in enumerate(preds):
            indeg[i] = len(p)
            for j in p:
                succs[j].append(i)
        costs = [self._cost(r) for r in recs]
        te = [0.0] * len(self.engs)
        fin = [0.0] * n
        dep = [0.0] * n
        bl = [0.0] * n
        for i in range(n - 1, -1, -1):
            m = 0.0
            for j in succs[i]:
                if bl[j] > m:
                    m = bl[j]
            bl[i] = m + costs[i][0] + costs[i][1] + 0.15
        ALPHA = getattr(self, "alpha", 0.01)
        heap = [(0.0 - ALPHA * bl[i], i) for i in range(n) if indeg[i] == 0]
        heapq.heapify(heap)
        order = []
        while heap:
            key, i = heapq.heappop(heap)
            e = recs[i][1].idx
            est = max(te[e], dep[i])
            k2 = est - ALPHA * bl[i]
            if heap and k2 > heap[0][0] + 1e-9 and k2 > key + 1e-9:
                heapq.heappush(heap, (k2, i))
                continue
            c, lat = costs[i]
            te[e] = est + c
            fin[i] = est + c + lat
            order.append(i)
            for j in succs[i]:
                hop = 0.05 if recs[j][1].idx == e else 0.2
                if fin[i] + hop > dep[j]:
                    dep[j] = fin[i] + hop
                indeg[j] -= 1
                if indeg[j] == 0:
                    heapq.heappush(heap, (max(dep[j], te[recs[j][1].idx]) - ALPHA * bl[j], j))
        assert len(order) == n, (len(order), n)
        if not getattr(self, "sched", True):
            order = list(range(n))
        self.sim_time = getattr(self, "sim_time", 0.0) + max(te)
        busy = [0.0] * len(self.engs)
        for i in range(n):
            busy[recs[i][1].idx] += costs[i][0]
        self.sim_log = getattr(self, "sim_log", [])
        self.sim_log.append((n, max(te), [round(b) for b in busy]))
        for i in order:
            rec = recs[i]
            kind, eng = rec[0], rec[1]
            if kind == "op":
                eng.op(rec[2], rec[3], rec[4])
            elif kind == "dma":
                out, in_, kw = rec[2]
                eng.dma(out, in_, rec[3], rec[4], part=rec[5], extra_reads=rec[6], **kw)
            elif kind == "idma":
                eng.indirect_dma(rec[3], rec[4], extra_reads=rec[5], **rec[2])
            else:
                eng._wait_dma(rec[2])
        self.recording = True

    def push_scope(self):
        self._stack.append(None)
        self.scopes.append(len(self.all_res))

    def pop_scope(self):
        self.barrier()
        while True:
            cm = self._stack.pop()
            if cm is None:
                break
            cm.__exit__(None, None, None)
        n0 = self.scopes.pop()
        for R in self.all_res[n0:]:
            if R.dsem is not None:
                self.free_dsems.append((R.dsem, R.dn))
        del self.all_res[n0:]
        for e in self.engs:
            e.seen_d = {}

    def barrier(self):
        self.flush()
        self.recording = False
        sp = self.sp
        self.drain_all(sp)
        ins = sp.eng.nop()
        sp.n += 1
        ins.then_inc(sp.sem, 1)
        for e in self.engs:
            if e is not sp:
                e._wait_eng(sp.idx, sp.n)
        for R in self.all_res:
            R.w = None
            R.r = {}
        self.recording = True

    def close(self):
        while self._stack:
            cm = self._stack.pop()
            if cm is not None:
                cm.__exit__(None, None, None)


def groups_of(NSEQ, SEQ, with_meta=True, gmax=512):
    g = [(0, 128)] if with_meta else []
    for s in range(NSEQ):
        t = 0
        while t < SEQ:
            n = min(gmax, SEQ - t)
            g.append((128 + s * SEQ + t, n))
            t += n
    return g


class MK:
    def __init__(self, NSEQ, SEQ, CAP, debug=()):
        self.NSEQ, self.SEQ, self.CAP = NSEQ, SEQ, CAP
        self.T = NSEQ * SEQ
        self.NT = 128 + self.T
        self.NTILES = self.NT // 128
        self.debug = set(debug)
        nc = bass.Bass("TRN2", target_bir_lowering=False)
        self.fw = FW(nc)
        self.nc = NCProxy(nc)
        T, NT = self.T, self.NT

        def inp(name, shape, dt=F32):
            return nc.dram_tensor(name, list(shape), dt, kind="ExternalInput").ap()

        self.x = inp("x", [T, D])
        self.meta = inp("meta", [16, D])
        self.g_mix = inp("g_mix", [1, D])
        self.w_in = inp("w_in", [D, D_IN])
        self.ident = inp("ident", [128, 128])
        self.out = nc.dram_tensor("out", [T, D], F32, kind="ExternalOutput").ap()

        def scr(name, shape, dt):
            kind = "ExternalOutput" if name in self.debug else "Internal"
            return nc.dram_tensor(name, list(shape), dt, kind=kind).ap()

        self.PF = {}
        for nm, rows in [("qkv", 3072), ("z", 1024), ("qg", 512), ("kg", 512), ("rg", 1024),
                         ("gd", 1024), ("gg", 1024)]:
            self.PF[nm] = scr("pf_" + nm, [rows, NT], BF16)
        self.PF_LR = scr("pf_lr", [16, NT], F32)
        self.PT_VG = scr("pt_vg", [NT, 1024], BF16)
        self.PT_AB = scr("pt_ab", [NT, 16], F32)

    def load_consts(self):
        fw, nc = self.fw, self.nc
        self.idf, self.idfR = fw.sbuf("idf", [128, 128], F32)
        self.idb, self.idbR = fw.sbuf("idb", [128, 128], BF16)
        fw.sp.dma(self.idf[:], self.ident, self.idfR, "w")
        fw.pool.dma(self.idb[:], self.ident, self.idbR, "w")

    def phase1(self):
        fw, nc = self.fw, self.nc
        NT, NTILES = self.NT, self.NTILES
        self.hnT, self.hnTR = fw.sbuf("hnT", [128, 8, NT], BF16)
        fw.push_scope()
        gb, gbR = fw.sbuf("gb", [128, D], F32)
        fw.sp.dma(gb[:], self.g_mix.partition_broadcast(128), gbR, "w")
        xb = [fw.sbuf(f"xb{i}", [128, D], F32) for i in range(3)]
        junk, junkR = fw.sbuf("junk", [128, D], BF16)
        ssb = [fw.sbuf(f"ss{i}", [128, 1], F32) for i in range(2)]
        hnb = [fw.sbuf(f"hn{i}", [128, D], BF16) for i in range(2)]
        ptb = [fw.psum(f"pt{i}", [128, 8, 128], BF16) for i in range(2)]
        for i in range(NTILES):
            xt, xR = xb[i % 3]
            ss, ssR = ssb[i % 2]
            hn, hnR = hnb[i % 2]
            pt, ptR = ptb[i % 2]
            if i == 0:
                fw.pool.op(lambda: nc.gpsimd.memset(xt[:], 0.0), writes=[xR])
                fw.sp.dma(xt[112:128, :], self.meta, xR, "w")
            else:
                fw.sp.dma(xt[:], self.x[(i - 1) * 128:i * 128, :], xR, "w")
            self.rms_rstd(xt, xR, junk, junkR, ss, ssR)
            fw.dve.op(lambda: nc.vector.scalar_tensor_tensor(
                out=hn[:], in0=xt[:], scalar=ss[:], in1=gb[:], op0=ALU.mult, op1=ALU.mult),
                reads=[xR, ssR, gbR], writes=[hnR])
            for kc in range(8):
                fw.pe.op(lambda: nc.tensor.transpose(out=pt[:, kc, :], in_=hn[:, kc * 128:(kc + 1) * 128],
                                                     identity=self.idb[:]),
                         reads=[hnR, self.idbR], writes=[ptR])
            fw.act.op(lambda: nc.scalar.copy(out=self.hnT[:, :, i * 128:(i + 1) * 128], in_=pt[:]),
                      reads=[ptR], writes=[self.hnTR])
        fw.pop_scope()

    def rms_rstd(self, xt, xR, junk, junkR, ss, ssR, n=D):
        fw, nc = self.fw, self.nc
        fw.act.op(lambda: nc.scalar.activation(out=junk[:], in_=xt[:], func=AF.Square, accum_out=ss[:]),
                  reads=[xR], writes=[junkR, ssR])
        fw.dve.op(lambda: nc.vector.tensor_scalar(out=ss[:], in0=ss[:], scalar1=1.0 / n, scalar2=EPS,
                                                  op0=ALU.mult, op1=ALU.add), reads=[ssR], writes=[ssR])
        fw.act.op(lambda: nc.scalar.sqrt(out=ss[:], in_=ss[:]), reads=[ssR], writes=[ssR])
        fw.dve.op(lambda: nc.vector.reciprocal(out=ss[:], in_=ss[:]), reads=[ssR], writes=[ssR])

    def phase2(self):
        fw, nc = self.fw, self.nc
        NT = self.NT
        fw.push_scope()
        groups = groups_of(self.NSEQ, self.SEQ)
        w_v = self.w_in.rearrange("(kc p) c -> p kc c", p=128)
        wb = [fw.sbuf(f"wb{i}", [128, 8, 512], BF16) for i in range(2)]
        pb = [fw.psum(f"pp{i}", [128, 512], F32) for i in range(4)]
        SG = 2048
        stg = [fw.sbuf(f"stg{i}", [128, SG], BF16) for i in range(3)]
        cnt = {"w": 0, "p": 0, "s": 0, "e": 0}

        def evac(out_ap, outR, ps, psR):
            if cnt["e"] % 2 == 0:
                fw.act.op(lambda: nc.scalar.copy(out=out_ap, in_=ps), reads=[psR], writes=[outR])
            else:
                fw.dve.op(lambda: nc.vector.tensor_copy(out=out_ap, in_=ps), reads=[psR], writes=[outR])
            cnt["e"] += 1

        cwp, cwpR = fw.sbuf("cwp", [128, 24, 4], F32)
        fw.sp.dma(cwp[:], self.convT, cwpR, "w")
        xrb = [fw.sbuf(f"xrb{i}", [128, 516], F32) for i in range(2)]
        accb = [fw.sbuf(f"accb{i}", [128, 512], F32) for i in range(2)]
        mh, mhR = fw.sbuf("mhist", [128, 4], F32)
        cst_ = {"i": 0, "prev": None}

        def conv_evac(st, sR, off, ps, psR, n0, N, ci):
            xr, xrR = xrb[cst_["i"] % 2]
            acc, accR = accb[cst_["i"] % 2]
            cst_["i"] += 1
            is_meta = n0 == 0
            seq_start = (not is_meta) and (n0 - 128) % self.SEQ == 0
            if is_meta:
                fw.pool.op(lambda: nc.gpsimd.memset(xr[:, 0:4], 0.0), writes=[xrR])
            elif seq_start:
                fw.pool.op(lambda: nc.gpsimd.tensor_copy(out=xr[:, 0:3], in_=mh[:, 0:3]), reads=[mhR], writes=[xrR])
            else:
                pxr, pxrR, pN = cst_["prev"]
                fw.pool.op(lambda: nc.gpsimd.tensor_copy(out=xr[:, 0:3], in_=pxr[:, pN:pN + 3]), reads=[pxrR], writes=[xrR])
            fw.act.op(lambda: nc.scalar.copy(out=xr[:, 3:3 + N], in_=ps[:, :N]), reads=[psR], writes=[xrR])
            if is_meta:
                fw.pool.op(lambda: nc.gpsimd.tensor_copy(out=mh[:, 0:3], in_=xr[:, N:N + 3]), reads=[xrR], writes=[mhR])
            fw.act.op(lambda: nc.scalar.mul(out=acc[:, :N], in_=xr[:, 0:N], mul=cwp[:, ci, 0:1]), reads=[xrR, cwpR], writes=[accR])
            for j in (1, 2):
                fw.dve.op(lambda: nc.vector.scalar_tensor_tensor(out=acc[:, :N], in0=xr[:, j:j + N], scalar=cwp[:, ci, j:j + 1], in1=acc[:, :N],
                                                                 op0=ALU.mult, op1=ALU.add), reads=[xrR, cwpR, accR], writes=[accR])
            fw.dve.op(lambda: nc.vector.scalar_tensor_tensor(out=st[:, off:off + N], in0=xr[:, 3:3 + N], scalar=cwp[:, ci, 3:4], in1=acc[:, :N],
                                                             op0=ALU.mult, op1=ALU.add), reads=[xrR, cwpR, accR], writes=[sR])
            cst_["prev"] = (xr, xrR, N)

        segs = [("qkv", 0, 3072), ("z", 3072, 1024), ("qg", 4112, 512), ("kg", 4624, 512),
                ("rg", 6160, 1024), ("gd", 7200, 1024), ("gg", 8224, 1024)]
        for nm, c0, ncols in segs:
            dst = self.PF[nm]
            for b0 in range(0, ncols, 512):
                wt, wR = wb[cnt["w"] % 2]
                cnt["w"] += 1
                fw.pool.dma(wt[:], w_v[:, :, c0 + b0:c0 + b0 + 512], wR, "w")
                for cc in range(4):
                    r0 = b0 + cc * 128
                    gi = 0
                    while gi < len(groups):
                        st, sR = stg[cnt["s"] % 3]
                        cnt["s"] += 1
                        n_start = groups[gi][0]
                        off = 0
                        while gi < len(groups) and off + groups[gi][1] <= SG and groups[gi][0] == n_start + off:
                            n0, N = groups[gi]
                            ps, psR = pb[cnt["p"] % 4]
                            cnt["p"] += 1
                            for kc in range(8):
                                fw.pe.op(lambda: nc.tensor.matmul(
                                    ps[:, :N], lhsT=wt[:, kc, cc * 128:(cc + 1) * 128], rhs=self.hnT[:, kc, n0:n0 + N],
                                    start=(kc == 0), stop=(kc == 7)), reads=[wR, self.hnTR], writes=[psR])
                            if nm == "qkv":
                                conv_evac(st, sR, off, ps, psR, n0, N, r0 // 128)
                            else:
                                evac(st[:, off:off + N], sR, ps[:, :N], psR)
                            off += N
                            gi += 1
                        fw.sp.dma(dst[r0:r0 + 128, n_start:n_start + off], st[:, :off], sR, "r")
        wl, wlR = fw.sbuf("wl", [128, 8, 16], BF16)
        fw.pool.dma(wl[:], w_v[:, :, 7184:7200], wlR, "w")
        lst = [fw.sbuf(f"lst{i}", [16, 512], F32) for i in range(2)]
        for gi, (n0, N) in enumerate(groups):
            ps, psR = pb[cnt["p"] % 4]
            cnt["p"] += 1
            st, sR = lst[gi % 2]
            for kc in range(8):
                fw.pe.op(lambda: nc.tensor.matmul(ps[:16, :N], lhsT=wl[:, kc, :], rhs=self.hnT[:, kc, n0:n0 + N],
                                                  start=(kc == 0), stop=(kc == 7)),
                         reads=[wlR, self.hnTR], writes=[psR])
            evac(st[:, :N], sR, ps[:16, :N], psR)
            fw.sp.dma(self.PF_LR[:, n0:n0 + N], st[:, :N], sR, "r")
        wv = [fw.sbuf(f"wv{i}", [128, 8, 512], BF16) for i in range(2)]
        for hf in range(2):
            fw.pool.dma(wv[hf][0][:], w_v[:, :, 5136 + hf * 512:5136 + (hf + 1) * 512], wv[hf][1], "w")
        wab, wabR = fw.sbuf("wab", [128, 8, 16], BF16)
        fw.pool.dma(wab[:], w_v[:, :, 4096:4112], wabR, "w")
        vst = [fw.sbuf(f"vst{i}", [128, 1024], BF16) for i in range(2)]
        ast = [fw.sbuf(f"ast{i}", [128, 16], F32) for i in range(2)]
        for i in range(self.NTILES):
            st, sR = vst[i % 2]
            for hf in range(2):
                ps, psR = pb[cnt["p"] % 4]
                cnt["p"] += 1
                for kc in range(8):
                    fw.pe.op(lambda: nc.tensor.matmul(ps[:], lhsT=self.hnT[:, kc, i * 128:(i + 1) * 128],
                                                      rhs=wv[hf][0][:, kc, :], start=(kc == 0), stop=(kc == 7)),
                             reads=[wv[hf][1], self.hnTR], writes=[psR])
                evac(st[:, hf * 512:(hf + 1) * 512], sR, ps[:], psR)
            fw.sp.dma(self.PT_VG[i * 128:(i + 1) * 128, :], st[:], sR, "r")
            ps, psR = pb[cnt["p"] % 4]
            cnt["p"] += 1
            at, aR = ast[i % 2]
            for kc in range(8):
                fw.pe.op(lambda: nc.tensor.matmul(ps[:, :16], lhsT=self.hnT[:, kc, i * 128:(i + 1) * 128],
                                                  rhs=wab[:, kc, :], start=(kc == 0), stop=(kc == 7)),
                         reads=[wabR, self.hnTR], writes=[psR])
            evac(at[:], aR, ps[:, :16], psR)
            fw.sp.dma(self.PT_AB[i * 128:(i + 1) * 128, :], at[:], aR, "r")
        fw.pop_scope()

    def finish(self):
        self.fw.flush()
        self.fw.recording = False
        self.fw.drain_all(self.fw.sp)
        self.fw.barrier()
        self.fw.close()
        return self.nc._nc


def run_interleaved(gens):
    gens = list(gens)
    while gens:
        for g in list(gens):
            try:
                next(g)
            except StopIteration:
                gens.remove(g)


class Slots:
    def __init__(self, t, R):
        self.t, self.R = t, R
        self.i = 0

    def next(self):
        i = self.i % 4
        self.i += 1
        return self.t[:, i * 128:(i + 1) * 128], self.R


def bf16_view(ap):
    return ap.bitcast(BF16)[:, 0:128]


def p3_setup(self):
    nc = self.nc
    NT = self.NT
    self.convT = nc.dram_tensor("convT", [128, 24, 4], F32, kind="ExternalInput").ap()
    self.a_log = nc.dram_tensor("a_log", [1, 8], F32, kind="ExternalInput").ap()
    self.dt_bias = nc.dram_tensor("dt_bias", [1, 8], F32, kind="ExternalInput").ap()
    self.nh_dn = nc.dram_tensor("nh_dn", [128, 1], F32, kind="ExternalInput").ap()
    self.consts = nc.dram_tensor("consts", [128, 4 * 128 + 2], F32, kind="ExternalInput").ap()
    kind = "ExternalOutput" if "od" in self.debug else "Internal"
    self.OD = nc.dram_tensor("od", [1024, NT], BF16, kind=kind).ap()


def load_consts2(self):
    fw, nc = self.fw, self.nc
    self.cst, self.cstR = fw.sbuf("cst", [128, 4 * 128 + 2], F32)
    fw.sp.dma(self.cst[:], self.consts, self.cstR, "w")
    c = self.cst
    self.tri, self.trs, self.mlow = c[:, 128:256], c[:, 256:384], c[:, 384:512]
    self.ind = c[:, 512:514]
    self.onesf, self.onesfR = fw.sbuf("onesf", [128, 128], F32)
    self.onesb, self.onesbR = fw.sbuf("onesb", [128, 128], BF16)
    self.mhalf, self.mhalfR = fw.sbuf("mhalf", [128, 16], F32)
    fw.pool.op(lambda: nc.gpsimd.memset(self.onesf[:], 1.0), writes=[self.onesfR])
    fw.pool.op(lambda: nc.gpsimd.memset(self.onesb[:], 1.0), writes=[self.onesbR])
    fw.pool.op(lambda: nc.gpsimd.memset(self.mhalf[:], -0.5), writes=[self.mhalfR])
    self.epsc, self.epsR = fw.sbuf("epsc", [128, 2], F32)
    self.eps1, self.eps4 = self.epsc[:, 0:1], self.epsc[:, 1:2]
    fw.pool.op(lambda: nc.gpsimd.memset(self.epsc[:, 0:1], EPS), writes=[self.epsR])
    fw.pool.op(lambda: nc.gpsimd.memset(self.epsc[:, 1:2], 4 * EPS), writes=[self.epsR])


def phase3a(self):
    fw, nc = self.fw, self.nc
    NTL = self.NTILES
    mk = lambda nm, w: fw.sbuf(nm, [128, NTL, w], F32)
    self.G, self.GR_ = mk("G", 8)
    self.BETA, self.BETAR = mk("BETA", 8)
    self.HB, self.HBR = mk("HB", 8)
    self.KBS, self.KBSR = mk("KBS", 8)
    self.EGR, self.EGRR = mk("EGR", 8)
    self.EGL, self.EGLR = mk("EGL", 16)
    fw.push_scope()
    dtb, dtbR = fw.sbuf("dtb", [128, 8], F32)
    negA, negAR = fw.sbuf("negA", [128, 8], F32)
    fw.sp.dma(dtb[:], self.dt_bias.partition_broadcast(128), dtbR, "w")
    fw.sp.dma(negA[:], self.a_log.partition_broadcast(128), negAR, "w")
    fw.act.op(lambda: nc.scalar.activation(out=negA[:], in_=negA[:], func=AF.Exp), reads=[negAR], writes=[negAR])
    fw.dve.op(lambda: nc.vector.tensor_scalar(out=negA[:], in0=negA[:], scalar1=-1.0, scalar2=None, op0=ALU.mult),
              reads=[negAR], writes=[negAR])
    abb = [fw.sbuf(f"abb{i}", [128, 16], F32) for i in range(2)]
    t8, t8R = fw.sbuf("t8", [128, 16], F32)
    r2, r2R = fw.sbuf("r2", [128, 16], F32)
    ex, exR = fw.sbuf("ex", [128, 32], F32)
    ps, psR = fw.psum("p3a", [128, 32], F32)
    E1, E1R = fw.sbuf("E1", [128, NTL, 16], F32)
    for i in range(NTL):
        ab, abR = abb[i % 2]
        fw.sp.dma(ab[:], self.PT_AB[i * 128:(i + 1) * 128, :], abR, "w")
        fw.dve.op(lambda: nc.vector.tensor_tensor(out=t8[:, 0:8], in0=ab[:, 0:8], in1=dtb[:], op=ALU.add),
                  reads=[abR, dtbR], writes=[t8R])
        fw.dve.op(lambda: nc.vector.tensor_scalar(out=t8[:, 8:16], in0=ab[:, 8:16], scalar1=-1.0, scalar2=None,
                                                  op0=ALU.mult), reads=[abR], writes=[t8R])
        fw.act.op(lambda: nc.scalar.activation(out=E1[:, i, :], in_=t8[:], func=AF.Exp), reads=[t8R], writes=[E1R])
    SP, SPR = fw.sbuf("SP", [128, NTL, 8], F32)
    fw.act.op(lambda: nc.scalar.activation(out=SP[:], in_=E1[:, :, 0:8], func=AF.Ln, bias=1.0),
              reads=[E1R], writes=[SPR])
    fw.dve.op(lambda: nc.vector.tensor_tensor(out=self.G[:], in0=SP[:], in1=negA[:].unsqueeze(1).to_broadcast([128, NTL, 8]),
                                              op=ALU.mult), reads=[SPR, negAR], writes=[self.GR_])
    fw.dve.op(lambda: nc.vector.tensor_scalar(out=E1[:, :, 8:16], in0=E1[:, :, 8:16], scalar1=1.0, scalar2=None,
                                              op0=ALU.add), reads=[E1R], writes=[E1R])
    fw.dve.op(lambda: nc.vector.reciprocal(out=self.BETA[:], in_=E1[:, :, 8:16]), reads=[E1R], writes=[self.BETAR])
    fw.dve.op(lambda: nc.vector.tensor_scalar(out=self.HB[:], in0=self.BETA[:], scalar1=0.5, scalar2=None,
                                              op0=ALU.mult), reads=[self.BETAR], writes=[self.HBR])
    for i in range(NTL):
        for c in range(2):
            fw.dve.op(lambda: nc.vector.tensor_scalar(out=r2[:, c * 8:(c + 1) * 8], in0=self.G[:, i, :],
                                                      scalar1=self.ind[:, c:c + 1], scalar2=None, op0=ALU.mult),
                      reads=[self.GR_, self.cstR], writes=[r2R])
        fw.pe.op(lambda: nc.tensor.matmul(ps[:, 0:8], lhsT=self.tri, rhs=self.G[:, i, :], start=True, stop=True),
                 reads=[self.cstR, self.GR_], writes=[psR])
        fw.pe.op(lambda: nc.tensor.matmul(ps[:, 8:16], lhsT=self.trs, rhs=self.G[:, i, :], start=True, stop=True),
                 reads=[self.cstR, self.GR_], writes=[psR])
        fw.pe.op(lambda: nc.tensor.matmul(ps[:, 16:32], lhsT=self.onesf[:], rhs=r2[:], start=True, stop=True),
                 reads=[self.onesfR, r2R], writes=[psR])
        fw.act.op(lambda: nc.scalar.activation(out=ex[:], in_=ps[:], func=AF.Exp), reads=[psR], writes=[exR])
        fw.dve.op(lambda: nc.vector.tensor_tensor(out=self.KBS[:, i, :], in0=self.BETA[:, i, :], in1=ex[:, 0:8],
                                                  op=ALU.mult), reads=[self.BETAR, exR], writes=[self.KBSR])
        fw.dve.op(lambda: nc.vector.tensor_copy(out=self.EGR[:, i, :], in_=ex[:, 8:16]), reads=[exR], writes=[self.EGRR])
        fw.dve.op(lambda: nc.vector.tensor_copy(out=self.EGL[:, i, :], in_=ex[:, 16:32]), reads=[exR], writes=[self.EGLR])
    fw.pop_scope()


def dn_head(self, h, hb):
    fw, nc = self.fw, self.nc
    SEQ = self.SEQ
    groups = groups_of(self.NSEQ, SEQ)
    pg, pgR = hb["pg"]
    slA = Slots(pg, pgR)
    slD = Slots(*hb["pd"])
    PFq = self.PF["qkv"]
    (S32, S32R), (Sb, SbR), (Sm, SmR) = hb["S32"], hb["Sb"], hb["Sm"]
    B = lambda k: hb[k]
    for seg in range(3):
        for j in range(4):
            dg, dgR = hb["dg"][seg][j]
            fw.pool.op(lambda: nc.gpsimd.tensor_scalar(out=dg[:], in0=self.idf[:], scalar1=self.cw[:, seg * 8 + h, j:j + 1],
                                                        scalar2=None, op0=ALU.mult),
                       reads=[self.idfR, self.cwR], writes=[dgR])
    yield
    for (n0, N) in groups:
        is_meta = n0 == 0
        seq_start = (not is_meta) and (n0 - 128) % SEQ == 0
        if is_meta:
            fw.pool.op(lambda: nc.gpsimd.memset(S32[:], 0.0), writes=[S32R])
            fw.pool.op(lambda: nc.gpsimd.memset(Sb[:], 0.0), writes=[SbR])
        elif seq_start:
            fw.pool.op(lambda: nc.gpsimd.tensor_copy(out=S32[:], in_=Sm[:]), reads=[SmR], writes=[S32R])
            fw.act.op(lambda: nc.scalar.copy(out=Sb[:], in_=Sm[:]), reads=[SmR], writes=[SbR])
        th, thR = B("th")
        for seg in range(3):
            raw, rawR = hb["raw"][seg]
            rows = slice(seg * 1024 + h * 128, seg * 1024 + (h + 1) * 128)
            if is_meta:
                fw.pool.op(lambda: nc.gpsimd.memset(raw[:, 0:4], 0.0), writes=[rawR])
                fw.sp.dma(raw[:, 3:3 + N], PFq[rows, 0:N], rawR, "w")
            elif seq_start:
                fw.sp.dma(raw[:, 0:3], PFq[rows, 125:128], rawR, "w")
                fw.sp.dma(raw[:, 3:3 + N], PFq[rows, n0:n0 + N], rawR, "w", part=True)
            else:
                fw.sp.dma(raw[:, 0:3 + N], PFq[rows, n0 - 3:n0 + N], rawR, "w")
            for j in range(4):
                dg, dgR = hb["dg"][seg][j]
                fw.pe.op(lambda: nc.tensor.matmul(pg[:, :N], lhsT=dg[:], rhs=raw[:, j:j + N], start=(j == 0), stop=(j == 3)),
                         reads=[dgR, rawR], writes=[pgR])
            yield
            fw.act.op(lambda: nc.scalar.activation(out=th[:, :N], in_=pg[:, :N], func=AF.Tanh, scale=0.5),
                      reads=[pgR], writes=[thR])
            c2, c2R = hb["c2"][seg]
            fw.dve.op(lambda: nc.vector.scalar_tensor_tensor(out=c2[:, :N], in0=th[:, :N], scalar=1.0, in1=pg[:, :N],
                                                             op0=ALU.add, op1=ALU.mult), reads=[thR, pgR], writes=[c2R])
            yield
        sq, sqR = B("sq")
        ssb, ssbR = B("ssb")
        rn, rnR = B("rn")
        nT = {}
        for seg in range(2):
            c2, c2R = hb["c2"][seg]
            fw.act.op(lambda: nc.scalar.activation(out=sq[:, :N], in_=c2[:, :N], func=AF.Square), reads=[c2R], writes=[sqR])
            fw.pe.op(lambda: nc.tensor.matmul(pg[:, :N], lhsT=self.onesb[:], rhs=sq[:, :N], start=True, stop=True),
                     reads=[self.onesbR, sqR], writes=[pgR])
            yield
            fw.act.op(lambda: nc.scalar.activation(out=ssb[:, :N], in_=pg[:, :N], func=AF.Sqrt, bias=self.eps4[:, 0:1]),
                      reads=[pgR, self.epsR], writes=[ssbR])
            fw.dve.op(lambda: nc.vector.reciprocal(out=rn[:, :N], in_=ssb[:, :N]), reads=[ssbR], writes=[rnR])
            nt, ntR = hb["nT"][seg]
            sc = 128 ** -0.5 if seg == 0 else 1.0
            fw.dve.op(lambda: nc.vector.scalar_tensor_tensor(out=nt[:, :N], in0=c2[:, :N], scalar=sc, in1=rn[:, :N],
                                                             op0=ALU.mult, op1=ALU.mult), reads=[c2R, rnR], writes=[ntR])
            yield
        (qnT, qnR), (knT, knR) = hb["nT"]
        vcT, vcR = hb["c2"][2]
        oT, oTR = B("oT")
        for tl in range(N // 128):
            ti = n0 // 128 + tl
            cs = slice(tl * 128, (tl + 1) * 128)
            gB, gBR = B("gB")
            ngB, ngBR = B("ngB")
            fw.act.op(lambda: nc.scalar.mul(out=gB[:], in_=self.onesf[:], mul=self.G[:, ti, h:h + 1]),
                      reads=[self.onesfR, self.GR_], writes=[gBR])
            fw.act.op(lambda: nc.scalar.mul(out=ngB[:], in_=self.onesf[:], mul=self.NG[:, ti, h:h + 1]),
                      reads=[self.onesfR, self.NGR], writes=[ngBR])
            s_kk, s_kkR = slD.next()
            fw.pe.op(lambda: nc.tensor.matmul(s_kk, lhsT=knT[:, cs], rhs=knT[:, cs], start=True, stop=True),
                     reads=[knR], writes=[s_kkR])
            s_qk, s_qkR = slD.next()
            fw.pe.op(lambda: nc.tensor.matmul(s_qk, lhsT=qnT[:, cs], rhs=knT[:, cs], start=True, stop=True),
                     reads=[qnR, knR], writes=[s_qkR])
            s_df, s_dfR = slD.next()
            fw.pe.op(lambda: nc.tensor.matmul(s_df, lhsT=self.tri, rhs=gB[:], start=True, stop=False),
                     reads=[self.cstR, gBR], writes=[s_dfR])
            fw.pe.op(lambda: nc.tensor.matmul(s_df, lhsT=ngB[:], rhs=self.tri, start=False, stop=True),
                     reads=[self.cstR, ngBR], writes=[s_dfR])
            s_gc, s_gcR = slA.next()
            fw.pe.op(lambda: nc.tensor.matmul(s_gc, lhsT=gB[:], rhs=self.tri, start=True, stop=True),
                     reads=[self.cstR, gBR], writes=[s_gcR])
            yield
            dmf, dmfR = B("dmf")
            Dm, DmR = B("Dm")
            Ds, DsR = B("Ds")
            egr, egrR = B("egr")
            qdT, qdR = B("qdT")
            fw.dve.op(lambda: nc.vector.tensor_scalar(out=dmf[:], in0=s_df, scalar1=0.0, scalar2=None, op0=ALU.min),
                      reads=[s_dfR], writes=[dmfR])
            fw.act.op(lambda: nc.scalar.activation(out=dmf[:], in_=dmf[:], func=AF.Exp), reads=[dmfR], writes=[dmfR])
            fw.pool.op(lambda: nc.gpsimd.tensor_tensor(out=Dm[:], in0=dmf[:], in1=self.mlow, op=ALU.mult),
                       reads=[dmfR, self.cstR], writes=[DmR])
            fw.pool.op(lambda: nc.gpsimd.tensor_tensor(out=Ds[:], in0=Dm[:], in1=self.idf[:], op=ALU.subtract),
                       reads=[DmR, self.idfR], writes=[DsR])
            fw.act.op(lambda: nc.scalar.activation(out=egr[:], in_=s_gc, func=AF.Exp), reads=[s_gcR], writes=[egrR])
            fw.dve.op(lambda: nc.vector.tensor_tensor(out=qdT[:], in0=qnT[:, cs], in1=egr[:], op=ALU.mult),
                      reads=[qnR, egrR], writes=[qdR])
            P, PR = hb["Pm"][0]
            fw.dve.op(lambda: nc.vector.scalar_tensor_tensor(out=P[:], in0=s_kk, scalar=self.BETA[:, ti, h:h + 1], in1=Ds[:],
                                                             op0=ALU.mult, op1=ALU.mult),
                      reads=[s_kkR, self.BETAR, DsR], writes=[PR])
            QK, QKR = B("QK")
            fw.dve.op(lambda: nc.vector.tensor_tensor(out=QK[:], in0=s_qk, in1=Dm[:], op=ALU.mult),
                      reads=[s_qkR, DmR], writes=[QKR])
            yield
            t_p, t_pR = slA.next()
            fw.pe.op(lambda: nc.tensor.transpose(out=bf16_view(t_p), in_=P[:], identity=self.idb[:]),
                     reads=[PR, self.idbR], writes=[t_pR])
            t_qk, t_qkR = slA.next()
            fw.pe.op(lambda: nc.tensor.transpose(out=bf16_view(t_qk), in_=QK[:], identity=self.idb[:]),
                     reads=[QKR, self.idbR], writes=[t_qkR])
            t_k, t_kR = slA.next()
            fw.pe.op(lambda: nc.tensor.transpose(out=bf16_view(t_k), in_=knT[:, cs], identity=self.idb[:]),
                     reads=[knR, self.idbR], writes=[t_kR])
            t_v, t_vR = slA.next()
            fw.pe.op(lambda: nc.tensor.transpose(out=bf16_view(t_v), in_=vcT[:, cs], identity=self.idb[:]),
                     reads=[vcR, self.idbR], writes=[t_vR])
            yield
            Q, QR = hb["Qm"][0]
            X, XR = hb["Xm"][0]
            QKT, QKTR = B("QKT")
            kb, kbR = B("kb")
            kdec, kdecR = B("kdec")
            vb, vbR = B("vb")
            fw.act.op(lambda: nc.scalar.copy(out=Q[:], in_=bf16_view(t_p)), reads=[t_pR], writes=[QR])
            fw.dve.op(lambda: nc.vector.tensor_tensor(out=X[:], in0=self.idb[:], in1=Q[:], op=ALU.subtract),
                      reads=[self.idbR, QR], writes=[XR])
            fw.act.op(lambda: nc.scalar.copy(out=QKT[:], in_=bf16_view(t_qk)), reads=[t_qkR], writes=[QKTR])
            fw.act.op(lambda: nc.scalar.mul(out=kb[:], in_=bf16_view(t_k), mul=self.KBS[:, ti, h:h + 1]),
                      reads=[t_kR, self.KBSR], writes=[kbR])
            fw.act.op(lambda: nc.scalar.mul(out=kdec[:], in_=bf16_view(t_k), mul=self.EGR[:, ti, h:h + 1]),
                      reads=[t_kR, self.EGRR], writes=[kdecR])
            fw.act.op(lambda: nc.scalar.mul(out=vb[:], in_=bf16_view(t_v), mul=self.HB[:, ti, h:h + 1]),
                      reads=[t_vR, self.HBR], writes=[vbR])
            yield
            for lvl in range(1, 6):
                Pn, PnR = hb["Pm"][lvl % 2]
                Qn, QnR = hb["Qm"][lvl % 2]
                Xn, XnR = hb["Xm"][lvl % 2]
                if lvl < 5:
                    s_q, s_qR = slD.next()
                    fw.pe.op(lambda: nc.tensor.matmul(s_q, lhsT=P[:], rhs=Q[:], start=True, stop=True),
                             reads=[PR, QR], writes=[s_qR])
                s_p, s_pR = slA.next()
                fw.pe.op(lambda: nc.tensor.matmul(s_p, lhsT=Q[:], rhs=P[:], start=True, stop=True),
                         reads=[PR, QR], writes=[s_pR])
                yield
                fw.act.op(lambda: nc.scalar.copy(out=Pn[:], in_=s_p), reads=[s_pR], writes=[PnR])
                if lvl < 5:
                    fw.dve.op(lambda: nc.vector.tensor_copy(out=Qn[:], in_=s_q), reads=[s_qR], writes=[QnR])
                s_x, s_xR = slD.next()
                fw.pe.op(lambda: nc.tensor.matmul(s_x, lhsT=Pn[:], rhs=X[:], start=True, stop=True),
                         reads=[PnR, XR], writes=[s_xR])
                yield
                fw.dve.op(lambda: nc.vector.tensor_tensor(out=Xn[:], in0=X[:], in1=s_x, op=ALU.add),
                          reads=[XR, s_xR], writes=[XnR])
                P, PR, Q, QR, X, XR = Pn, PnR, Qn, QnR, Xn, XnR
            s_u, s_uR = slA.next()
            fw.pe.op(lambda: nc.tensor.matmul(s_u, lhsT=X[:], rhs=vb[:], start=True, stop=True),
                     reads=[XR, vbR], writes=[s_uR])
            s_w, s_wR = slD.next()
            fw.pe.op(lambda: nc.tensor.matmul(s_w, lhsT=kb[:], rhs=X[:], start=True, stop=True),
                     reads=[XR, kbR], writes=[s_wR])
            yield
            usb, usbR = B("usb")
            nwT, nwTR = B("nwT")
            vnew, vnewR = B("vnew")
            fw.act.op(lambda: nc.scalar.copy(out=usb[:], in_=s_u), reads=[s_uR], writes=[usbR])
            fw.dve.op(lambda: nc.vector.tensor_scalar(out=nwT[:], in0=s_w, scalar1=-1.0, scalar2=None, op0=ALU.mult),
                      reads=[s_wR], writes=[nwTR])
            yield
            for c in range(2):
                r = slice(c * 64, (c + 1) * 64)
                s_ws, s_wsR = slD.next()
                fw.pe.op(lambda: nc.tensor.matmul(s_ws[r, :], lhsT=nwT[:, r], rhs=Sb[:], start=True, stop=True),
                         reads=[nwTR, SbR], writes=[s_wsR])
                yield
                fw.dve.op(lambda: nc.vector.tensor_tensor(out=vnew[r, :], in0=usb[r, :], in1=s_ws[r, :], op=ALU.add),
                          reads=[usbR, s_wsR], writes=[vnewR])
                s_o, s_oR = slA.next()
                fw.pe.op(lambda: nc.tensor.matmul(s_o[:, 0:64], lhsT=Sb[:], rhs=qdT[:, r], start=True, stop=False),
                         reads=[SbR, qdR], writes=[s_oR])
                fw.pe.op(lambda: nc.tensor.matmul(s_o[:, 0:64], lhsT=vnew[r, :], rhs=QKT[r, r], start=False, stop=True),
                         reads=[vnewR, QKTR], writes=[s_oR])
                s_s, s_sR = slD.next()
                fw.pe.op(lambda: nc.tensor.matmul(s_s, lhsT=kdec[r, :], rhs=vnew[r, :], start=True, stop=True),
                         reads=[kdecR, vnewR], writes=[s_sR])
                yield
                fw.act.op(lambda: nc.scalar.copy(out=oT[:, tl * 128 + c * 64:tl * 128 + (c + 1) * 64], in_=s_o[:, 0:64]),
                          reads=[s_oR], writes=[oTR])
                fw.dve.op(lambda: nc.vector.scalar_tensor_tensor(out=S32[:], in0=S32[:], scalar=self.EGL[:, ti, c * 8 + h:c * 8 + h + 1],
                                                                 in1=s_s, op0=ALU.mult, op1=ALU.add),
                          reads=[S32R, self.EGLR, s_sR], writes=[S32R])
                fw.act.op(lambda: nc.scalar.copy(out=Sb[:], in_=S32[:]), reads=[S32R], writes=[SbR])
                yield
        if is_meta:
            fw.pool.op(lambda: nc.gpsimd.tensor_copy(out=Sm[:], in_=S32[:]), reads=[S32R], writes=[SmR])
        fw.act.op(lambda: nc.scalar.activation(out=sq[:, :N], in_=oT[:, :N], func=AF.Square), reads=[oTR], writes=[sqR])
        fw.pe.op(lambda: nc.tensor.matmul(pg[:, :N], lhsT=self.onesb[:], rhs=sq[:, :N], start=True, stop=True),
                 reads=[self.onesbR, sqR], writes=[pgR])
        zt, ztR = B("zt")
        fw.sp.dma(zt[:, :N], self.PF["z"][h * 128:(h + 1) * 128, n0:n0 + N], ztR, "w")
        yield
        fw.act.op(lambda: nc.scalar.activation(out=ssb[:, :N], in_=pg[:, :N], func=AF.Sqrt, scale=1.0 / 128, bias=self.eps1[:, 0:1]),
                  reads=[pgR, self.epsR], writes=[ssbR])
        fw.dve.op(lambda: nc.vector.reciprocal(out=rn[:, :N], in_=ssb[:, :N]), reads=[ssbR], writes=[rnR])
        fw.act.op(lambda: nc.scalar.activation(out=th[:, :N], in_=zt[:, :N], func=AF.Tanh, scale=0.5),
                  reads=[ztR], writes=[thR])
        fw.dve.op(lambda: nc.vector.scalar_tensor_tensor(out=th[:, :N], in0=th[:, :N], scalar=1.0, in1=zt[:, :N],
                                                         op0=ALU.add, op1=ALU.mult), reads=[thR, ztR], writes=[thR])
        fw.dve.op(lambda: nc.vector.scalar_tensor_tensor(out=rn[:, :N], in0=oT[:, :N], scalar=self.nhh[:, 0:1], in1=rn[:, :N],
                                                         op0=ALU.mult, op1=ALU.mult), reads=[oTR, self.nhhR, rnR], writes=[rnR])
        od, odR = B("od")
        fw.dve.op(lambda: nc.vector.tensor_tensor(out=od[:, :N], in0=rn[:, :N], in1=th[:, :N], op=ALU.mult),
                  reads=[rnR, thR], writes=[odR])
        fw.sp.dma(self.OD[h * 128:(h + 1) * 128, n0:n0 + N], od[:, :N], odR, "r")
        yield


def phase3(self, HP=4):
    fw, nc = self.fw, self.nc
    fw.push_scope()
    self.cw, self.cwR = fw.sbuf("cw", [128, 24, 4], F32)
    fw.sp.dma(self.cw[:], self.convT, self.cwR, "w")
    self.nhh, self.nhhR = fw.sbuf("nhh", [128, 1], F32)
    fw.sp.dma(self.nhh[:], self.nh_dn, self.nhhR, "w")
    fw.dve.op(lambda: nc.vector.tensor_scalar(out=self.nhh[:], in0=self.nhh[:], scalar1=0.5, scalar2=None, op0=ALU.mult),
              reads=[self.nhhR], writes=[self.nhhR])
    hbs = []
    for p in range(HP):
        hb = {}
        hb["pg"] = fw.psum(f"pg{p}", [128, 512], F32)
        hb["pd"] = fw.psum(f"pd{p}", [128, 512], F32)
        sq = lambda nm, dt=BF16, w=128: fw.sbuf(f"{nm}{p}", [128, w], dt)
        hb["S32"], hb["Sb"], hb["Sm"] = sq("S32", F32), sq("Sb"), sq("Sm", F32)
        hb["dg"] = [[sq(f"dg{s}{j}_") for j in range(4)] for s in range(3)]
        hb["raw"] = [sq(f"raw{s}_", BF16, 516) for s in range(3)]
        hb["c2"] = [sq("c2q", F32, 512), sq("c2k", F32, 512), sq("c2v", BF16, 512)]
        hb["nT"] = [sq("qnT", BF16, 512), sq("knT", BF16, 512)]
        for nm, dt, w in [("th", F32, 512), ("sq", BF16, 512), ("ssb", F32, 512), ("rn", F32, 512), ("oT", F32, 512),
                          ("zt", BF16, 512), ("od", BF16, 512),
                          ("gB", F32, 128), ("ngB", F32, 128), ("dmf", F32, 128), ("Dm", F32, 128), ("Ds", F32, 128),
                          ("egr", F32, 128), ("qdT", BF16, 128), ("QK", BF16, 128), ("QKT", BF16, 128), ("kb", BF16, 128),
                          ("kdec", BF16, 128), ("vb", BF16, 128), ("usb", F32, 128), ("nwT", BF16, 128), ("vnew", BF16, 128)]:
            hb[nm] = sq(nm, dt, w)
        hb["Pm"] = [sq("Pm0"), sq("Pm1")]
        hb["Qm"] = [sq("Qm0"), sq("Qm1")]
        hb["Xm"] = [sq("Xm0"), sq("Xm1")]
        hbs.append(hb)
    for h0 in range(0, 8, HP):
        run_interleaved([dn_head(self, h0 + p, hbs[p]) for p in range(HP)])
    fw.pop_scope()


MK.p3_setup = p3_setup
MK.load_consts2 = load_consts2
MK.phase3a = phase3a
MK.phase3 = phase3


def p4_setup(self):
    nc = self.nc
    self.w_alpha = nc.dram_tensor("w_alpha", [16, 512], F32, kind="ExternalInput").ap()
    self.nb_alpha = nc.dram_tensor("b_alphaT", [128, 4], F32, kind="ExternalInput").ap()
    self.nh_gla = nc.dram_tensor("nh_glaT", [128, 2], F32, kind="ExternalInput").ap()
    kind = "ExternalOutput" if "og" in self.debug else "Internal"
    self.OG = nc.dram_tensor("og", [1024, self.NT], BF16, kind=kind).ap()


def gla_head(self, h, hb):
    fw, nc = self.fw, self.nc
    SEQ = self.SEQ
    groups = groups_of(self.NSEQ, SEQ)
    pg, pgR = hb["pg"]
    pd, pdR = hb["pd"]
    slA = Slots(pg, pgR)
    slD = Slots(pd, pdR)
    (S32, S32R), (Sb, SbR), (Sm, SmR) = hb["S32"], hb["Sb"], hb["Sm"]
    B = lambda k: hb[k]
    sc = 128 ** -0.5
    for (n0, N) in groups:
        is_meta = n0 == 0
        seq_start = (not is_meta) and (n0 - 128) % SEQ == 0
        nch = N // 64
        ntl = N // 128
        if is_meta:
            fw.pool.op(lambda: nc.gpsimd.memset(S32[:], 0.0), writes=[S32R])
            fw.pool.op(lambda: nc.gpsimd.memset(Sb[:], 0.0), writes=[SbR])
        elif seq_start:
            fw.pool.op(lambda: nc.gpsimd.tensor_copy(out=S32[:], in_=Sm[:]), reads=[SmR], writes=[S32R])
            fw.act.op(lambda: nc.scalar.copy(out=Sb[:], in_=Sm[:]), reads=[SmR], writes=[SbR])
        qt, qtR = B("qt")
        kt, ktR = B("kt")
        lr, lrR = B("lr")
        vt, vtR = B("vt")
        fw.sp.dma(qt[:, :N], self.PF["qg"][h * 128:(h + 1) * 128, n0:n0 + N], qtR, "w")
        fw.sp.dma(kt[:, :N], self.PF["kg"][h * 128:(h + 1) * 128, n0:n0 + N], ktR, "w")
        fw.sp.dma(lr[:, :N], self.PF_LR[:, n0:n0 + N], lrR, "w")
        fw.sp.dma(vt[:, :ntl, :], self.PT_VG[n0:n0 + N, h * 256:(h + 1) * 256].rearrange("(t p) c -> p t c", p=128), vtR, "w")
        fw.pe.op(lambda: nc.tensor.matmul(pg[:, :N], lhsT=self.wal[:, h * 128:(h + 1) * 128], rhs=lr[:, :N], start=True, stop=True),
                 reads=[self.walR, lrR], writes=[pgR])
        yield
        e0, e0R = B("e0")
        cc, ccR = B("cc")
        fw.act.op(lambda: nc.scalar.activation(out=e0[:, :N], in_=pg[:, :N], func=AF.Exp, scale=-1.0, bias=self.nba[:, h:h + 1]),
                  reads=[pgR, self.nbaR], writes=[e0R])
        fw.act.op(lambda: nc.scalar.activation(out=e0[:, :N], in_=e0[:, :N], func=AF.Ln, bias=1.0), reads=[e0R], writes=[e0R])
        fw.dve.op(lambda: nc.vector.tensor_tensor_scan(out=cc[:, :N], data0=self.rmask[:, :N], data1=e0[:, :N], initial=0.0,
                                                       op0=ALU.mult, op1=ALU.add), reads=[self.rmaskR, e0R], writes=[ccR])
        yield
        c3 = cc[:, :N].rearrange("p (c k) -> p c k", k=64)
        d1, d1R = B("d1")
        d13 = d1[:, :N].rearrange("p (c k) -> p c k", k=64)
        ea, eaR = B("ea")
        qg, qgR = B("qg")
        kg, kgR = B("kg")
        qd, qdR = B("qd")
        kd, kdR = B("kd")
        e3, e3R = B("e3")
        fw.dve.op(lambda: nc.vector.tensor_tensor(out=d13, in0=c3, in1=c3[:, :, 31:32].to_broadcast([128, nch, 64]), op=ALU.subtract),
                  reads=[ccR], writes=[d1R])
        fw.act.op(lambda: nc.scalar.activation(out=ea[:, :N], in_=d1[:, :N], func=AF.Exp, scale=-1.0 / 16), reads=[d1R], writes=[eaR])
        fw.dve.op(lambda: nc.vector.scalar_tensor_tensor(out=qg[:, :N], in0=qt[:, :N], scalar=sc, in1=ea[:, :N], op0=ALU.mult, op1=ALU.mult),
                  reads=[qtR, eaR], writes=[qgR])
        fw.act.op(lambda: nc.scalar.activation(out=ea[:, :N], in_=d1[:, :N], func=AF.Exp, scale=1.0 / 16), reads=[d1R], writes=[eaR])
        fw.dve.op(lambda: nc.vector.tensor_tensor(out=kg[:, :N], in0=kt[:, :N], in1=ea[:, :N], op=ALU.mult),
                  reads=[ktR, eaR], writes=[kgR])
        yield
        fw.act.op(lambda: nc.scalar.activation(out=e3[:, :N], in_=cc[:, :N], func=AF.Exp, scale=-1.0 / 16), reads=[ccR], writes=[e3R])
        fw.dve.op(lambda: nc.vector.scalar_tensor_tensor(out=qd[:, :N], in0=qt[:, :N], scalar=sc, in1=e3[:, :N], op0=ALU.mult, op1=ALU.mult),
                  reads=[qtR, e3R], writes=[qdR])
        fw.dve.op(lambda: nc.vector.tensor_tensor(out=d13, in0=c3, in1=c3[:, :, 63:64].to_broadcast([128, nch, 64]), op=ALU.subtract),
                  reads=[ccR], writes=[d1R])
        fw.act.op(lambda: nc.scalar.activation(out=ea[:, :N], in_=d1[:, :N], func=AF.Exp, scale=1.0 / 16), reads=[d1R], writes=[eaR])
        fw.dve.op(lambda: nc.vector.tensor_tensor(out=kd[:, :N], in0=kt[:, :N], in1=ea[:, :N], op=ALU.mult),
                  reads=[ktR, eaR], writes=[kdR])
        yield
        oT = hb["oT"]
        for tl in range(ntl):
            cs = slice(tl * 128, (tl + 1) * 128)
            s_at, s_atR = slD.next()
            fw.pe.op(lambda: nc.tensor.matmul(s_at, lhsT=kg[:, cs], rhs=qg[:, cs], start=True, stop=True),
                     reads=[kgR, qgR], writes=[s_atR])
            t_k, t_kR = slA.next()
            fw.pe.op(lambda: nc.tensor.transpose(out=bf16_view(t_k), in_=kd[:, cs], identity=self.idb[:]),
                     reads=[kdR, self.idbR], writes=[t_kR])
            yield
            am, amR = B("am")
            ktk, ktkR = B("ktk")
            fw.dve.op(lambda: nc.vector.tensor_tensor(out=am[:], in0=s_at, in1=self.tri, op=ALU.mult),
                      reads=[s_atR, self.cstR], writes=[amR])
            fw.act.op(lambda: nc.scalar.copy(out=ktk[:], in_=bf16_view(t_k)), reads=[t_kR], writes=[ktkR])
            yield
            for c in range(2):
                r = slice(c * 64, (c + 1) * 64)
                col = tl * 128 + c * 64
                for hf in range(2):
                    s_o, s_oR = slA.next()
                    fw.pe.op(lambda: nc.tensor.matmul(s_o[:, 0:64], lhsT=Sb[:, hf * 128:(hf + 1) * 128], rhs=qd[:, col:col + 64],
                                                      start=True, stop=False), reads=[SbR, qdR], writes=[s_oR])
                    fw.pe.op(lambda: nc.tensor.matmul(s_o[:, 0:64], lhsT=vt[r, tl, hf * 128:(hf + 1) * 128], rhs=am[r, r],
                                                      start=False, stop=True), reads=[vtR, amR], writes=[s_oR])
                    o_t, o_R = oT[hf]
                    fw.act.op(lambda: nc.scalar.copy(out=o_t[:, col:col + 64], in_=s_o[:, 0:64]), reads=[s_oR], writes=[o_R])
                fw.pe.op(lambda: nc.tensor.matmul(pd[:, 0:256], lhsT=ktk[r, :], rhs=vt[r, tl, :], start=True, stop=True),
                         reads=[ktkR, vtR], writes=[pdR])
                yield
                fw.dve.op(lambda: nc.vector.scalar_tensor_tensor(out=S32[:], in0=S32[:], scalar=e3[:, col + 63:col + 64], in1=pd[:, 0:256],
                                                                 op0=ALU.mult, op1=ALU.add), reads=[S32R, e3R, pdR], writes=[S32R])
                fw.act.op(lambda: nc.scalar.copy(out=Sb[:], in_=S32[:]), reads=[S32R], writes=[SbR])
                yield
        if is_meta:
            fw.pool.op(lambda: nc.gpsimd.tensor_copy(out=Sm[:], in_=S32[:]), reads=[S32R], writes=[SmR])
        sq = hb["sq"]
        for hf in range(2):
            o_t, o_R = oT[hf]
            s_t, s_R = sq[hf]
            fw.act.op(lambda: nc.scalar.activation(out=s_t[:, :N], in_=o_t[:, :N], func=AF.Square), reads=[o_R], writes=[s_R])
            fw.pe.op(lambda: nc.tensor.matmul(pg[:, :N], lhsT=self.onesb[:], rhs=s_t[:, :N], start=(hf == 0), stop=(hf == 1)),
                     reads=[self.onesbR, s_R], writes=[pgR])
        yield
        ssb, ssbR = B("ssb")
        rn, rnR = B("rn")
        fw.act.op(lambda: nc.scalar.activation(out=ssb[:, :N], in_=pg[:, :N], func=AF.Sqrt, scale=1.0 / 256, bias=self.eps1[:, 0:1]),
                  reads=[pgR, self.epsR], writes=[ssbR])
        fw.dve.op(lambda: nc.vector.reciprocal(out=rn[:, :N], in_=ssb[:, :N]), reads=[ssbR], writes=[rnR])
        for hf in range(2):
            o_t, o_R = oT[hf]
            rt, rtR = B("rt")
            th, thR = B("th")
            og, ogR = B("og")
            rows = slice(h * 256 + hf * 128, h * 256 + (hf + 1) * 128)
            fw.sp.dma(rt[:, :N], self.PF["rg"][rows, n0:n0 + N], rtR, "w")
            fw.act.op(lambda: nc.scalar.activation(out=th[:, :N], in_=rt[:, :N], func=AF.Tanh, scale=0.5), reads=[rtR], writes=[thR])
            fw.dve.op(lambda: nc.vector.scalar_tensor_tensor(out=th[:, :N], in0=th[:, :N], scalar=1.0, in1=rt[:, :N], op0=ALU.add, op1=ALU.mult),
                      reads=[thR, rtR], writes=[thR])
            fw.dve.op(lambda: nc.vector.scalar_tensor_tensor(out=ssb[:, :N], in0=o_t[:, :N], scalar=self.nhg[:, hf:hf + 1], in1=rn[:, :N],
                                                             op0=ALU.mult, op1=ALU.mult), reads=[o_R, self.nhgR, rnR], writes=[ssbR])
            fw.dve.op(lambda: nc.vector.tensor_tensor(out=og[:, :N], in0=ssb[:, :N], in1=th[:, :N], op=ALU.mult),
                      reads=[ssbR, thR], writes=[ogR])
            fw.sp.dma(self.OG[rows, n0:n0 + N], og[:, :N], ogR, "r")
            yield


def gla_stream(self, h, hb, seqs, bk):
    fw, nc = self.fw, self.nc
    SEQ = self.SEQ
    groups = [(0, 128)] + [g_ for g_ in groups_of(self.NSEQ, SEQ, with_meta=False, gmax=256) if (g_[0] - 128) // SEQ in seqs]
    (S32, S32R), (Sb, SbR), (Sm, SmR) = hb["S32"], hb["Sb"], hb["Sm"]
    B = lambda k: hb[k]
    sc = 128 ** -0.5
    for (n0, N) in groups:
        is_meta = n0 == 0
        seq_start = (not is_meta) and (n0 - 128) % SEQ == 0
        nch = N // 64
        ntl = N // 128
        if is_meta:
            fw.pool.op(lambda: nc.gpsimd.memset(S32[:], 0.0), writes=[S32R])
            fw.pool.op(lambda: nc.gpsimd.memset(Sb[:], 0.0), writes=[SbR])
        elif seq_start:
            fw.pool.op(lambda: nc.gpsimd.tensor_copy(out=S32[:], in_=Sm[:]), reads=[SmR], writes=[S32R])
            fw.act.op(lambda: nc.scalar.copy(out=Sb[:], in_=Sm[:]), reads=[SmR], writes=[SbR])
        qt, qtR = B("qt")
        kt, ktR = B("kt")
        lr, lrR = B("lr")
        vt, vtR = B("vt")
        fw.sp.dma(qt[:, :N], self.PF["qg"][h * 128:(h + 1) * 128, n0:n0 + N], qtR, "w")
        fw.sp.dma(kt[:, :N], self.PF["kg"][h * 128:(h + 1) * 128, n0:n0 + N], ktR, "w")
        fw.sp.dma(lr[:, :N], self.PF_LR[:, n0:n0 + N], lrR, "w")
        fw.sp.dma(vt[:, :ntl, :], self.PT_VG[n0:n0 + N, h * 256:(h + 1) * 256].rearrange("(t p) c -> p t c", p=128), vtR, "w")
        g1 = []
        yield from acq(bk, 1, g1)
        pg, pgR = g1[0]
        fw.pe.op(lambda: nc.tensor.matmul(pg[:, :N], lhsT=self.wal[:, h * 128:(h + 1) * 128], rhs=lr[:, :N], start=True, stop=True),
                 reads=[self.walR, lrR], writes=[pgR])
        yield
        e0, e0R = B("e0")
        cc, ccR = B("cc")
        fw.act.op(lambda: nc.scalar.activation(out=e0[:, :N], in_=pg[:, :N], func=AF.Exp, scale=-1.0, bias=self.nba[:, h:h + 1]),
                  reads=[pgR, self.nbaR], writes=[e0R])
        bk.release(g1)
        fw.act.op(lambda: nc.scalar.activation(out=e0[:, :N], in_=e0[:, :N], func=AF.Ln, bias=1.0), reads=[e0R], writes=[e0R])
        fw.dve.op(lambda: nc.vector.tensor_tensor_scan(out=cc[:, :N], data0=self.rmask[:, :N], data1=e0[:, :N], initial=0.0,
                                                       op0=ALU.mult, op1=ALU.add), reads=[self.rmaskR, e0R], writes=[ccR])
        yield
        c3 = cc[:, :N].rearrange("p (c k) -> p c k", k=64)
        d1, d1R = B("d1")
        d13 = d1[:, :N].rearrange("p (c k) -> p c k", k=64)
        ea, eaR = B("ea")
        qg, qgR = B("qg")
        kg, kgR = B("kg")
        qd, qdR = B("qd")
        kd, kdR = B("kd")
        e3, e3R = B("e3")
        fw.dve.op(lambda: nc.vector.tensor_tensor(out=d13, in0=c3, in1=c3[:, :, 31:32].to_broadcast([128, nch, 64]), op=ALU.subtract),
                  reads=[ccR], writes=[d1R])
        fw.act.op(lambda: nc.scalar.activation(out=ea[:, :N], in_=d1[:, :N], func=AF.Exp, scale=-1.0 / 16), reads=[d1R], writes=[eaR])
        fw.dve.op(lambda: nc.vector.scalar_tensor_tensor(out=qg[:, :N], in0=qt[:, :N], scalar=sc, in1=ea[:, :N], op0=ALU.mult, op1=ALU.mult),
                  reads=[qtR, eaR], writes=[qgR])
        fw.act.op(lambda: nc.scalar.activation(out=ea[:, :N], in_=d1[:, :N], func=AF.Exp, scale=1.0 / 16), reads=[d1R], writes=[eaR])
        fw.dve.op(lambda: nc.vector.tensor_tensor(out=kg[:, :N], in0=kt[:, :N], in1=ea[:, :N], op=ALU.mult),
                  reads=[ktR, eaR], writes=[kgR])
        yield
        fw.act.op(lambda: nc.scalar.activation(out=e3[:, :N], in_=cc[:, :N], func=AF.Exp, scale=-1.0 / 16), reads=[ccR], writes=[e3R])
        fw.dve.op(lambda: nc.vector.scalar_tensor_tensor(out=qd[:, :N], in0=qt[:, :N], scalar=sc, in1=e3[:, :N], op0=ALU.mult, op1=ALU.mult),
                  reads=[qtR, e3R], writes=[qdR])
        fw.dve.op(lambda: nc.vector.tensor_tensor(out=d13, in0=c3, in1=c3[:, :, 63:64].to_broadcast([128, nch, 64]), op=ALU.subtract),
                  reads=[ccR], writes=[d1R])
        fw.act.op(lambda: nc.scalar.activation(out=ea[:, :N], in_=d1[:, :N], func=AF.Exp, scale=1.0 / 16), reads=[d1R], writes=[eaR])
        fw.dve.op(lambda: nc.vector.tensor_tensor(out=kd[:, :N], in0=kt[:, :N], in1=ea[:, :N], op=ALU.mult),
                  reads=[ktR, eaR], writes=[kdR])
        yield
        oT = hb["oT"]
        for tl in range(ntl):
            cs = slice(tl * 128, (tl + 1) * 128)
            g2 = []
            yield from acq(bk, 2, g2)
            (b_at, s_atR), (b_tk, t_kR) = g2
            s_at, t_k = b_at[:, 0:128], b_tk[:, 0:128]
            fw.pe.op(lambda: nc.tensor.matmul(s_at, lhsT=kg[:, cs], rhs=qg[:, cs], start=True, stop=True),
                     reads=[kgR, qgR], writes=[s_atR])
            fw.pe.op(lambda: nc.tensor.transpose(out=bf16_view(t_k), in_=kd[:, cs], identity=self.idb[:]),
                     reads=[kdR, self.idbR], writes=[t_kR])
            yield
            am, amR = B("am")
            ktk, ktkR = B("ktk")
            fw.dve.op(lambda: nc.vector.tensor_tensor(out=am[:], in0=s_at, in1=self.tri, op=ALU.mult),
                      reads=[s_atR, self.cstR], writes=[amR])
            fw.act.op(lambda: nc.scalar.copy(out=ktk[:], in_=bf16_view(t_k)), reads=[t_kR], writes=[ktkR])
            bk.release(g2)
            yield
            for c in range(2):
                r = slice(c * 64, (c + 1) * 64)
                col = tl * 128 + c * 64
                g3 = []
                yield from acq(bk, 2, g3)
                (b_o, s_oR), (pd, pdR) = g3
                for hf in range(2):
                    s_o = b_o[:, hf * 128:(hf + 1) * 128]
                    fw.pe.op(lambda: nc.tensor.matmul(s_o[:, 0:64], lhsT=Sb[:, hf * 128:(hf + 1) * 128], rhs=qd[:, col:col + 64],
                                                      start=True, stop=False), reads=[SbR, qdR], writes=[s_oR])
                    fw.pe.op(lambda: nc.tensor.matmul(s_o[:, 0:64], lhsT=vt[r, tl, hf * 128:(hf + 1) * 128], rhs=am[r, r],
                                                      start=False, stop=True), reads=[vtR, amR], writes=[s_oR])
                    o_t, o_R = oT[hf]
                    fw.act.op(lambda: nc.scalar.copy(out=o_t[:, col:col + 64], in_=s_o[:, 0:64]), reads=[s_oR], writes=[o_R])
                fw.pe.op(lambda: nc.tensor.matmul(pd[:, 0:256], lhsT=ktk[r, :], rhs=vt[r, tl, :], start=True, stop=True),
                         reads=[ktkR, vtR], writes=[pdR])
                yield
                fw.dve.op(lambda: nc.vector.scalar_tensor_tensor(out=S32[:], in0=S32[:], scalar=e3[:, col + 63:col + 64], in1=pd[:, 0:256],
                                                                 op0=ALU.mult, op1=ALU.add), reads=[S32R, e3R, pdR], writes=[S32R])
                fw.act.op(lambda: nc.scalar.copy(out=Sb[:], in_=S32[:]), reads=[S32R], writes=[SbR])
                bk.release(g3)
                yield
        if is_meta:
            fw.pool.op(lambda: nc.gpsimd.tensor_copy(out=Sm[:], in_=S32[:]), reads=[S32R], writes=[SmR])
        sq = hb["sq"]
        g4 = []
        yield from acq(bk, 1, g4)
        pg, pgR = g4[0]
        for hf in range(2):
            o_t, o_R = oT[hf]
            s_t, s_R = sq[hf]
            fw.act.op(lambda: nc.scalar.activation(out=s_t[:, :N], in_=o_t[:, :N], func=AF.Square), reads=[o_R], writes=[s_R])
            fw.pe.op(lambda: nc.tensor.matmul(pg[:, :N], lhsT=self.onesb[:], rhs=s_t[:, :N], start=(hf == 0), stop=(hf == 1)),
                     reads=[self.onesbR, s_R], writes=[pgR])
        yield
        ssb, ssbR = B("ssb")
        rn, rnR = B("rn")
        fw.act.op(lambda: nc.scalar.activation(out=ssb[:, :N], in_=pg[:, :N], func=AF.Sqrt, scale=1.0 / 256, bias=self.eps1[:, 0:1]),
                  reads=[pgR, self.epsR], writes=[ssbR])
        bk.release(g4)
        fw.dve.op(lambda: nc.vector.reciprocal(out=rn[:, :N], in_=ssb[:, :N]), reads=[ssbR], writes=[rnR])
        for hf in range(2):
            o_t, o_R = oT[hf]
            rt, rtR = B("rt")
            th, thR = B("th")
            og, ogR = B("og")
            rows = slice(h * 256 + hf * 128, h * 256 + (hf + 1) * 128)
            fw.sp.dma(rt[:, :N], self.PF["rg"][rows, n0:n0 + N], rtR, "w")
            fw.act.op(lambda: nc.scalar.activation(out=th[:, :N], in_=rt[:, :N], func=AF.Tanh, scale=0.5), reads=[rtR], writes=[thR])
            fw.dve.op(lambda: nc.vector.scalar_tensor_tensor(out=th[:, :N], in0=th[:, :N], scalar=1.0, in1=rt[:, :N], op0=ALU.add, op1=ALU.mult),
                      reads=[thR, rtR], writes=[thR])
            fw.dve.op(lambda: nc.vector.scalar_tensor_tensor(out=ssb[:, :N], in0=o_t[:, :N], scalar=self.nhg[:, hf:hf + 1], in1=rn[:, :N],
                                                             op0=ALU.mult, op1=ALU.mult), reads=[o_R, self.nhgR, rnR], writes=[ssbR])
            fw.dve.op(lambda: nc.vector.tensor_tensor(out=og[:, :N], in0=ssb[:, :N], in1=th[:, :N], op=ALU.mult),
                      reads=[ssbR, thR], writes=[ogR])
            fw.sp.dma(self.OG[rows, n0:n0 + N], og[:, :N], ogR, "r")
            yield


def phase4(self):
    fw, nc = self.fw, self.nc
    fw.push_scope()
    self.wal, self.walR = fw.sbuf("wal", [16, 512], F32)
    fw.sp.dma(self.wal[:], self.w_alpha, self.walR, "w")
    self.nba, self.nbaR = fw.sbuf("nba", [128, 4], F32)
    fw.sp.dma(self.nba[:], self.nb_alpha, self.nbaR, "w")
    fw.dve.op(lambda: nc.vector.tensor_scalar(out=self.nba[:], in0=self.nba[:], scalar1=-1.0, scalar2=None, op0=ALU.mult),
              reads=[self.nbaR], writes=[self.nbaR])
    self.nhg, self.nhgR = fw.sbuf("nhg", [128, 2], F32)
    fw.sp.dma(self.nhg[:], self.nh_gla, self.nhgR, "w")
    fw.dve.op(lambda: nc.vector.tensor_scalar(out=self.nhg[:], in0=self.nhg[:], scalar1=0.5, scalar2=None, op0=ALU.mult),
              reads=[self.nhgR], writes=[self.nhgR])
    self.rmask, self.rmaskR = fw.sbuf("rmask", [128, 512], F32)
    fw.pool.op(lambda: nc.gpsimd.memset(self.rmask[:], 1.0), writes=[self.rmaskR])
    fw.pool.op(lambda: nc.gpsimd.memset(self.rmask[:].rearrange("p (c k) -> p c k", k=64)[:, :, 0:1], 0.0), writes=[self.rmaskR])
    hbs = []
    for p in range(4):
        hb = {}
        hb["pg"] = fw.psum(f"gpg{p}", [128, 512], F32)
        hb["pd"] = fw.psum(f"gpd{p}", [128, 512], F32)
        sq = lambda nm, dt=BF16, w=512: fw.sbuf(f"g{nm}{p}", [128, w], dt)
        hb["S32"], hb["Sb"], hb["Sm"] = sq("S32", F32, 256), sq("Sb", BF16, 256), sq("Sm", F32, 256)
        hb["qt"], hb["kt"] = sq("qt"), sq("kt")
        hb["lr"] = fw.sbuf(f"glr{p}", [16, 512], F32)
        hb["vt"] = fw.sbuf(f"gvt{p}", [128, 4, 256], BF16)
        for nm in ("e0", "cc", "d1", "ea", "e3", "ssb", "rn", "th"):
            hb[nm] = sq(nm, F32)
        for nm in ("qg", "kg", "qd", "kd", "rt", "og"):
            hb[nm] = sq(nm)
        hb["am"], hb["ktk"] = sq("am", BF16, 128), sq("ktk", BF16, 128)
        hb["oT"] = [sq("oT0", F32), sq("oT1", F32)]
        hb["sq"] = [sq("sq0"), sq("sq1")]
        hbs.append(hb)
    run_interleaved([gla_head(self, p, hbs[p]) for p in range(4)])
    fw.pop_scope()


MK.p4_setup = p4_setup
MK.phase4 = phase4


def p5_setup(self):
    nc = self.nc
    T, CAP = self.T, self.CAP
    inp = lambda nm, shp: nc.dram_tensor(nm, list(shp), F32, kind="ExternalInput").ap()
    self.w_pd = inp("w_proj_dn", [D, D])
    self.w_pg = inp("w_proj_gla", [D, D])
    self.w_o = inp("w_out", [D, D])
    self.g_ffn = inp("g_ffn", [1, D])
    self.g_fin = inp("g_fin", [1, D])
    self.w_rt = inp("w_rt", [D, 72])
    self.b_rt = inp("b_rt", [1, 72])
    self.ebase = inp("ebase", [1, 64])
    self.ustr = inp("ustr", [128, 128])
    self.w1 = inp("w1", [N_EXP, D, 512])
    self.w3 = inp("w3", [N_EXP, D, 512])
    self.w2 = inp("w2", [N_EXP, 512, D])
    scr = lambda nm, shp, dt: nc.dram_tensor(nm, list(shp), dt, kind=("ExternalOutput" if nm in self.debug else "Internal")).ap()
    self.H1 = scr("h1", [T, D], F32)
    self.XS = scr("xs", [N_EXP * CAP, D], BF16)
    self.YS = scr("ys", [N_EXP * CAP, D], F32)
    self.DBG = scr("dbg", [T, 8], F32)


def phase5(self):
    fw, nc = self.fw, self.nc
    T, CAP, NT = self.T, self.CAP, self.NT
    TT = T // 128
    self.SLOT, self.SLOTR = fw.sbuf("SLOT", [128, TT, 2], I32)
    self.bc_reg = nc.gpsimd.to_reg(N_EXP * CAP - 1)
    self.GATE, self.GATER = fw.sbuf("GATE", [128, TT, 2], F32)
    fw.push_scope()
    wv = lambda w: w.rearrange("(kc p) c -> p kc c", p=128)
    wpd, wpdR = fw.sbuf("wpd", [128, 8, D], BF16)
    wpg, wpgR = fw.sbuf("wpg", [128, 8, D], BF16)
    wo, woR = fw.sbuf("wo", [128, 8, D], BF16)
    for t_, R_, w_ in ((wpd, wpdR, self.w_pd), (wpg, wpgR, self.w_pg), (wo, woR, self.w_o)):
        for kc in range(8):
            fw.pool.dma(t_[:, kc, :], wv(w_)[:, kc, :], R_, "w", part=True)
    wr, wrR = fw.sbuf("wr", [128, 8, 72], F32)
    fw.sp.dma(wr[:], wv(self.w_rt), wrR, "w")
    br, brR = fw.sbuf("br", [128, 72], F32)
    fw.sp.dma(br[:], self.b_rt.partition_broadcast(128), brR, "w")
    gf, gfR = fw.sbuf("gf", [128, D], F32)
    fw.sp.dma(gf[:], self.g_ffn.partition_broadcast(128), gfR, "w")
    eb, ebR = fw.sbuf("eb", [128, 64], F32)
    fw.sp.dma(eb[:], self.ebase.partition_broadcast(128), ebR, "w")
    us, usR = fw.sbuf("us", [128, 128], BF16)
    fw.pool.dma(us[:], self.ustr, usR, "w")
    zt, ztR = fw.sbuf("zt5", [128, 2048], BF16)
    fw.pool.op(lambda: nc.gpsimd.memset(zt[:], 0.0), writes=[ztR])
    nrows = N_EXP * CAP
    xsR = fw.res("xs_dram")
    r0 = 0
    while r0 < nrows:
        nr = min(256, nrows - r0)
        fw.sp.dma(self.XS[r0:r0 + nr, :].rearrange("(p a) c -> p (a c)", p=128), zt[:, :(nr // 128) * D], ztR, "r", part=True,
                  extra_writes=[xsR])
        r0 += nr
    Csum, CsumR = fw.sbuf("Csum", [128, 64], F32)
    Csb, CsbR = fw.sbuf("Csb", [128, 64], BF16)
    fw.pool.op(lambda: nc.gpsimd.memset(Csum[:], 0.0), writes=[CsumR])
    fw.pool.op(lambda: nc.gpsimd.memset(Csb[:], 0.0), writes=[CsbR])
    odb = [fw.sbuf(f"odb{i}", [128, 8, 512], BF16) for i in range(2)]
    ogb = [fw.sbuf(f"ogb{i}", [128, 8, 512], BF16) for i in range(2)]
    mg, mgR = fw.sbuf("mg", [128, 8, 512], BF16)
    gdb = [fw.sbuf(f"gdb{i}", [128, 512], BF16) for i in range(2)]
    ggb = [fw.sbuf(f"ggb{i}", [128, 512], BF16) for i in range(2)]
    sgd = [fw.sbuf(f"sgd{i}", [128, 512], F32) for i in range(1)] * 2
    sgg = [fw.sbuf(f"sgg{i}", [128, 512], F32) for i in range(1)] * 2
    m1b = [fw.sbuf(f"m1b{i}", [128, 512], F32) for i in range(1)] * 2
    py = [fw.psum(f"py{i}", [128, 512], F32) for i in range(4)]
    pm = [fw.psum(f"pm{i}", [128, 512], F32) for i in range(2)]
    ptr, ptrR = fw.psum("ptr", [128, 4, 128], F32)
    pl, plR = fw.psum("pl", [128, 512], F32)
    xb = [fw.sbuf(f"x5b{i}", [128, D], F32) for i in range(4)]
    h1b = [fw.sbuf(f"h1b{i}", [128, D], F32) for i in range(4)]
    hnb = [fw.sbuf(f"hn5b{i}", [128, D], F32) for i in range(4)]
    hbb = [fw.sbuf(f"hb5b{i}", [128, D], BF16) for i in range(4)]
    SB = []
    NP5 = 4
    for par_ in range(NP5):
        small = lambda nm, w, dt=F32: fw.sbuf(f"{nm}_{par_}", [128, w], dt)
        d_ = {}
        for nm, w, dt in [("ss", 1, F32), ("lg", 72, F32), ("m8", 8, F32), ("ng", 1, F32), ("goh", 8, F32), ("eg", 8, F32), ("se", 1, F32),
                          ("msk", 64, F32), ("ein", 8, F32), ("e8", 8, F32), ("oh", 16, F32), ("dv", 4, F32), ("A12", 128, F32),
                          ("Cb", 64, BF16), ("pos", 64, F32), ("val", 64, F32), ("tmp", 64, F32), ("sl", 2, F32)]:
            d_[nm] = small("s5" + nm, w, dt)
        d_["junk"] = SB[0]["junk"] if SB else fw.sbuf("junk5_s", [128, D], BF16)
        d_["hT"] = fw.sbuf(f"hT5_{par_}", [128, 8, 128], F32)
        SB.append(d_)
    cntb = [0]
    prog = {"g1": 0, "g2": [0] * 4}
    cprog = [0]
    grp = groups_of(self.NSEQ, self.SEQ, with_meta=False)
    mgb = [(mg, mgR), fw.sbuf("mg2", [128, 8, 512], BF16)]

    def g1():
        for gi, (n0, N) in enumerate(grp):
            while min(prog["g2"]) < gi - 1:
                yield
            od, odR = odb[gi % 2]
            og, ogR = ogb[gi % 2]
            fw.sp.dma(od[:, :, :N], self.OD.rearrange("(kc p) n -> p kc n", p=128)[:, :, n0:n0 + N], odR, "w")
            fw.sp.dma(og[:, :, :N], self.OG.rearrange("(kc p) n -> p kc n", p=128)[:, :, n0:n0 + N], ogR, "w")
            for cc in range(8):
                cs = slice(cc * 128, (cc + 1) * 128)
                gd, gdR = gdb[cc % 2]
                gg, ggR = ggb[cc % 2]
                sd, sdR = sgd[cc % 2]
                sg, sgR = sgg[cc % 2]
                m1, m1R = m1b[cc % 2]
                fw.sp.dma(gd[:, :N], self.PF["gd"][cs, n0:n0 + N], gdR, "w")
                fw.sp.dma(gg[:, :N], self.PF["gg"][cs, n0:n0 + N], ggR, "w")
                pyd, pydR = py[cntb[0] % 4]
                pyg, pygR = py[(cntb[0] + 1) % 4]
                cntb[0] += 2
                for kc in range(8):
                    fw.pe.op(lambda: nc.tensor.matmul(pyd[:, :N], lhsT=wpd[:, kc, cs], rhs=od[:, kc, :N], start=(kc == 0), stop=(kc == 7)),
                             reads=[wpdR, odR], writes=[pydR])
                for kc in range(8):
                    fw.pe.op(lambda: nc.tensor.matmul(pyg[:, :N], lhsT=wpg[:, kc, cs], rhs=og[:, kc, :N], start=(kc == 0), stop=(kc == 7)),
                             reads=[wpgR, ogR], writes=[pygR])
                fw.act.op(lambda: nc.scalar.activation(out=sd[:, :N], in_=gd[:, :N], func=AF.Tanh, scale=0.5), reads=[gdR], writes=[sdR])
                fw.act.op(lambda: nc.scalar.activation(out=sg[:, :N], in_=gg[:, :N], func=AF.Tanh, scale=0.5), reads=[ggR], writes=[sgR])
                fw.pool.op(lambda: nc.gpsimd.tensor_scalar(out=sd[:, :N], in0=sd[:, :N], scalar1=0.5, scalar2=0.5, op0=ALU.mult, op1=ALU.add),
                           reads=[sdR], writes=[sdR])
                fw.pool.op(lambda: nc.gpsimd.tensor_scalar(out=sg[:, :N], in0=sg[:, :N], scalar1=0.5, scalar2=0.5, op0=ALU.mult, op1=ALU.add),
                           reads=[sgR], writes=[sgR])
                fw.dve.op(lambda: nc.vector.tensor_tensor(out=m1[:, :N], in0=pyd[:, :N], in1=sd[:, :N], op=ALU.mult),
                          reads=[pydR, sdR], writes=[m1R])
                fw.dve.op(lambda: nc.vector.tensor_tensor(out=sg[:, :N], in0=pyg[:, :N], in1=sg[:, :N], op=ALU.mult),
                          reads=[pygR, sgR], writes=[sgR])
                fw.pool.op(lambda: nc.gpsimd.tensor_tensor(out=mgb[gi % 2][0][:, cc, :N], in0=m1[:, :N], in1=sg[:, :N], op=ALU.add),
                           reads=[m1R, sgR], writes=[mgb[gi % 2][1]])
                yield

            prog["g1"] = gi + 1
            yield

    def g2(par):
        d_ = SB[par]
        (ss, ssR), (lg, lgR), (m8, m8R), (ng, ngR), (goh, gohR), (eg, egR), (se, seR) = [d_[n] for n in ("ss", "lg", "m8", "ng", "goh", "eg", "se")]
        (msk, mskR), (ein, einR), (e8, e8R), (oh, ohR), (dv, dvR), (A12, A12R) = [d_[n] for n in ("msk", "ein", "e8", "oh", "dv", "A12")]
        (Cb, CbR), (pos, posR), (val, valR), (tmp, tmpR), (sl, slR) = [d_[n] for n in ("Cb", "pos", "val", "tmp", "sl")]
        junk, junkR = d_["junk"]
        hT, hTR = d_["hT"]
        for gi, (n0, N) in enumerate(grp):
            while prog["g1"] < gi + 1:
                yield
            for tl in range(par, N // 128, 4):
                tt = (n0 - 128) // 128 + tl
                ts_ = slice(tl * 128, (tl + 1) * 128)
                xt, xR = xb[tt % 4]
                h1, h1R = h1b[tt % 4]
                hn, hnR = hnb[tt % 4]
                hb_, hbR = hbb[tt % 4]
                fw.sp.dma(xt[:], self.x[tt * 128:(tt + 1) * 128, :], xR, "w")
                for hf in range(2):
                    pmm, pmR = pm[hf]
                    for kc in range(8):
                        fw.pe.op(lambda: nc.tensor.matmul(pmm[:], lhsT=mgb[gi % 2][0][:, kc, ts_], rhs=wo[:, kc, hf * 512:(hf + 1) * 512],
                                                          start=(kc == 0), stop=(kc == 7)), reads=[mgb[gi % 2][1], woR], writes=[pmR])
                    fw.dve.op(lambda: nc.vector.tensor_tensor(out=h1[:, hf * 512:(hf + 1) * 512], in0=pmm[:], in1=xt[:, hf * 512:(hf + 1) * 512],
                                                              op=ALU.add), reads=[pmR, xR], writes=[h1R])
                fw.sp.dma(self.H1[tt * 128:(tt + 1) * 128, :], h1[:], h1R, "r")
                yield
                fw.act.op(lambda: nc.scalar.activation(out=junk[:], in_=h1[:], func=AF.Square, accum_out=ss[:]), reads=[h1R], writes=[junkR, ssR])
                fw.dve.op(lambda: nc.vector.tensor_scalar(out=ss[:], in0=ss[:], scalar1=1.0 / D, scalar2=EPS, op0=ALU.mult, op1=ALU.add),
                          reads=[ssR], writes=[ssR])
                fw.act.op(lambda: nc.scalar.sqrt(out=ss[:], in_=ss[:]), reads=[ssR], writes=[ssR])
                fw.dve.op(lambda: nc.vector.reciprocal(out=ss[:], in_=ss[:]), reads=[ssR], writes=[ssR])
                fw.dve.op(lambda: nc.vector.scalar_tensor_tensor(out=hn[:], in0=h1[:], scalar=ss[:], in1=gf[:], op0=ALU.mult, op1=ALU.mult),
                          reads=[h1R, ssR, gfR], writes=[hnR])
                fw.act.op(lambda: nc.scalar.copy(out=hb_[:], in_=hn[:]), reads=[hnR], writes=[hbR])
                for q4 in range(2):
                    for k4 in range(4):
                        kc = q4 * 4 + k4
                        fw.pe.op(lambda: nc.tensor.transpose(out=ptr[:, k4, :], in_=hn[:, kc * 128:(kc + 1) * 128], identity=self.idf[:]),
                                 reads=[hnR, self.idfR], writes=[ptrR])
                    fw.act.op(lambda: nc.scalar.copy(out=hT[:, q4 * 4:(q4 + 1) * 4, :], in_=ptr[:]), reads=[ptrR], writes=[hTR])
                for kc in range(8):
                    fw.pe.op(lambda: nc.tensor.matmul(pl[:, 0:72], lhsT=hT[:, kc, :], rhs=wr[:, kc, :], start=(kc == 0), stop=(kc == 7)),
                             reads=[hTR, wrR], writes=[plR])
                fw.dve.op(lambda: nc.vector.tensor_tensor(out=lg[:], in0=pl[:, 0:72], in1=br[:], op=ALU.add), reads=[plR, brR], writes=[lgR])
                yield
                fw.dve.op(lambda: nc.vector.max(out=m8[:], in_=lg[:, 0:8]), reads=[lgR], writes=[m8R])
                fw.dve.op(lambda: nc.vector.tensor_scalar(out=goh[:], in0=lg[:, 0:8], scalar1=m8[:, 0:1], scalar2=None, op0=ALU.is_equal),
                          reads=[lgR, m8R], writes=[gohR])
                fw.dve.op(lambda: nc.vector.tensor_scalar(out=ng[:], in0=m8[:, 0:1], scalar1=-1.0, scalar2=None, op0=ALU.mult),
                          reads=[m8R], writes=[ngR])
                fw.act.op(lambda: nc.scalar.activation(out=eg[:], in_=lg[:, 0:8], func=AF.Exp, bias=ng[:], accum_out=se[:]),
                          reads=[lgR, ngR], writes=[egR, seR])
                fw.dve.op(lambda: nc.vector.reciprocal(out=se[:], in_=se[:]), reads=[seR], writes=[seR])
                fw.dve.op(lambda: nc.vector.tensor_tensor(out=msk[:].rearrange("p (g e) -> p g e", e=8),
                                                          in0=lg[:, 8:72].rearrange("p (g e) -> p g e", e=8),
                                                          in1=goh[:].unsqueeze(2).to_broadcast([128, 8, 8]), op=ALU.mult),
                          reads=[lgR, gohR], writes=[mskR])
                fw.dve.op(lambda: nc.vector.tensor_reduce(out=ein[:], in_=msk[:].rearrange("p (g e) -> p e g", e=8), axis=AX.X, op=ALU.add),
                          reads=[mskR], writes=[einR])
                fw.dve.op(lambda: nc.vector.max(out=e8[:], in_=ein[:]), reads=[einR], writes=[e8R])
                yield
                for k in range(2):
                    fw.dve.op(lambda: nc.vector.tensor_scalar(out=oh[:, k * 8:(k + 1) * 8], in0=ein[:], scalar1=e8[:, k:k + 1], scalar2=None,
                                                              op0=ALU.is_equal), reads=[einR, e8R], writes=[ohR])
                fw.dve.op(lambda: nc.vector.tensor_tensor(out=dv[:, 0:1], in0=e8[:, 1:2], in1=e8[:, 0:1], op=ALU.subtract),
                          reads=[e8R], writes=[dvR])
                fw.act.op(lambda: nc.scalar.activation(out=dv[:, 1:2], in_=dv[:, 0:1], func=AF.Exp), reads=[dvR], writes=[dvR])
                fw.dve.op(lambda: nc.vector.tensor_scalar(out=dv[:, 2:3], in0=dv[:, 1:2], scalar1=1.0, scalar2=None, op0=ALU.add),
                          reads=[dvR], writes=[dvR])
                fw.dve.op(lambda: nc.vector.reciprocal(out=dv[:, 2:3], in_=dv[:, 2:3]), reads=[dvR], writes=[dvR])
                fw.dve.op(lambda: nc.vector.tensor_tensor(out=dv[:, 3:4], in0=dv[:, 1:2], in1=dv[:, 2:3], op=ALU.mult),
                          reads=[dvR], writes=[dvR])
                fw.dve.op(lambda: nc.vector.tensor_scalar(out=self.GATE[:, tt, :], in0=dv[:, 2:4], scalar1=se[:, 0:1], scalar2=None, op0=ALU.mult),
                          reads=[dvR, seR], writes=[self.GATER])
                for k in range(2):
                    fw.dve.op(lambda: nc.vector.tensor_tensor(out=A12[:, k * 64:(k + 1) * 64].rearrange("p (g e) -> p g e", e=8),
                                                              in0=goh[:].unsqueeze(2).to_broadcast([128, 8, 8]),
                                                              in1=oh[:, k * 8:(k + 1) * 8].unsqueeze(1).to_broadcast([128, 8, 8]), op=ALU.mult),
                              reads=[gohR, ohR], writes=[A12R])
                fw.dve.op(lambda: nc.vector.tensor_tensor(out=Cb[:], in0=A12[:, 0:64], in1=A12[:, 64:128], op=ALU.add), reads=[A12R], writes=[CbR])
                yield
                while cprog[0] < tt:
                    yield
                fw.pe.op(lambda: nc.tensor.matmul(pl[:, 128:192], lhsT=us[:], rhs=Cb[:], start=True, stop=False), reads=[usR, CbR], writes=[plR])
                fw.pe.op(lambda: nc.tensor.matmul(pl[:, 128:192], lhsT=self.onesb[:], rhs=Csb[:], start=False, stop=True),
                         reads=[self.onesbR, CsbR], writes=[plR])
                fw.dve.op(lambda: nc.vector.tensor_tensor(out=pos[:], in0=pl[:, 128:192], in1=eb[:], op=ALU.add), reads=[plR, ebR], writes=[posR])
                fw.dve.op(lambda: nc.vector.tensor_scalar(out=val[:], in0=pl[:, 128:192], scalar1=float(CAP), scalar2=1.0e7, op0=ALU.is_ge, op1=ALU.mult),
                          reads=[plR], writes=[valR])
                fw.dve.op(lambda: nc.vector.tensor_tensor(out=pos[:], in0=pos[:], in1=val[:], op=ALU.add), reads=[posR, valR], writes=[posR])
                fw.dve.op(lambda: nc.vector.tensor_tensor(out=Csum[:], in0=Csum[:], in1=Cb[:], op=ALU.add), reads=[CsumR, CbR], writes=[CsumR])
                fw.act.op(lambda: nc.scalar.copy(out=Csb[:], in_=Csum[:]), reads=[CsumR], writes=[CsbR])
                cprog[0] = tt + 1
                for k in range(2):
                    fw.dve.op(lambda: nc.vector.tensor_tensor(out=tmp[:], in0=A12[:, k * 64:(k + 1) * 64], in1=pos[:], op=ALU.mult),
                              reads=[A12R, posR], writes=[tmpR])
                    fw.dve.op(lambda: nc.vector.reduce_sum(out=sl[:, k:k + 1], in_=tmp[:], axis=AX.X), reads=[tmpR], writes=[slR])
                fw.dve.op(lambda: nc.vector.tensor_copy(out=self.SLOT[:, tt, :], in_=sl[:]), reads=[slR], writes=[self.SLOTR])
                fw.dve.op(lambda: nc.vector.tensor_scalar(out=sl[:], in0=sl[:], scalar1=float(N_EXP * CAP), scalar2=None, op0=ALU.is_lt),
                          reads=[slR], writes=[slR])
                fw.dve.op(lambda: nc.vector.tensor_tensor(out=self.GATE[:, tt, :], in0=self.GATE[:, tt, :], in1=sl[:], op=ALU.mult),
                          reads=[self.GATER, slR], writes=[self.GATER])
                fw.pool.wait_dma(ztR, tokens=[xsR])
                for k in range(2):
                    fw.pool.indirect_dma(hbR, "r", extra_reads=[self.SLOTR, xsR],
                                         out=self.XS[:, :], out_offset=bass.IndirectOffsetOnAxis(ap=self.SLOT[:, tt, k:k + 1], axis=0),
                                         in_=hb_[:, :], in_offset=None, bounds_check=self.bc_reg, oob_is_err=False)
                if "dbg" in self.debug:
                    dbt, dbtR = fw.sbuf(f"dbt{tt}", [128, 8], F32)
                    fw.dve.op(lambda: nc.vector.tensor_copy(out=dbt[:, 0:2], in_=sl[:]), reads=[slR], writes=[dbtR])
                    fw.dve.op(lambda: nc.vector.tensor_copy(out=dbt[:, 2:4], in_=self.GATE[:, tt, :]), reads=[self.GATER], writes=[dbtR])
                    fw.dve.op(lambda: nc.vector.tensor_copy(out=dbt[:, 4:8], in_=lg[:, 0:4]), reads=[lgR], writes=[dbtR])
                    fw.sp.dma(self.DBG[tt * 128:(tt + 1) * 128, :], dbt[:], dbtR, "r")

            prog["g2"][par] = gi + 1
            yield

    run_interleaved([g1()] + [g2(p_) for p_ in range(4)])
    fw.pop_scope()


def phase6(self):
    fw, nc = self.fw, self.nc
    CAP = self.CAP
    fw.push_scope()
    NWB = 3
    w1b = [fw.sbuf(f"w1b{i}", [128, 8, 512], BF16) for i in range(NWB)]
    w3b = [fw.sbuf(f"w3b{i}", [128, 8, 512], BF16) for i in range(NWB)]
    w2b = [fw.sbuf(f"w2b{i}", [128, 4, D], BF16) for i in range(NWB)]
    xsb = [fw.sbuf(f"xsb{i}", [128, D], BF16) for i in range(3)]
    xT, xTR = fw.sbuf("xT6", [128, 8, CAP], BF16)
    hid, hidR = fw.sbuf("hid", [128, 4, CAP], BF16)
    thb = [fw.sbuf(f"th6{i}", [128, CAP], F32) for i in range(2)]
    yb = [fw.sbuf(f"yb{i}", [128, D], F32) for i in range(2)]
    ptb = [fw.psum(f"pt6{i}", [128, 8, 128], BF16) for i in range(2)]
    ph = [fw.psum(f"ph{i}", [128, 512], F32) for i in range(4)]
    pyy = [fw.psum(f"py6{i}", [128, 512], F32) for i in range(2)]
    nb = CAP // 128
    xi = 0
    yi = 0
    si = 0
    for e in range(N_EXP):
        w1t, w1R = w1b[e % NWB]
        w3t, w3R = w3b[e % NWB]
        w2t, w2R = w2b[e % NWB]
        for kc in range(0, 8, 2):
            fw.pool.dma(w1t[:, kc:kc + 2, :], self.w1[e].rearrange("(kc p) f -> p kc f", p=128)[:, kc:kc + 2, :], w1R, "w", part=(kc > 0))
        for kc in range(0, 8, 2):
            fw.pool.dma(w3t[:, kc:kc + 2, :], self.w3[e].rearrange("(kc p) f -> p kc f", p=128)[:, kc:kc + 2, :], w3R, "w", part=(kc > 0))
        for fc in range(4):
            fw.pool.dma(w2t[:, fc, :], self.w2[e].rearrange("(fc p) d -> p fc d", p=128)[:, fc, :], w2R, "w", part=(fc > 0))
        for b in range(nb):
            xs, xsR = xsb[xi % 3]
            pt, ptR = ptb[xi % 2]
            xi += 1
            fw.sp.dma(xs[:], self.XS[e * CAP + b * 128:e * CAP + (b + 1) * 128, :], xsR, "w")
            for kc in range(8):
                fw.pe.op(lambda: nc.tensor.transpose(out=pt[:, kc, :], in_=xs[:, kc * 128:(kc + 1) * 128], identity=self.idb[:]),
                         reads=[xsR, self.idbR], writes=[ptR])
            fw.act.op(lambda: nc.scalar.copy(out=xT[:, :, b * 128:(b + 1) * 128], in_=pt[:]), reads=[ptR], writes=[xTR])
        for fc in range(4):
            fs = slice(fc * 128, (fc + 1) * 128)
            p1, p1R = ph[(2 * fc) % 4]
            p3, p3R = ph[(2 * fc + 1) % 4]
            for kc in range(8):
                fw.pe.op(lambda: nc.tensor.matmul(p1[:, :CAP], lhsT=w1t[:, kc, fs], rhs=xT[:, kc, :], start=(kc == 0), stop=(kc == 7)),
                         reads=[w1R, xTR], writes=[p1R])
            for kc in range(8):
                fw.pe.op(lambda: nc.tensor.matmul(p3[:, :CAP], lhsT=w3t[:, kc, fs], rhs=xT[:, kc, :], start=(kc == 0), stop=(kc == 7)),
                         reads=[w3R, xTR], writes=[p3R])
            th, thR = thb[fc % 2]
            fw.act.op(lambda: nc.scalar.activation(out=th[:], in_=p1[:, :CAP], func=AF.Tanh, scale=0.5), reads=[p1R], writes=[thR])
            fw.dve.op(lambda: nc.vector.scalar_tensor_tensor(out=th[:], in0=th[:], scalar=1.0, in1=p1[:, :CAP], op0=ALU.add, op1=ALU.mult),
                      reads=[thR, p1R], writes=[thR])
            fw.dve.op(lambda: nc.vector.tensor_tensor(out=hid[:, fc, :], in0=th[:], in1=p3[:, :CAP], op=ALU.mult),
                      reads=[thR, p3R], writes=[hidR])
        for b in range(nb):
            y, yR = yb[yi % 2]
            yi += 1
            for hf in range(2):
                pp, ppR = pyy[hf]
                for fc in range(4):
                    fw.pe.op(lambda: nc.tensor.matmul(pp[:], lhsT=hid[:, fc, b * 128:(b + 1) * 128], rhs=w2t[:, fc, hf * 512:(hf + 1) * 512],
                                                      start=(fc == 0), stop=(fc == 3)), reads=[hidR, w2R], writes=[ppR])
                fw.act.op(lambda: nc.scalar.mul(out=y[:, hf * 512:(hf + 1) * 512], in_=pp[:], mul=0.5), reads=[ppR], writes=[yR])
            fw.sp.dma(self.YS[e * CAP + b * 128:e * CAP + (b + 1) * 128, :], y[:], yR, "r")
    fw.pop_scope()


def phase7(self):
    fw, nc = self.fw, self.nc
    T, CAP = self.T, self.CAP
    fw.push_scope()
    gn, gnR = fw.sbuf("gn", [128, D], F32)
    fw.sp.dma(gn[:], self.g_fin.partition_broadcast(128), gnR, "w")
    h1b = [fw.sbuf(f"h7b{i}", [128, D], F32) for i in range(4)]
    y1b = [fw.sbuf(f"y1b{i}", [128, D], F32) for i in range(4)]
    y2b = [fw.sbuf(f"y2b{i}", [128, D], F32) for i in range(4)]
    ob = [fw.sbuf(f"ob{i}", [128, D], F32) for i in range(4)]
    junk, junkR = fw.sbuf("junk7", [128, D], BF16)
    ssb = [fw.sbuf(f"ss7{i}", [128, 1], F32) for i in range(4)]
    for tt in range(T // 128):
        h1, h1R = h1b[tt % 4]
        y1, y1R = y1b[tt % 4]
        y2, y2R = y2b[tt % 4]
        o, oR = ob[tt % 4]
        ss, ssR = ssb[tt % 4]
        fw.sp.dma(h1[:], self.H1[tt * 128:(tt + 1) * 128, :], h1R, "w")
        for k, (yt, yR) in enumerate(((y1, y1R), (y2, y2R))):
            if tt < 4:
                fw.pool.op(lambda: nc.gpsimd.memset(yt[:], 0.0), writes=[yR])
            fw.pool.indirect_dma(yR, "w", extra_reads=[self.SLOTR],
                                 out=yt[:, :], out_offset=None, in_=self.YS[:, :],
                                 in_offset=bass.IndirectOffsetOnAxis(ap=self.SLOT[:, tt, k:k + 1], axis=0),
                                 bounds_check=self.bc_reg, oob_is_err=False)
        fw.dve.op(lambda: nc.vector.scalar_tensor_tensor(out=h1[:], in0=y1[:], scalar=self.GATE[:, tt, 0:1], in1=h1[:], op0=ALU.mult, op1=ALU.add),
                  reads=[y1R, self.GATER, h1R], writes=[h1R])
        fw.dve.op(lambda: nc.vector.scalar_tensor_tensor(out=h1[:], in0=y2[:], scalar=self.GATE[:, tt, 1:2], in1=h1[:], op0=ALU.mult, op1=ALU.add),
                  reads=[y2R, self.GATER, h1R], writes=[h1R])
        fw.act.op(lambda: nc.scalar.activation(out=junk[:], in_=h1[:], func=AF.Square, accum_out=ss[:]), reads=[h1R], writes=[junkR, ssR])
        fw.dve.op(lambda: nc.vector.tensor_scalar(out=ss[:], in0=ss[:], scalar1=1.0 / D, scalar2=EPS, op0=ALU.mult, op1=ALU.add),
                  reads=[ssR], writes=[ssR])
        fw.act.op(lambda: nc.scalar.sqrt(out=ss[:], in_=ss[:]), reads=[ssR], writes=[ssR])
        fw.dve.op(lambda: nc.vector.reciprocal(out=ss[:], in_=ss[:]), reads=[ssR], writes=[ssR])
        fw.dve.op(lambda: nc.vector.scalar_tensor_tensor(out=o[:], in0=h1[:], scalar=ss[:], in1=gn[:], op0=ALU.mult, op1=ALU.mult),
                  reads=[h1R, ssR, gnR], writes=[oR])
        fw.sp.dma(self.out[tt * 128:(tt + 1) * 128, :], o[:], oR, "r")
    fw.pop_scope()


def build_all(self):
    fw = self.fw
    self.p3_setup(); self.p4_setup(); self.p5_setup()
    self.load_consts(); self.load_consts2()
    self.marks = []
    mark = lambda nm: self.marks.append((nm, fw.pe.n, fw.act.n, fw.dve.n, getattr(fw, "sim_time", 0.0)))
    fw.push_scope(); self.phase1(); fw.flush(); mark("p1")
    fw.sched = False; self.phase2(); fw.flush(); fw.sched = True; mark("p2"); fw.pop_scope()
    fw.push_scope(); self.phase3a(); self.phase3a_extra(); mark("p3a"); self.phase3c(); mark("p3"); fw.pop_scope()
    self.phase4(); mark("p4")
    self.phase5(); mark("p5")
    self.phase6(); mark("p6")
    self.phase7(); mark("p7")
    return self.finish()


MK.p5_setup = p5_setup
MK.phase5 = phase5
MK.phase6 = phase6
MK.phase7 = phase7
MK.build_all = build_all


NCORES = 8
NSEQ_CORE = 4
SEQ_LEN = 2048
CAPACITY = 384
_CACHE = {}


def _consts():
    k = np.arange(128)
    same = (k[:, None] // 64) == (k[None, :] // 64)
    ident = np.eye(128, dtype=np.float32)
    tri = (same & (k[:, None] <= k[None, :])).astype(np.float32)
    trs = (same & (k[:, None] > k[None, :])).astype(np.float32)
    mlow = np.ascontiguousarray(tri.T)
    ind = np.stack([(k < 64), (k >= 64)], 1).astype(np.float32)
    return np.ascontiguousarray(np.concatenate([ident, tri, trs, mlow, ind], 1).astype(np.float32))


def kernel(x, meta_tokens, norm_mix, w_in, conv_dn, a_log, dt_bias, norm_head_dn, w_proj_dn,
           w_alpha, b_alpha, norm_head_gla, w_proj_gla, w_out, norm_ffn, w_group, b_group,
           w_router, b_router, w1, w3, w2, norm_final):
    f = lambda a: np.ascontiguousarray(np.asarray(a, dtype=np.float32))
    x = f(x)
    if "nc" not in _CACHE:
        mk = MK(NSEQ_CORE, SEQ_LEN, CAPACITY)
        _CACHE["nc"] = mk.build_all()
    nc = _CACHE["nc"]
    conv = f(conv_dn)[0]
    shared = {
        "meta": f(meta_tokens), "g_mix": f(norm_mix).reshape(1, D), "w_in": f(w_in)[0],
        "ident": np.eye(128, dtype=np.float32),
        "convT": np.ascontiguousarray(conv.T.reshape(24, 128, 4).transpose(1, 0, 2)),
        "a_log": f(a_log).reshape(1, 8), "dt_bias": f(dt_bias).reshape(1, 8),
        "nh_dn": f(norm_head_dn).reshape(128, 1), "consts": _consts(),
        "w_alpha": f(w_alpha)[0], "b_alphaT": np.ascontiguousarray(f(b_alpha)[0].reshape(4, 128).T),
        "nh_glaT": np.ascontiguousarray(f(norm_head_gla)[0].reshape(2, 128).T),
        "w_proj_dn": f(w_proj_dn)[0], "w_proj_gla": f(w_proj_gla)[0], "w_out": f(w_out)[0],
        "g_ffn": f(norm_ffn).reshape(1, D), "g_fin": f(norm_final).reshape(1, D),
        "w_rt": np.ascontiguousarray(np.concatenate([f(w_group)[0], f(w_router)[0]], 1)),
        "b_rt": np.ascontiguousarray(np.concatenate([f(b_group)[0], f(b_router)[0]])[None]),
        "ebase": (np.arange(64, dtype=np.float32) * CAPACITY)[None],
        "ustr": (np.arange(128)[:, None] < np.arange(128)[None, :]).astype(np.float32),
        "w1": f(w1)[0], "w3": f(w3)[0], "w2": f(w2)[0],
    }
    in_maps = []
    for c in range(NCORES):
        m = dict(shared)
        m["x"] = np.ascontiguousarray(x[c * NSEQ_CORE:(c + 1) * NSEQ_CORE].reshape(NSEQ_CORE * SEQ_LEN, D))
        in_maps.append(m)
    res = run_bass_kernel_spmd(nc, in_maps, core_ids=list(range(NCORES)))
    outs = [np.asarray(r["out"], dtype=np.float32).reshape(NSEQ_CORE, SEQ_LEN, D) for r in res.results]
    return np.concatenate(outs, 0)


class Banks:
    def __init__(self, banks):
        self.banks = banks
        self.i = 0

    def next(self):
        t, R = self.banks[self.i % len(self.banks)]
        self.i += 1
        return t, R


class BankPool:
    def __init__(self, banks):
        self.free = list(banks)

    def acquire(self, k):
        if len(self.free) < k:
            return None
        got, self.free = self.free[:k], self.free[k:]
        return got

    def release(self, got):
        self.free.extend(got)


def acq(bk, k, out):
    while True:
        got = bk.acquire(k)
        if got is not None:
            out.extend(got)
            return
        yield


def v4(t):
    return t[:, :].rearrange("p (s c) -> p s c", s=4)


def v4b(t):
    return t[:, :].bitcast(BF16).rearrange("p (s c) -> p s c", s=4)[:, :, 0:128]


def phase3a_extra(self):
    fw, nc = self.fw, self.nc
    NTL = self.NTILES
    kind = "ExternalOutput" if "gct" in self.debug else "Internal"
    self.GCT = nc.dram_tensor("gct", [8, self.NT], F32, kind=kind).ap()
    self.GC, self.GCR = fw.sbuf("GC", [128, NTL, 8], F32)
    fw.push_scope()
    ps, psR = fw.psum("p3x", [128, 8], F32)
    pt, ptR = fw.psum("p3t", [8, 128], F32)
    rb = [fw.sbuf(f"gcrow{i}", [8, 128], F32) for i in range(2)]
    for i in range(NTL):
        fw.pe.op(lambda: nc.tensor.matmul(ps[:, 0:8], lhsT=self.tri, rhs=self.G[:, i, :], start=True, stop=True),
                 reads=[self.cstR, self.GR_], writes=[psR])
        fw.dve.op(lambda: nc.vector.tensor_copy(out=self.GC[:, i, :], in_=ps[:, 0:8]), reads=[psR], writes=[self.GCR])
        fw.pe.op(lambda: nc.tensor.transpose(out=pt[:, :], in_=self.GC[:, i, :], identity=self.idf[:]),
                 reads=[self.GCR, self.idfR], writes=[ptR])
        r, rR = rb[i % 2]
        fw.act.op(lambda: nc.scalar.copy(out=r[:], in_=pt[:]), reads=[ptR], writes=[rR])
        fw.sp.dma(self.GCT[:, i * 128:(i + 1) * 128], r[:], rR, "r")
    fw.pop_scope()


def dn_group(self, g, gb):
    fw, nc = self.fw, self.nc
    SEQ = self.SEQ
    h0 = g * 4
    groups = groups_of(self.NSEQ, SEQ, gmax=256)
    bk = Banks(gb["banks"])
    PFq = self.PF["qkv"]
    B = lambda k: gb[k]
    S32, S32R = B("S32")
    Sb, SbR = B("Sb")
    Sm, SmR = B("Sm")
    bc = lambda ap: ap.unsqueeze(2).to_broadcast([128, 4, 128])
    for b in range(4):
        for seg in range(3):
            for j in range(4):
                dg, dgR = gb["dg"][b][seg][j]
                fw.pool.op(lambda: nc.gpsimd.tensor_scalar(out=dg[:], in0=self.idf[:], scalar1=self.cw[:, seg * 8 + h0 + b, j:j + 1],
                                                            scalar2=None, op0=ALU.mult), reads=[self.idfR, self.cwR], writes=[dgR])
    yield
    qn4, qnR = B("qn4")
    kn4, knR = B("kn4")
    vc4, vcR = B("vc4")
    qd4, qdR = B("qd4")
    oT4, oTR = B("oT4")
    gr4, grR = B("gr4")
    eg4, egR = B("eg4")
    th, thR = B("th")
    sq, sqR = B("sq")
    ssb, ssbR = B("ssb")
    rn, rnR = B("rn")
    for (n0, N) in groups:
        is_meta = n0 == 0
        seq_start = (not is_meta) and (n0 - 128) % SEQ == 0
        if is_meta:
            fw.pool.op(lambda: nc.gpsimd.memset(S32[:], 0.0), writes=[S32R])
            fw.pool.op(lambda: nc.gpsimd.memset(Sb[:], 0.0), writes=[SbR])
        elif seq_start:
            fw.pool.op(lambda: nc.gpsimd.tensor_copy(out=S32[:], in_=Sm[:]), reads=[SmR], writes=[S32R])
            fw.act.op(lambda: nc.scalar.copy(out=Sb[:], in_=Sm[:]), reads=[SmR], writes=[SbR])
        for b in range(4):
            h = h0 + b
            fw.sp.dma(gr4[:, b, :N], self.GCT[h:h + 1, n0:n0 + N].partition_broadcast(128), grR, "w", part=(b > 0))
            for seg in range(3):
                raw, rawR = gb["raw"][seg]
                rows = slice(seg * 1024 + h * 128, seg * 1024 + (h + 1) * 128)
                if is_meta:
                    fw.pool.op(lambda: nc.gpsimd.memset(raw[:, 0:4], 0.0), writes=[rawR])
                    fw.sp.dma(raw[:, 3:3 + N], PFq[rows, 0:N], rawR, "w")
                elif seq_start:
                    fw.sp.dma(raw[:, 0:3], PFq[rows, 125:128], rawR, "w")
                    fw.sp.dma(raw[:, 3:3 + N], PFq[rows, n0:n0 + N], rawR, "w", part=True)
                else:
                    fw.sp.dma(raw[:, 0:3 + N], PFq[rows, n0 - 3:n0 + N], rawR, "w")
                pg, pgR = bk.next()
                for j in range(4):
                    dg, dgR = gb["dg"][b][seg][j]
                    fw.pe.op(lambda: nc.tensor.matmul(pg[:, :N], lhsT=dg[:], rhs=raw[:, j:j + N], start=(j == 0), stop=(j == 3)),
                             reads=[dgR, rawR], writes=[pgR])
                yield
                fw.act.op(lambda: nc.scalar.activation(out=th[:, :N], in_=pg[:, :N], func=AF.Tanh, scale=0.5), reads=[pgR], writes=[thR])
                if seg < 2:
                    c2, c2R = gb["c2"][seg]
                    fw.dve.op(lambda: nc.vector.scalar_tensor_tensor(out=c2[:, :N], in0=th[:, :N], scalar=1.0, in1=pg[:, :N],
                                                                     op0=ALU.add, op1=ALU.mult), reads=[thR, pgR], writes=[c2R])
                else:
                    fw.dve.op(lambda: nc.vector.scalar_tensor_tensor(out=vc4[:, b, :N], in0=th[:, :N], scalar=1.0, in1=pg[:, :N],
                                                                     op0=ALU.add, op1=ALU.mult), reads=[thR, pgR], writes=[vcR])
                yield
            for seg in range(2):
                c2, c2R = gb["c2"][seg]
                fw.act.op(lambda: nc.scalar.activation(out=sq[:, :N], in_=c2[:, :N], func=AF.Square), reads=[c2R], writes=[sqR])
                pg, pgR = bk.next()
                fw.pe.op(lambda: nc.tensor.matmul(pg[:, :N], lhsT=self.onesb[:], rhs=sq[:, :N], start=True, stop=True),
                         reads=[self.onesbR, sqR], writes=[pgR])
                yield
                fw.act.op(lambda: nc.scalar.activation(out=ssb[:, :N], in_=pg[:, :N], func=AF.Sqrt, bias=self.eps4[:, 0:1]),
                          reads=[pgR, self.epsR], writes=[ssbR])
                fw.dve.op(lambda: nc.vector.reciprocal(out=rn[:, :N], in_=ssb[:, :N]), reads=[ssbR], writes=[rnR])
                dst, dstR = (qn4, qnR) if seg == 0 else (kn4, knR)
                sc = 128 ** -0.5 if seg == 0 else 1.0
                fw.pool.op(lambda: nc.gpsimd.tensor_tensor(out=c2[:, :N], in0=c2[:, :N], in1=rn[:, :N], op=ALU.mult),
                           reads=[c2R, rnR], writes=[c2R])
                fw.pool.op(lambda: nc.gpsimd.tensor_scalar(out=dst[:, b, :N], in0=c2[:, :N], scalar1=sc, scalar2=None, op0=ALU.mult),
                           reads=[c2R], writes=[dstR])
                yield
        fw.act.op(lambda: nc.scalar.activation(out=eg4[:, :, :N], in_=gr4[:, :, :N], func=AF.Exp), reads=[grR], writes=[egR])
        fw.dve.op(lambda: nc.vector.tensor_tensor(out=qd4[:, :, :N], in0=qn4[:, :, :N], in1=eg4[:, :, :N], op=ALU.mult),
                  reads=[qnR, egR], writes=[qdR])
        yield
        for tl in range(N // 128):
            ti = n0 // 128 + tl
            cs = slice(tl * 128, (tl + 1) * 128)
            hs = slice(h0, h0 + 4)
            dm, dmR = B("dm")
            Dm, DmR = B("Dm")
            DsB, DsBR = B("DsB")
            fw.pool.op(lambda: nc.gpsimd.tensor_tensor(out=dm[:], in0=gr4[:, :, cs], in1=bc(self.GC[:, ti, hs]), op=ALU.subtract),
                       reads=[grR, self.GCR], writes=[dmR])
            fw.pool.op(lambda: nc.gpsimd.tensor_scalar(out=dm[:], in0=dm[:], scalar1=0.0, scalar2=None, op0=ALU.max),
                       reads=[dmR], writes=[dmR])
            fw.act.op(lambda: nc.scalar.activation(out=dm[:], in_=dm[:], func=AF.Exp, scale=-1.0), reads=[dmR], writes=[dmR])
            fw.pool.op(lambda: nc.gpsimd.tensor_tensor(out=Dm[:], in0=dm[:], in1=self.mlow4[:], op=ALU.mult),
                       reads=[dmR, self.m4R], writes=[DmR])
            fw.pool.op(lambda: nc.gpsimd.tensor_tensor(out=DsB[:], in0=Dm[:], in1=self.id4f[:], op=ALU.subtract),
                       reads=[DmR, self.m4R], writes=[DsBR])
            fw.pool.op(lambda: nc.gpsimd.tensor_tensor(out=DsB[:], in0=DsB[:], in1=bc(self.BETA[:, ti, hs]), op=ALU.mult),
                       reads=[DsBR, self.BETAR], writes=[DsBR])
            p_kk, p_kkR = bk.next()
            for b in range(4):
                fw.pe.op(lambda: nc.tensor.matmul(p_kk[:, b * 128:(b + 1) * 128], lhsT=kn4[:, b, cs], rhs=kn4[:, b, cs], start=True, stop=True),
                         reads=[knR], writes=[p_kkR])
            p_qk, p_qkR = bk.next()
            for b in range(4):
                fw.pe.op(lambda: nc.tensor.matmul(p_qk[:, b * 128:(b + 1) * 128], lhsT=qn4[:, b, cs], rhs=kn4[:, b, cs], start=True, stop=True),
                         reads=[qnR, knR], writes=[p_qkR])
            yield
            P, PR = gb["Pm"][0]
            QK, QKR = B("QK")
            fw.dve.op(lambda: nc.vector.tensor_tensor(out=P[:], in0=v4(p_kk), in1=DsB[:], op=ALU.mult), reads=[p_kkR, DsBR], writes=[PR])
            fw.dve.op(lambda: nc.vector.tensor_tensor(out=QK[:], in0=v4(p_qk), in1=Dm[:], op=ALU.mult), reads=[p_qkR, DmR], writes=[QKR])
            t_p, t_pR = bk.next()
            for b in range(4):
                fw.pe.op(lambda: nc.tensor.transpose(out=v4b(t_p)[:, b, :], in_=P[:, b, :], identity=self.idb[:]),
                         reads=[PR, self.idbR], writes=[t_pR])
            t_qk, t_qkR = bk.next()
            for b in range(4):
                fw.pe.op(lambda: nc.tensor.transpose(out=v4b(t_qk)[:, b, :], in_=QK[:, b, :], identity=self.idb[:]),
                         reads=[QKR, self.idbR], writes=[t_qkR])
            yield
            Q, QR = gb["Qm"][0]
            X, XR = gb["Xm"][0]
            QKT, QKTR = B("QKT")
            fw.act.op(lambda: nc.scalar.copy(out=Q[:], in_=v4b(t_p)), reads=[t_pR], writes=[QR])
            fw.pool.op(lambda: nc.gpsimd.tensor_tensor(out=X[:], in0=self.id4b[:], in1=Q[:], op=ALU.subtract),
                       reads=[self.m4R, QR], writes=[XR])
            fw.act.op(lambda: nc.scalar.copy(out=QKT[:], in_=v4b(t_qk)), reads=[t_qkR], writes=[QKTR])
            t_k, t_kR = bk.next()
            for b in range(4):
                fw.pe.op(lambda: nc.tensor.transpose(out=v4b(t_k)[:, b, :], in_=kn4[:, b, cs], identity=self.idb[:]),
                         reads=[knR, self.idbR], writes=[t_kR])
            t_v, t_vR = bk.next()
            for b in range(4):
                fw.pe.op(lambda: nc.tensor.transpose(out=v4b(t_v)[:, b, :], in_=vc4[:, b, cs], identity=self.idb[:]),
                         reads=[vcR, self.idbR], writes=[t_vR])
            yield
            ktok, ktokR = B("ktok")
            kb, kbR = B("kb")
            kdec, kdecR = B("kdec")
            vb, vbR = B("vb")
            fw.act.op(lambda: nc.scalar.copy(out=ktok[:], in_=v4b(t_k)), reads=[t_kR], writes=[ktokR])
            fw.pool.op(lambda: nc.gpsimd.tensor_tensor(out=kb[:], in0=ktok[:], in1=bc(self.KBS[:, ti, hs]), op=ALU.mult),
                       reads=[ktokR, self.KBSR], writes=[kbR])
            fw.pool.op(lambda: nc.gpsimd.tensor_tensor(out=kdec[:], in0=ktok[:], in1=bc(self.EGR[:, ti, hs]), op=ALU.mult),
                       reads=[ktokR, self.EGRR], writes=[kdecR])
            fw.dve.op(lambda: nc.vector.tensor_tensor(out=vb[:], in0=v4b(t_v), in1=bc(self.HB[:, ti, hs]), op=ALU.mult),
                      reads=[t_vR, self.HBR], writes=[vbR])
            yield
            for lvl in range(1, 6):
                Pn, PnR = gb["Pm"][lvl % 2]
                Qn, QnR = gb["Qm"][lvl % 2]
                Xn, XnR = gb["Xm"][lvl % 2]
                if lvl < 5:
                    s_q, s_qR = bk.next()
                    for b in range(4):
                        fw.pe.op(lambda: nc.tensor.matmul(s_q[:, b * 128:(b + 1) * 128], lhsT=P[:, b, :], rhs=Q[:, b, :], start=True, stop=True),
                                 reads=[PR, QR], writes=[s_qR])
                s_p, s_pR = bk.next()
                for b in range(4):
                    fw.pe.op(lambda: nc.tensor.matmul(s_p[:, b * 128:(b + 1) * 128], lhsT=Q[:, b, :], rhs=P[:, b, :], start=True, stop=True),
                             reads=[PR, QR], writes=[s_pR])
                yield
                fw.act.op(lambda: nc.scalar.copy(out=Pn[:], in_=v4(s_p)), reads=[s_pR], writes=[PnR])
                if lvl < 5:
                    fw.dve.op(lambda: nc.vector.tensor_copy(out=Qn[:], in_=v4(s_q)), reads=[s_qR], writes=[QnR])
                s_x, s_xR = bk.next()
                for b in range(4):
                    fw.pe.op(lambda: nc.tensor.matmul(s_x[:, b * 128:(b + 1) * 128], lhsT=Pn[:, b, :], rhs=X[:, b, :], start=True, stop=True),
                             reads=[PnR, XR], writes=[s_xR])
                yield
                fw.dve.op(lambda: nc.vector.tensor_tensor(out=Xn[:], in0=X[:], in1=v4(s_x), op=ALU.add), reads=[XR, s_xR], writes=[XnR])
                P, PR, Q, QR, X, XR = Pn, PnR, Qn, QnR, Xn, XnR
            s_u, s_uR = bk.next()
            for b in range(4):
                fw.pe.op(lambda: nc.tensor.matmul(s_u[:, b * 128:(b + 1) * 128], lhsT=X[:, b, :], rhs=vb[:, b, :], start=True, stop=True),
                         reads=[XR, vbR], writes=[s_uR])
            s_w, s_wR = bk.next()
            for b in range(4):
                fw.pe.op(lambda: nc.tensor.matmul(s_w[:, b * 128:(b + 1) * 128], lhsT=kb[:, b, :], rhs=X[:, b, :], start=True, stop=True),
                         reads=[XR, kbR], writes=[s_wR])
            yield
            usb, usbR = B("usb")
            wT, wTR = B("wT")
            vnew, vnewR = B("vnew")
            fw.act.op(lambda: nc.scalar.copy(out=usb[:], in_=v4(s_u)), reads=[s_uR], writes=[usbR])
            fw.dve.op(lambda: nc.vector.tensor_copy(out=wT[:], in_=v4(s_w)), reads=[s_wR], writes=[wTR])
            yield
            for c in range(2):
                r = slice(c * 64, (c + 1) * 64)
                col = tl * 128 + c * 64
                s_ws, s_wsR = bk.next()
                for b in range(4):
                    fw.pe.op(lambda: nc.tensor.matmul(s_ws[r, b * 128:(b + 1) * 128], lhsT=wT[:, b, r], rhs=Sb[:, b, :], start=True, stop=True),
                             reads=[wTR, SbR], writes=[s_wsR])
                yield
                fw.dve.op(lambda: nc.vector.tensor_tensor(out=vnew[r, :, :], in0=usb[r, :, :], in1=s_ws[r, :].rearrange("p (s c) -> p s c", s=4),
                                                          op=ALU.subtract), reads=[usbR, s_wsR], writes=[vnewR])
                s_o, s_oR = bk.next()
                for b in range(4):
                    fw.pe.op(lambda: nc.tensor.matmul(s_o[:, b * 128:b * 128 + 64], lhsT=Sb[:, b, :], rhs=qd4[:, b, col:col + 64], start=True, stop=False),
                             reads=[SbR, qdR], writes=[s_oR])
                    fw.pe.op(lambda: nc.tensor.matmul(s_o[:, b * 128:b * 128 + 64], lhsT=vnew[r, b, :], rhs=QKT[r, b, r], start=False, stop=True),
                             reads=[vnewR, QKTR], writes=[s_oR])
                s_s, s_sR = bk.next()
                for b in range(4):
                    fw.pe.op(lambda: nc.tensor.matmul(s_s[:, b * 128:(b + 1) * 128], lhsT=kdec[r, b, :], rhs=vnew[r, b, :], start=True, stop=True),
                             reads=[kdecR, vnewR], writes=[s_sR])
                yield
                fw.act.op(lambda: nc.scalar.copy(out=oT4[:, :, col:col + 64], in_=v4(s_o)[:, :, 0:64]), reads=[s_oR], writes=[oTR])
                fw.dve.op(lambda: nc.vector.tensor_tensor(out=S32[:], in0=S32[:], in1=bc(self.EGL[:, ti, c * 8 + h0:c * 8 + h0 + 4]), op=ALU.mult),
                          reads=[S32R, self.EGLR], writes=[S32R])
                fw.dve.op(lambda: nc.vector.tensor_tensor(out=S32[:], in0=S32[:], in1=v4(s_s), op=ALU.add), reads=[S32R, s_sR], writes=[S32R])
                fw.act.op(lambda: nc.scalar.copy(out=Sb[:], in_=S32[:]), reads=[S32R], writes=[SbR])
                yield
        if is_meta:
            fw.pool.op(lambda: nc.gpsimd.tensor_copy(out=Sm[:], in_=S32[:]), reads=[S32R], writes=[SmR])
        for b in range(4):
            h = h0 + b
            fw.act.op(lambda: nc.scalar.activation(out=sq[:, :N], in_=oT4[:, b, :N], func=AF.Square), reads=[oTR], writes=[sqR])
            pg, pgR = bk.next()
            fw.pe.op(lambda: nc.tensor.matmul(pg[:, :N], lhsT=self.onesb[:], rhs=sq[:, :N], start=True, stop=True),
                     reads=[self.onesbR, sqR], writes=[pgR])
            zt, ztR = B("zt")
            fw.sp.dma(zt[:, :N], self.PF["z"][h * 128:(h + 1) * 128, n0:n0 + N], ztR, "w")
            yield
            fw.act.op(lambda: nc.scalar.activation(out=ssb[:, :N], in_=pg[:, :N], func=AF.Sqrt, scale=1.0 / 128, bias=self.eps1[:, 0:1]),
                      reads=[pgR, self.epsR], writes=[ssbR])
            fw.dve.op(lambda: nc.vector.reciprocal(out=rn[:, :N], in_=ssb[:, :N]), reads=[ssbR], writes=[rnR])
            fw.act.op(lambda: nc.scalar.activation(out=th[:, :N], in_=zt[:, :N], func=AF.Tanh, scale=0.5), reads=[ztR], writes=[thR])
            fw.pool.op(lambda: nc.gpsimd.scalar_tensor_tensor(out=th[:, :N], in0=th[:, :N], scalar=1.0, in1=zt[:, :N], op0=ALU.add, op1=ALU.mult),
                       reads=[thR, ztR], writes=[thR]) if False else \
                fw.dve.op(lambda: nc.vector.scalar_tensor_tensor(out=th[:, :N], in0=th[:, :N], scalar=1.0, in1=zt[:, :N], op0=ALU.add, op1=ALU.mult),
                          reads=[thR, ztR], writes=[thR])
            fw.dve.op(lambda: nc.vector.scalar_tensor_tensor(out=rn[:, :N], in0=oT4[:, b, :N], scalar=self.nhh[:, 0:1], in1=rn[:, :N],
                                                             op0=ALU.mult, op1=ALU.mult), reads=[oTR, self.nhhR, rnR], writes=[rnR])
            od, odR = B("od")
            fw.pool.op(lambda: nc.gpsimd.tensor_tensor(out=od[:, :N], in0=rn[:, :N], in1=th[:, :N], op=ALU.mult),
                       reads=[rnR, thR], writes=[odR])
            fw.sp.dma(self.OD[h * 128:(h + 1) * 128, n0:n0 + N], od[:, :N], odR, "r")
            yield


def phase3b(self):
    fw, nc = self.fw, self.nc
    fw.push_scope()
    self.cw, self.cwR = fw.sbuf("cw", [128, 24, 4], F32)
    fw.sp.dma(self.cw[:], self.convT, self.cwR, "w")
    self.nhh, self.nhhR = fw.sbuf("nhh", [128, 1], F32)
    fw.sp.dma(self.nhh[:], self.nh_dn, self.nhhR, "w")
    fw.dve.op(lambda: nc.vector.tensor_scalar(out=self.nhh[:], in0=self.nhh[:], scalar1=0.5, scalar2=None, op0=ALU.mult),
              reads=[self.nhhR], writes=[self.nhhR])
    self.mlow4, self.m4R = fw.sbuf("mlow4", [128, 4, 128], F32)
    self.id4f, _ = fw.sbuf("id4f", [128, 4, 128], F32)
    self.id4b, _ = fw.sbuf("id4b", [128, 4, 128], BF16)
    for b in range(4):
        fw.pool.op(lambda: nc.gpsimd.tensor_copy(out=self.mlow4[:, b, :], in_=self.mlow), reads=[self.cstR], writes=[self.m4R])
        fw.pool.op(lambda: nc.gpsimd.tensor_copy(out=self.id4f[:, b, :], in_=self.idf[:]), reads=[self.idfR], writes=[self.m4R])
        fw.pool.op(lambda: nc.gpsimd.tensor_copy(out=self.id4b[:, b, :], in_=self.idf[:]), reads=[self.idfR], writes=[self.m4R])
    gbs = []
    NG = 256
    for p in range(2):
        gb = {}
        gb["banks"] = [fw.psum(f"bk{p}_{i}", [128, 512], F32) for i in range(4)]
        t2 = lambda nm, dt=BF16, w=NG: fw.sbuf(f"{nm}{p}", [128, w], dt)
        t4 = lambda nm, dt=BF16, w=128: fw.sbuf(f"{nm}{p}", [128, 4, w], dt)
        gb["S32"], gb["Sb"], gb["Sm"] = t4("S32_", F32), t4("Sb_"), t4("Sm_", F32)
        gb["dg"] = [[[t2(f"dg{b}{s}{j}_", BF16, 128) for j in range(4)] for s in range(3)] for b in range(4)]
        gb["raw"] = [t2(f"raw{s}_", BF16, NG + 4) for s in range(3)]
        gb["c2"] = [t2("c2q_", F32), t2("c2k_", F32)]
        for nm in ("qn4", "kn4", "vc4", "qd4"):
            gb[nm] = t4(nm, BF16, NG)
        for nm in ("oT4", "gr4", "eg4"):
            gb[nm] = t4(nm, F32, NG)
        for nm, dt in [("th", F32), ("sq", BF16), ("ssb", F32), ("rn", F32), ("zt", BF16), ("od", BF16)]:
            gb[nm] = t2(nm + "_", dt)
        for nm, dt in [("dm", F32), ("Dm", F32), ("DsB", F32), ("usb", F32), ("QK", BF16), ("QKT", BF16), ("ktok", BF16),
                       ("kb", BF16), ("kdec", BF16), ("vb", BF16), ("wT", BF16), ("vnew", BF16)]:
            gb[nm] = t4(nm + "_", dt)
        gb["Pm"] = [t4("Pm0_"), t4("Pm1_")]
        gb["Qm"] = [t4("Qm0_"), t4("Qm1_")]
        gb["Xm"] = [t4("Xm0_"), t4("Xm1_")]
        gbs.append(gb)
    run_interleaved([dn_group(self, p, gbs[p]) for p in range(2)])
    fw.pop_scope()


MK.phase3a_extra = phase3a_extra
MK.phase3b = phase3b


def p3_streams(self, g, sh, bk):
    fw, nc = self.fw, self.nc
    SEQ = self.SEQ
    h0 = g * 4
    hs = slice(h0, h0 + 4)
    NTL = self.NTILES
    TPS = SEQ // 128
    PFq = self.PF["qkv"]
    bc = lambda ap, w=128: ap.unsqueeze(2).to_broadcast([128, 4, w])
    prog = sh["prog"]

    def gen_P():
        for t in range(NTL):
            while prog["F"] < t - 1 or prog["B"] < t - 1:
                yield
            par = t % 2
            n0 = t * 128
            qn4, qnR = sh["qn4"][par]
            kn4, knR = sh["kn4"][par]
            vc4, vcR = sh["vc4"][par]
            qd4, qdR = sh["qd4"][par]
            gr4, grR = sh["gr4"][par]
            fw.sp.dma(gr4[:], self.GCT[hs, n0:n0 + 128].partition_broadcast(128), grR, "w")
            for seg in range(3):
                raw, rawR = sh["raw"][seg]
                fw.sp.dma(raw[:, :, 0:128], PFq[seg * 1024 + h0 * 128:seg * 1024 + (h0 + 4) * 128, n0:n0 + 128].rearrange("(b p) n -> p b n", p=128),
                          rawR, "w")
                th, thR = sh["th"][seg % 2]
                fw.act.op(lambda: nc.scalar.activation(out=th[:], in_=raw[:, :, 0:128], func=AF.Tanh, scale=0.5), reads=[rawR], writes=[thR])
                if seg < 2:
                    c2, c2R = sh["c2"][seg]
                    fw.dve.op(lambda: nc.vector.scalar_tensor_tensor(out=c2[:], in0=th[:], scalar=1.0, in1=raw[:, :, 0:128], op0=ALU.add, op1=ALU.mult),
                              reads=[thR, rawR], writes=[c2R])
                else:
                    fw.dve.op(lambda: nc.vector.scalar_tensor_tensor(out=vc4[:], in0=th[:], scalar=1.0, in1=raw[:, :, 0:128], op0=ALU.add, op1=ALU.mult),
                              reads=[thR, rawR], writes=[vcR])
                yield
            gst = []
            yield from acq(bk, 1, gst)
            pst, pstR = gst[0]
            for seg in range(2):
                c2, c2R = sh["c2"][seg]
                sq, sqR = sh["sq"][seg]
                fw.act.op(lambda: nc.scalar.activation(out=sq[:], in_=c2[:], func=AF.Square), reads=[c2R], writes=[sqR])
                for b in range(4):
                    fw.pe.op(lambda: nc.tensor.matmul(pst[:, seg * 4 + b:seg * 4 + b + 1], lhsT=sq[:, b, :], rhs=self.onesb[:, 0:1],
                                                      start=True, stop=True), reads=[self.onesbR, sqR], writes=[pstR])
            yield
            st8, st8R = sh["st8"]
            fw.dve.op(lambda: nc.vector.tensor_scalar(out=st8[:], in0=pst[:, 0:8], scalar1=4 * EPS, scalar2=None, op0=ALU.add),
                      reads=[pstR], writes=[st8R])
            bk.release(gst)
            fw.pool.op(lambda: nc.gpsimd.tensor_tensor(out=st8[:], in0=st8[:], in1=self.mhalf[:, 0:8], op=ALU.pow),
                       reads=[st8R, self.mhalfR], writes=[st8R])
            yield
            gnq = []
            yield from acq(bk, 2, gnq)
            for seg in range(2):
                pn, pnR = gnq[seg]
                dgn, dgnR = sh["dgn"][seg]
                fw.pool.op(lambda: nc.gpsimd.tensor_tensor(out=dgn[:], in0=self.id4f[:], in1=bc(st8[:, seg * 4:(seg + 1) * 4]), op=ALU.mult),
                           reads=[self.m4R, st8R], writes=[dgnR])
                for b in range(4):
                    fw.pe.op(lambda: nc.tensor.matmul(pn[:, b * 128:(b + 1) * 128], lhsT=self.onesb[:], rhs=dgn[:, b, :], start=True, stop=True),
                             reads=[self.onesbR, dgnR], writes=[pnR])
            yield
            for seg in range(2):
                pn, pnR = gnq[seg]
                c2, c2R = sh["c2"][seg]
                dst, dstR = (qn4, qnR) if seg == 0 else (kn4, knR)
                sc = 128 ** -0.5 if seg == 0 else 1.0
                fw.dve.op(lambda: nc.vector.scalar_tensor_tensor(out=dst[:], in0=c2[:], scalar=sc, in1=v4(pn), op0=ALU.mult, op1=ALU.mult),
                          reads=[c2R, pnR], writes=[dstR])
            bk.release(gnq)
            yield
            eg4, egR = sh["eg4"]
            fw.act.op(lambda: nc.scalar.activation(out=eg4[:], in_=gr4[:], func=AF.Exp), reads=[grR], writes=[egR])
            fw.pool.op(lambda: nc.gpsimd.tensor_tensor(out=qd4[:], in0=qn4[:], in1=eg4[:], op=ALU.mult), reads=[qnR, egR], writes=[qdR])
            prog["P"] = t + 1
            yield

    def gen_F():
        for t in range(NTL):
            while prog["P"] < t + 1 or prog["B"] < t - 1:
                yield
            par = t % 2
            ti = t
            qn4, qnR = sh["qn4"][par]
            kn4, knR = sh["kn4"][par]
            vc4, vcR = sh["vc4"][par]
            gr4, grR = sh["gr4"][par]
            QKT, QKTR = sh["QKT"][par]
            kdec, kdecR = sh["kdec"][par]
            usb, usbR = sh["usb"][par]
            wT, wTR = sh["wT"][par]
            dm, dmR = sh["dm"]
            Dm, DmR = sh["Dm"]
            DsB, DsBR = sh["DsB"]
            fw.pool.op(lambda: nc.gpsimd.tensor_tensor(out=DsB[:], in0=self.mstr4[:], in1=bc(self.BETA[:, ti, hs]), op=ALU.mult),
                       reads=[self.m4R, self.BETAR], writes=[DsBR])
            fw.dve.op(lambda: nc.vector.tensor_tensor(out=dm[:], in0=gr4[:], in1=bc(self.GC[:, ti, hs]), op=ALU.subtract),
                      reads=[grR, self.GCR], writes=[dmR])
            fw.pool.op(lambda: nc.gpsimd.tensor_scalar(out=dm[:], in0=dm[:], scalar1=3.0e38, scalar2=0.0, op0=ALU.min, op1=ALU.max),
                       reads=[dmR], writes=[dmR])
            fw.act.op(lambda: nc.scalar.activation(out=dm[:], in_=dm[:], func=AF.Exp, scale=-1.0), reads=[dmR], writes=[dmR])
            fw.pool.op(lambda: nc.gpsimd.tensor_tensor(out=Dm[:], in0=dm[:], in1=self.mlow4[:], op=ALU.mult),
                       reads=[dmR, self.m4R], writes=[DmR])
            fw.dve.op(lambda: nc.vector.tensor_tensor(out=DsB[:], in0=DsB[:], in1=dm[:], op=ALU.mult), reads=[DsBR, dmR], writes=[DsBR])
            gkq = []
            yield from acq(bk, 2, gkq)
            (p_kk, p_kkR), (p_qk, p_qkR) = gkq
            for b in range(4):
                fw.pe.op(lambda: nc.tensor.matmul(p_kk[:, b * 128:(b + 1) * 128], lhsT=kn4[:, b, :], rhs=kn4[:, b, :], start=True, stop=True),
                         reads=[knR], writes=[p_kkR])
            for b in range(4):
                fw.pe.op(lambda: nc.tensor.matmul(p_qk[:, b * 128:(b + 1) * 128], lhsT=qn4[:, b, :], rhs=kn4[:, b, :], start=True, stop=True),
                         reads=[qnR, knR], writes=[p_qkR])
            gkv = []
            yield from acq(bk, 2, gkv)
            (t_k, t_kR), (t_v, t_vR) = gkv
            for b in range(4):
                fw.pe.op(lambda: nc.tensor.transpose(out=v4b(t_k)[:, b, :], in_=kn4[:, b, :], identity=self.idb[:]),
                         reads=[knR, self.idbR], writes=[t_kR])
            for b in range(4):
                fw.pe.op(lambda: nc.tensor.transpose(out=v4b(t_v)[:, b, :], in_=vc4[:, b, :], identity=self.idb[:]),
                         reads=[vcR, self.idbR], writes=[t_vR])
            yield
            ktok, ktokR = sh["ktok"]
            kb, kbR = sh["kb"]
            vb, vbR = sh["vb"]
            fw.act.op(lambda: nc.scalar.copy(out=ktok[:], in_=v4b(t_k)), reads=[t_kR], writes=[ktokR])
            fw.dve.op(lambda: nc.vector.tensor_tensor(out=vb[:], in0=v4b(t_v), in1=bc(self.HB[:, ti, hs]), op=ALU.mult),
                      reads=[t_vR, self.HBR], writes=[vbR])
            bk.release(gkv)
            fw.pool.op(lambda: nc.gpsimd.tensor_tensor(out=kb[:], in0=ktok[:], in1=bc(self.KBS[:, ti, hs]), op=ALU.mult),
                       reads=[ktokR, self.KBSR], writes=[kbR])
            fw.pool.op(lambda: nc.gpsimd.tensor_tensor(out=kdec[:], in0=ktok[:], in1=bc(self.EGR[:, ti, hs]), op=ALU.mult),
                       reads=[ktokR, self.EGRR], writes=[kdecR])
            yield
            P, PR = sh["Pm"][0]
            QK, QKR = sh["QK"]
            fw.dve.op(lambda: nc.vector.tensor_tensor(out=P[:], in0=v4(p_kk), in1=DsB[:], op=ALU.mult), reads=[p_kkR, DsBR], writes=[PR])
            fw.dve.op(lambda: nc.vector.tensor_tensor(out=QK[:], in0=v4(p_qk), in1=Dm[:], op=ALU.mult), reads=[p_qkR, DmR], writes=[QKR])
            bk.release(gkq)
            gtp = []
            yield from acq(bk, 2, gtp)
            (t_p, t_pR), (t_qk, t_qkR) = gtp
            for b in range(4):
                fw.pe.op(lambda: nc.tensor.transpose(out=v4b(t_p)[:, b, :], in_=P[:, b, :], identity=self.idb[:]),
                         reads=[PR, self.idbR], writes=[t_pR])
            for b in range(4):
                fw.pe.op(lambda: nc.tensor.transpose(out=v4b(t_qk)[:, b, :], in_=QK[:, b, :], identity=self.idb[:]),
                         reads=[QKR, self.idbR], writes=[t_qkR])
            yield
            Q, QR = sh["Qm"][0]
            X, XR = sh["Xm"][0]
            fw.act.op(lambda: nc.scalar.copy(out=Q[:], in_=v4b(t_p)), reads=[t_pR], writes=[QR])
            fw.dve.op(lambda: nc.vector.tensor_tensor(out=X[:], in0=self.id4b[:], in1=v4b(t_p), op=ALU.subtract),
                      reads=[self.m4R, t_pR], writes=[XR])
            fw.act.op(lambda: nc.scalar.copy(out=QKT[:], in_=v4b(t_qk)), reads=[t_qkR], writes=[QKTR])
            bk.release(gtp)
            yield
            Pprev = None
            for r in range(1, 7):
                Pn, PnR = sh["Pm"][r % 2]
                Qn, QnR = sh["Qm"][r % 2]
                Xn, XnR = sh["Xm"][r % 2]
                need_sq = r <= 5
                need_q = r <= 4
                need_x = r >= 2
                nb_ = (1 if need_sq else 0) + (1 if need_q else 0) + (1 if need_x else 0)
                gl = []
                yield from acq(bk, nb_, gl)
                gi_ = iter(gl)
                if need_sq:
                    s_p, s_pR = next(gi_)
                    for b in range(4):
                        fw.pe.op(lambda: nc.tensor.matmul(s_p[:, b * 128:(b + 1) * 128], lhsT=Q[:, b, :], rhs=P[:, b, :], start=True, stop=True),
                                 reads=[PR, QR], writes=[s_pR])
                if need_x:
                    s_x, s_xR = next(gi_)
                    for b in range(4):
                        fw.pe.op(lambda: nc.tensor.matmul(s_x[:, b * 128:(b + 1) * 128], lhsT=P[:, b, :], rhs=X[:, b, :], start=True, stop=True),
                                 reads=[PR, XR], writes=[s_xR])
                if need_q:
                    s_q, s_qR = next(gi_)
                    for b in range(4):
                        fw.pe.op(lambda: nc.tensor.matmul(s_q[:, b * 128:(b + 1) * 128], lhsT=P[:, b, :], rhs=Q[:, b, :], start=True, stop=True),
                                 reads=[PR, QR], writes=[s_qR])
                yield
                if need_sq:
                    fw.act.op(lambda: nc.scalar.copy(out=Pn[:], in_=v4(s_p)), reads=[s_pR], writes=[PnR])
                if need_x:
                    fw.dve.op(lambda: nc.vector.tensor_tensor(out=Xn[:], in0=X[:], in1=v4(s_x), op=ALU.add), reads=[XR, s_xR], writes=[XnR])
                    X, XR = Xn, XnR
                if need_q:
                    fw.act.op(lambda: nc.scalar.copy(out=Qn[:], in_=v4(s_q)), reads=[s_qR], writes=[QnR])
                bk.release(gl)
                if need_sq:
                    P, PR = Pn, PnR
                if need_q:
                    Q, QR = Qn, QnR
                yield
            guw = []
            yield from acq(bk, 2, guw)
            (s_u, s_uR), (s_w, s_wR) = guw
            for b in range(4):
                fw.pe.op(lambda: nc.tensor.matmul(s_u[:, b * 128:(b + 1) * 128], lhsT=X[:, b, :], rhs=vb[:, b, :], start=True, stop=True),
                         reads=[XR, vbR], writes=[s_uR])
            for b in range(4):
                fw.pe.op(lambda: nc.tensor.matmul(s_w[:, b * 128:(b + 1) * 128], lhsT=kb[:, b, :], rhs=X[:, b, :], start=True, stop=True),
                         reads=[XR, kbR], writes=[s_wR])
            yield
            fw.act.op(lambda: nc.scalar.copy(out=usb[:], in_=v4(s_u)), reads=[s_uR], writes=[usbR])
            fw.act.op(lambda: nc.scalar.copy(out=wT[:], in_=v4(s_w)), reads=[s_wR], writes=[wTR])
            bk.release(guw)
            prog["F"] = t + 1
            yield

    def gen_B():
        S32, S32R = sh["S32"]
        Sb, SbR = sh["Sb"]
        Sm, SmR = sh["Sm"]
        vnew, vnewR = sh["vnew"]
        for t in range(NTL):
            while prog["F"] < t + 1 or prog["O"] < t - 1:
                yield
            par = t % 2
            ti = t
            is_meta = t == 0
            seq_start = (not is_meta) and (t - 1) % TPS == 0
            qd4, qdR = sh["qd4"][par]
            QKT, QKTR = sh["QKT"][par]
            kdec, kdecR = sh["kdec"][par]
            usb, usbR = sh["usb"][par]
            wT, wTR = sh["wT"][par]
            oT4, oTR = sh["oT4"][par]
            if is_meta:
                fw.pool.op(lambda: nc.gpsimd.memset(S32[:], 0.0), writes=[S32R])
                fw.pool.op(lambda: nc.gpsimd.memset(Sb[:], 0.0), writes=[SbR])
            elif seq_start:
                fw.pool.op(lambda: nc.gpsimd.tensor_copy(out=S32[:], in_=Sm[:]), reads=[SmR], writes=[S32R])
                fw.act.op(lambda: nc.scalar.copy(out=Sb[:], in_=Sm[:]), reads=[SmR], writes=[SbR])
            for c in range(2):
                r = slice(c * 64, (c + 1) * 64)
                gw = []
                yield from acq(bk, 1, gw)
                s_ws, s_wsR = gw[0]
                for b in range(4):
                    fw.pe.op(lambda: nc.tensor.matmul(s_ws[r, b * 128:(b + 1) * 128], lhsT=wT[:, b, r], rhs=Sb[:, b, :], start=True, stop=True),
                             reads=[wTR, SbR], writes=[s_wsR])
                yield
                fw.dve.op(lambda: nc.vector.tensor_tensor(out=vnew[r, :, :], in0=usb[r, :, :], in1=s_ws[r, :].rearrange("p (s c) -> p s c", s=4),
                                                          op=ALU.subtract), reads=[usbR, s_wsR], writes=[vnewR])
                bk.release(gw)
                gso = []
                yield from acq(bk, 2, gso)
                (s_s, s_sR), (s_o, s_oR) = gso
                for b in range(4):
                    fw.pe.op(lambda: nc.tensor.matmul(s_s[:, b * 128:(b + 1) * 128], lhsT=kdec[r, b, :], rhs=vnew[r, b, :], start=True, stop=True),
                             reads=[kdecR, vnewR], writes=[s_sR])
                for b in range(4):
                    fw.pe.op(lambda: nc.tensor.matmul(s_o[:, b * 128:b * 128 + 64], lhsT=Sb[:, b, :], rhs=qd4[:, b, r], start=True, stop=False),
                             reads=[SbR, qdR], writes=[s_oR])
                    fw.pe.op(lambda: nc.tensor.matmul(s_o[:, b * 128:b * 128 + 64], lhsT=vnew[r, b, :], rhs=QKT[r, b, r], start=False, stop=True),
                             reads=[vnewR, QKTR], writes=[s_oR])
                yield
                fw.dve.op(lambda: nc.vector.tensor_tensor(out=S32[:], in0=S32[:], in1=bc(self.EGL[:, ti, c * 8 + h0:c * 8 + h0 + 4]), op=ALU.mult),
                          reads=[S32R, self.EGLR], writes=[S32R])
                fw.dve.op(lambda: nc.vector.tensor_tensor(out=S32[:], in0=S32[:], in1=v4(s_s), op=ALU.add), reads=[S32R, s_sR], writes=[S32R])
                fw.act.op(lambda: nc.scalar.copy(out=Sb[:], in_=S32[:]), reads=[S32R], writes=[SbR])
                fw.act.op(lambda: nc.scalar.copy(out=oT4[:, :, r], in_=v4(s_o)[:, :, 0:64]), reads=[s_oR], writes=[oTR])
                bk.release(gso)
                yield
            if is_meta:
                fw.pool.op(lambda: nc.gpsimd.tensor_copy(out=Sm[:], in_=S32[:]), reads=[S32R], writes=[SmR])
            prog["B"] = t + 1
            yield

    def gen_O():
        for t in range(NTL):
            while prog["B"] < t + 1:
                yield
            par = t % 2
            n0 = t * 128
            oT4, oTR = sh["oT4"][par]
            osq, osqR = sh["osq"]
            ors, orsR = sh["ors"]
            zt, ztR = sh["zt"]
            zth, zthR = sh["zth"]
            od, odR = sh["od"][par]
            rows = lambda ap: ap[h0 * 128:(h0 + 4) * 128, n0:n0 + 128].rearrange("(b p) n -> p b n", p=128)
            fw.sp.dma(zt[:], rows(self.PF["z"]), ztR, "w")
            fw.act.op(lambda: nc.scalar.activation(out=osq[:], in_=oT4[:], func=AF.Square), reads=[oTR], writes=[osqR])
            go = []
            yield from acq(bk, 1, go)
            pg, pgR = go[0]
            for b in range(4):
                fw.pe.op(lambda: nc.tensor.matmul(pg[:, b:b + 1], lhsT=osq[:, b, :], rhs=self.onesb[:, 0:1], start=True, stop=True),
                         reads=[self.onesbR, osqR], writes=[pgR])
            fw.act.op(lambda: nc.scalar.activation(out=zth[:], in_=zt[:], func=AF.Tanh, scale=0.5), reads=[ztR], writes=[zthR])
            yield
            so4, so4R = sh["so4"]
            fw.dve.op(lambda: nc.vector.tensor_scalar(out=so4[:], in0=pg[:, 0:4], scalar1=1.0 / 128, scalar2=EPS, op0=ALU.mult, op1=ALU.add),
                      reads=[pgR], writes=[so4R])
            bk.release(go)
            fw.pool.op(lambda: nc.gpsimd.tensor_tensor(out=so4[:], in0=so4[:], in1=self.mhalf[:, 0:4], op=ALU.pow),
                       reads=[so4R, self.mhalfR], writes=[so4R])
            fw.dve.op(lambda: nc.vector.scalar_tensor_tensor(out=zth[:], in0=zth[:], scalar=1.0, in1=zt[:], op0=ALU.add, op1=ALU.mult),
                      reads=[zthR, ztR], writes=[zthR])
            dgo, dgoR = sh["dgo"]
            fw.pool.op(lambda: nc.gpsimd.tensor_tensor(out=dgo[:], in0=self.id4f[:], in1=bc(so4[:, 0:4]), op=ALU.mult),
                       reads=[self.m4R, so4R], writes=[dgoR])
            go2 = []
            yield from acq(bk, 1, go2)
            pr, prR = go2[0]
            for b in range(4):
                fw.pe.op(lambda: nc.tensor.matmul(pr[:, b * 128:(b + 1) * 128], lhsT=self.onesb[:], rhs=dgo[:, b, :], start=True, stop=True),
                         reads=[self.onesbR, dgoR], writes=[prR])
            yield
            fw.dve.op(lambda: nc.vector.scalar_tensor_tensor(out=ors[:], in0=oT4[:], scalar=self.nhh[:, 0:1], in1=v4(pr), op0=ALU.mult, op1=ALU.mult),
                      reads=[oTR, self.nhhR, prR], writes=[orsR])
            bk.release(go2)
            fw.pool.op(lambda: nc.gpsimd.tensor_tensor(out=od[:], in0=ors[:], in1=zth[:], op=ALU.mult), reads=[orsR, zthR], writes=[odR])
            fw.sp.dma(rows(self.OD), od[:], odR, "r")
            prog["O"] = t + 1
            yield

    return [gen_P(), gen_F(), gen_B(), gen_O()]


def phase3c(self):
    fw, nc = self.fw, self.nc
    fw.push_scope()
    self.cw, self.cwR = fw.sbuf("cw", [128, 24, 4], F32)
    fw.sp.dma(self.cw[:], self.convT, self.cwR, "w")
    self.nhh, self.nhhR = fw.sbuf("nhh", [128, 1], F32)
    fw.sp.dma(self.nhh[:], self.nh_dn, self.nhhR, "w")
    fw.dve.op(lambda: nc.vector.tensor_scalar(out=self.nhh[:], in0=self.nhh[:], scalar1=0.5, scalar2=None, op0=ALU.mult),
              reads=[self.nhhR], writes=[self.nhhR])
    self.mlow4, self.m4R = fw.sbuf("mlow4", [128, 4, 128], F32)
    self.id4f, _ = fw.sbuf("id4f", [128, 4, 128], F32)
    self.id4b, _ = fw.sbuf("id4b", [128, 4, 128], BF16)
    self.mstr4, _ = fw.sbuf("mstr4", [128, 4, 128], F32)
    for b in range(4):
        fw.pool.op(lambda: nc.gpsimd.tensor_copy(out=self.mlow4[:, b, :], in_=self.mlow), reads=[self.cstR], writes=[self.m4R])
        fw.pool.op(lambda: nc.gpsimd.tensor_copy(out=self.id4f[:, b, :], in_=self.idf[:]), reads=[self.idfR], writes=[self.m4R])
        fw.pool.op(lambda: nc.gpsimd.tensor_copy(out=self.id4b[:, b, :], in_=self.idf[:]), reads=[self.idfR], writes=[self.m4R])
    fw.pool.op(lambda: nc.gpsimd.tensor_tensor(out=self.mstr4[:], in0=self.mlow4[:], in1=self.id4f[:], op=ALU.subtract),
               reads=[self.m4R], writes=[self.m4R])
    bk = BankPool([fw.psum(f"bkc{i}", [128, 512], F32) for i in range(8)])
    gens = []
    for p in range(2):
        sh = {"prog": {"P": 0, "F": 0, "B": 0, "O": 0}}
        t4 = lambda nm, dt=BF16, w=128: fw.sbuf(f"c3_{nm}{p}", [128, 4, w], dt)
        for nm, dt in [("qn4", BF16), ("kn4", BF16), ("vc4", BF16), ("qd4", BF16), ("gr4", F32),
                       ("QKT", BF16), ("kdec", BF16), ("usb", F32), ("wT", BF16), ("oT4", F32), ("od", BF16)]:
            sh[nm] = [t4(nm + "a", dt), t4(nm + "b", dt)]
        sh["raw"] = [t4(f"raw{s}_", BF16, 132) for s in range(3)]
        sh["th"] = [t4("tha", F32), t4("thb", F32)]
        sh["c2"] = [t4("c2q", F32), t4("c2k", F32)]
        sh["sq"] = [t4("sqq"), t4("sqk")]
        for nm, dt in [("eg4", F32), ("dm", F32), ("Dm", F32), ("DsB", F32), ("ktok", BF16), ("kb", BF16), ("vb", BF16), ("QK", BF16),
                       ("vnew", BF16), ("S32", F32), ("Sb", BF16), ("Sm", F32), ("osq", BF16), ("ors", F32), ("zt", BF16), ("zth", F32)]:
            sh[nm] = t4(nm + "_", dt)
        sh["Pm"] = [t4("Pm0_"), t4("Pm1_")]
        sh["Qm"] = [t4("Qm0_"), t4("Qm1_")]
        sh["Xm"] = [t4("Xm0_"), t4("Xm1_")]
        sh["st8"] = fw.sbuf(f"c3_st8{p}", [128, 8], F32)
        sh["so4"] = fw.sbuf(f"c3_so4{p}", [128, 4], F32)
        sh["dgn"] = [t4("dgnq", BF16), t4("dgnk", BF16)]
        sh["dgo"] = t4("dgo", BF16)
        gens += p3_streams(self, p, sh, bk)
    run_interleaved(gens)
    fw.pop_scope()


MK.phase3c = phase3c


def phase4b(self):
    fw, nc = self.fw, self.nc
    fw.push_scope()
    self.wal, self.walR = fw.sbuf("wal", [16, 512], F32)
    fw.sp.dma(self.wal[:], self.w_alpha, self.walR, "w")
    self.nba, self.nbaR = fw.sbuf("nba", [128, 4], F32)
    fw.sp.dma(self.nba[:], self.nb_alpha, self.nbaR, "w")
    fw.dve.op(lambda: nc.vector.tensor_scalar(out=self.nba[:], in0=self.nba[:], scalar1=-1.0, scalar2=None, op0=ALU.mult),
              reads=[self.nbaR], writes=[self.nbaR])
    self.nhg, self.nhgR = fw.sbuf("nhg", [128, 2], F32)
    fw.sp.dma(self.nhg[:], self.nh_gla, self.nhgR, "w")
    fw.dve.op(lambda: nc.vector.tensor_scalar(out=self.nhg[:], in0=self.nhg[:], scalar1=0.5, scalar2=None, op0=ALU.mult),
              reads=[self.nhgR], writes=[self.nhgR])
    self.rmask, self.rmaskR = fw.sbuf("rmask", [128, 256], F32)
    fw.pool.op(lambda: nc.gpsimd.memset(self.rmask[:], 1.0), writes=[self.rmaskR])
    fw.pool.op(lambda: nc.gpsimd.memset(self.rmask[:].rearrange("p (c k) -> p c k", k=64)[:, :, 0:1], 0.0), writes=[self.rmaskR])
    bk = BankPool([fw.psum(f"gbk{i}", [128, 512], F32) for i in range(8)])
    halves = [tuple(range(0, (self.NSEQ + 1) // 2)), tuple(range((self.NSEQ + 1) // 2, self.NSEQ))]
    halves = [hv for hv in halves if hv]
    gens = []
    W = 256
    for h in range(4):
        for si, seqs in enumerate(halves):
            p = f"{h}{si}"
            hb = {}
            sq = lambda nm, dt=BF16, w=W: fw.sbuf(f"g4{nm}{p}", [128, w], dt)
            hb["S32"], hb["Sb"], hb["Sm"] = sq("S32", F32, 256), sq("Sb", BF16, 256), sq("Sm", F32, 256)
            hb["qt"], hb["kt"] = sq("qt"), sq("kt")
            hb["lr"] = fw.sbuf(f"g4lr{p}", [16, W], F32)
            hb["vt"] = fw.sbuf(f"g4vt{p}", [128, W // 128, 256], BF16)
            for nm in ("e0", "cc", "d1", "ea", "e3", "ssb", "rn", "th"):
                hb[nm] = sq(nm, F32)
            for nm in ("qg", "kg", "qd", "kd", "rt", "og"):
                hb[nm] = sq(nm)
            hb["am"], hb["ktk"] = sq("am", BF16, 128), sq("ktk", BF16, 128)
            hb["oT"] = [sq("oT0", F32), sq("oT1", F32)]
            hb["sq"] = [sq("sq0"), sq("sq1")]
            gens.append(gla_stream(self, h, hb, seqs, bk))
    run_interleaved(gens)
    fw.pop_scope()


MK.phase4b = phase4b
```

```python
import numpy as np
import concourse.bass as bass
import concourse.mybir as mybir
from concourse.bass_utils import run_bass_kernel_spmd

F32 = mybir.dt.float32
BF16 = mybir.dt.bfloat16
I32 = mybir.dt.int32
ALU = mybir.AluOpType
AF = mybir.ActivationFunctionType
AX = mybir.AxisListType

D = 1024
D_IN = 9248
EPS = 1e-6
N_EXP = 64


INSTR_NAMES = {"matmul", "transpose", "activation", "copy", "mul", "sqrt", "square", "add", "tensor_tensor", "tensor_scalar",
               "scalar_tensor_tensor", "tensor_copy", "tensor_reduce", "reduce_sum", "reduce_max", "reciprocal", "max", "memset",
               "tensor_tensor_scan", "select", "iota", "tensor_add", "tensor_sub", "tensor_mul"}


class Deferred:
    __slots__ = ("eng", "name", "args", "kw")

    def __init__(self, eng, name, args, kw):
        self.eng, self.name, self.args, self.kw = eng, name, args, kw

    def emit(self):
        return getattr(self.eng, self.name)(*self.args, **self.kw)

    def free_size(self):
        ap = self.kw.get("out", self.args[0] if self.args else None)
        if self.name in ("matmul",):
            ap = self.kw.get("rhs", ap)
        elif self.name == "transpose":
            ap = self.kw.get("in_", ap)
        try:
            n = int(ap.free_size())
        except Exception:
            n = 128
        fp32 = False
        try:
            fp32 = (self.name in ("matmul", "transpose")) and self.kw.get("lhsT", self.kw.get("in_")).dtype == F32
        except Exception:
            pass
        return n, fp32


class EngProxy:
    def __init__(self, eng):
        object.__setattr__(self, "_eng", eng)

    def __getattr__(self, name):
        real = getattr(self._eng, name)
        if name in INSTR_NAMES:
            eng = self._eng
            return lambda *a, **k: Deferred(eng, name, a, k)
        return real


class NCProxy:
    def __init__(self, nc):
        object.__setattr__(self, "_nc", nc)
        for nm in ("tensor", "scalar", "vector", "gpsimd", "sync"):
            object.__setattr__(self, nm, EngProxy(getattr(nc, nm)))

    def __getattr__(self, name):
        return getattr(self._nc, name)


class Res:
    __slots__ = ("name", "w", "r", "dsem", "dn", "dbase", "fw", "excl")

    def __init__(self, fw, name):
        self.fw = fw
        self.name = name
        self.w = None
        self.r = {}
        self.dsem = None
        self.dn = 0
        self.excl = False
        self.dbase = 0


class Eng:
    def __init__(self, fw, idx, name, eng, sem):
        self.fw, self.idx, self.name, self.eng, self.sem = fw, idx, name, eng, sem
        self.n = 0
        self.seen = {}
        self.seen_d = {}

    def _wait_eng(self, idx, cnt):
        if idx == self.idx and self.name == "pe":
            return
        if self.seen.get(idx, 0) < cnt:
            self.eng.wait_ge(self.fw.engs[idx].sem, cnt)
            self.seen[idx] = cnt

    def _wait_dma(self, R):
        if R.dn > R.dbase and self.seen_d.get(R, 0) < R.dn:
            self.eng.wait_ge(R.dsem, 16 * R.dn)
            self.seen_d[R] = R.dn

    def _deps(self, reads, writes, dma_part=False):
        for R in reads:
            if R.w is not None:
                self._wait_eng(*R.w)
            if not dma_part:
                self._wait_dma(R)
        for R in writes:
            if R.w is not None:
                self._wait_eng(*R.w)
            for i, c in R.r.items():
                self._wait_eng(i, c)
            if not dma_part:
                self._wait_dma(R)

    def op(self, ins_fn, reads=(), writes=()):
        if self.fw.recording:
            self.fw.recs.append(("op", self, ins_fn(), tuple(reads), tuple(writes)))
            return None
        if isinstance(ins_fn, Deferred):
            d_ = ins_fn
            ins_fn = d_.emit
        if any(R.excl for R in reads):
            writes = list(writes) + [R for R in reads if R.excl and R not in writes]
            reads = [R for R in reads if not R.excl]
        self._deps(reads, writes)
        ins = ins_fn()
        self.n += 1
        ins.then_inc(self.sem, 1)
        for R in reads:
            R.r[self.idx] = self.n
        for R in writes:
            R.w = (self.idx, self.n)
            R.r = {}
        return ins

    def dma(self, out, in_, R, mode, part=False, extra_reads=(), extra_writes=(), **kw):
        if self.fw.recording:
            self.fw.recs.append(("dma", self, (out, in_, kw), R, mode, part, tuple(extra_reads), tuple(extra_writes)))
            return None
        if mode == "w":
            self._deps(extra_reads, (R,), dma_part=part)
        else:
            self._deps((R,) + tuple(extra_reads), (), dma_part=part)
        self.fw.give_dsem(R)
        ins = self.eng.dma_start(out=out, in_=in_, **kw)
        ins.then_inc(R.dsem, 16)
        R.dn += 1
        return ins

    def wait_dma(self, R, tokens=()):
        if self.fw.recording:
            self.fw.recs.append(("waitdma", self, R, tuple(tokens)))
            return
        self._wait_dma(R)

    def indirect_dma(self, R, mode, extra_reads=(), **kw):
        if self.fw.recording:
            self.fw.recs.append(("idma", self, kw, R, mode, tuple(extra_reads)))
            return None
        if mode == "w":
            self._deps(extra_reads, (R,))
        else:
            self._deps((R,) + tuple(extra_reads), ())
        self.fw.give_dsem(R)
        ins = self.eng.indirect_dma_start(**kw)
        ins.then_inc(R.dsem, 16)
        R.dn += 1
        return ins


class FW:
    def __init__(self, nc):
        self.nc = nc
        self._stack = []
        self.sem_i = 0
        self.engs = []
        for i, (nm, e) in enumerate([("pe", nc.tensor), ("act", nc.scalar), ("dve", nc.vector),
                                     ("pool", nc.gpsimd), ("sp", nc.sync)]):
            self.engs.append(Eng(self, i, nm, e, self.new_sem("e_" + nm)))
        self.pe, self.act, self.dve, self.pool, self.sp = self.engs
        self.all_res = []
        self.free_dsems = []
        self.scopes = []
        self.recording = True
        self.recs = []

    def give_dsem(self, R):
        if R.dsem is None:
            if self.free_dsems:
                R.dsem, R.dn = self.free_dsems.pop()
                R.dbase = R.dn
            else:
                R.dsem = self.new_sem("d")

    def new_sem(self, name):
        self.sem_i += 1
        return self.nc.alloc_semaphore(name=f"{name}_{self.sem_i}")

    def res(self, name):
        R = Res(self, name)
        self.all_res.append(R)
        return R

    def sbuf(self, name, shape, dtype):
        cm = self.nc.sbuf_tensor(name, list(shape), dtype)
        t = cm.__enter__()
        self._stack.append(cm)
        return t, self.res(name)

    def psum(self, name, shape, dtype):
        cm = self.nc.psum_tensor(name, list(shape), dtype)
        t = cm.__enter__()
        self._stack.append(cm)
        R = self.res(name)
        R.excl = True
        return t, R

    def drain_all(self, eng):
        for R in self.all_res:
            eng._wait_dma(R)
        for e in self.engs:
            if e.n and e is not eng:
                eng._wait_eng(e.idx, e.n)


    def _cost(self, rec):
        kind, eng = rec[0], rec[1]
        if kind == "op":
            d = rec[2]
            n, fp32 = d.free_size() if isinstance(d, Deferred) else (128, False)
            if eng.name == "pe":
                return (0.06 + n * 0.00037) * (4 if fp32 and d.name == "matmul" else 1), 0.0
            if eng.name == "act":
                return 0.22 + n * 0.00075, 0.0
            if eng.name == "dve":
                return 0.12 + n * (0.0066 if d.name == "reciprocal" else 0.00105), 0.0
            if d.kw.get("op", None) == ALU.pow:
                return 0.4 + n * 0.125, 0.0
            return 0.25 + n * 0.0019, 0.0
        if kind == "dma":
            try:
                nbytes = int(rec[2][0].nbytes())
            except Exception:
                nbytes = 65536
            return 0.15, 2.0 + nbytes / 120e3
        if kind == "idma":
            return 0.3, 3.0 + 4.0
        return 0.02, 0.0

    def flush(self):
        recs, self.recs = self.recs, []
        self.recording = False
        n = len(recs)
        if n == 0:
            self.recording = True
            return
        import heapq
        preds = [None] * n
        lastw, readers = {}, {}
        for i, rec in enumerate(recs):
            kind = rec[0]
            if kind == "op":
                rd, wr = list(rec[3]), list(rec[4])
            elif kind == "dma":
                R, mode = rec[3], rec[4]
                rd, wr = (list(rec[6]), [R]) if mode == "w" else ([R] + list(rec[6]), [])
                wr = wr + list(rec[7])
            elif kind == "idma":
                R, mode = rec[3], rec[4]
                rd, wr = (list(rec[5]), [R]) if mode == "w" else ([R] + list(rec[5]), [])
            else:
                rd, wr = [], [rec[2]] + list(rec[3])
            wr = wr + [R for R in rd if R.excl and R not in wr]
            rd = [R for R in rd if not R.excl]
            p = set()
            for R in rd:
                if R in lastw:
                    p.add(lastw[R])
            for R in wr:
                if R in lastw:
                    p.add(lastw[R])
                p.update(readers.get(R, ()))
            for R in rd:
                readers.setdefault(R, []).append(i)
            for R in wr:
                lastw[R] = i
                readers[R] = []
            p.discard(i)
            preds[i] = p
        succs = [[] for _ in range(n)]
        indeg = [0] * n
        for i, p in enumerate(preds):
            indeg[i] = len(p)
            for j in p:
                succs[j].append(i)
        costs = [self._cost(r) for r in recs]
        indeg0 = list(indeg)

        def run_sched(ALPHA):
            indeg = list(indeg0)
            te = [0.0] * len(self.engs)
            fin = [0.0] * n
            dep = [0.0] * n
            bl = [0.0] * n
            for i in range(n - 1, -1, -1):
                m = 0.0
                for j in succs[i]:
                    if bl[j] > m:
                        m = bl[j]
                bl[i] = m + costs[i][0] + costs[i][1] + 0.15
            heap = [(0.0 - ALPHA * bl[i], i) for i in range(n) if indeg[i] == 0]
            heapq.heapify(heap)
            order = []
            while heap:
                key, i = heapq.heappop(heap)
                e = recs[i][1].idx
                est = max(te[e], dep[i])
                k2 = est - ALPHA * bl[i]
                if heap and k2 > heap[0][0] + 1e-9 and k2 > key + 1e-9:
                    heapq.heappush(heap, (k2, i))
                    continue
                c, lat = costs[i]
                te[e] = est + c
                fin[i] = est + c + lat
                order.append(i)
                for j in succs[i]:
                    hop = 0.05 if recs[j][1].idx == e else 0.2
                    if fin[i] + hop > dep[j]:
                        dep[j] = fin[i] + hop
                    indeg[j] -= 1
                    if indeg[j] == 0:
                        heapq.heappush(heap, (max(dep[j], te[recs[j][1].idx]) - ALPHA * bl[j], j))
            return order, te

        best = None
        for al in getattr(self, "alphas", (0.0, 0.003, 0.01, 0.03)):
            o_, te_ = run_sched(al)
            if best is None or max(te_) < max(best[1]):
                best = (o_, te_, al)
        order, te, _al = best
        assert len(order) == n, (len(order), n)
        if not getattr(self, "sched", True):
            order = list(range(n))
        self.sim_time = getattr(self, "sim_time", 0.0) + max(te)
        busy = [0.0] * len(self.engs)
        for i in range(n):
            busy[recs[i][1].idx] += costs[i][0]
        self.sim_log = getattr(self, "sim_log", [])
        self.sim_log.append((n, max(te), [round(b) for b in busy]))
        for i in order:
            rec = recs[i]
            kind, eng = rec[0], rec[1]
            if kind == "op":
                eng.op(rec[2], rec[3], rec[4])
            elif kind == "dma":
                out, in_, kw = rec[2]
                eng.dma(out, in_, rec[3], rec[4], part=rec[5], extra_reads=rec[6], **kw)
            elif kind == "idma":
                eng.indirect_dma(rec[3], rec[4], extra_reads=rec[5], **rec[2])
            else:
                eng._wait_dma(rec[2])
        self.recording = True

    def push_scope(self):
        self._stack.append(None)
        self.scopes.append(len(self.all_res))

    def pop_scope(self):
        self.barrier()
        while True:
            cm = self._stack.pop()
            if cm is None:
                break
            cm.__exit__(None, None, None)
        n0 = self.scopes.pop()
        for R in self.all_res[n0:]:
            if R.dsem is not None:
                self.free_dsems.append((R.dsem, R.dn))
        del self.all_res[n0:]
        for e in self.engs:
            e.seen_d = {}

    def barrier(self):
        self.flush()
        self.recording = False
        sp = self.sp
        self.drain_all(sp)
        ins = sp.eng.nop()
        sp.n += 1
        ins.then_inc(sp.sem, 1)
        for e in self.engs:
            if e is not sp:
                e._wait_eng(sp.idx, sp.n)
        for R in self.all_res:
            R.w = None
            R.r = {}
        self.recording = True

    def close(self):
        while self._stack:
            cm = self._stack.pop()
            if cm is not None:
                cm.__exit__(None, None, None)


def groups_of(NSEQ, SEQ, with_meta=True, gmax=512):
    g = [(0, 128)] if with_meta else []
    for s in range(NSEQ):
        t = 0
        while t < SEQ:
            n = min(gmax, SEQ - t)
            g.append((128 + s * SEQ + t, n))
            t += n
    return g


class MK:
    def __init__(self, NSEQ, SEQ, CAP, debug=()):
        self.NSEQ, self.SEQ, self.CAP = NSEQ, SEQ, CAP
        self.T = NSEQ * SEQ
        self.NT = 128 + self.T
        self.NTILES = self.NT // 128
        self.debug = set(debug)
        nc = bass.Bass("TRN2", target_bir_lowering=False)
        self.fw = FW(nc)
        self.nc = NCProxy(nc)
        T, NT = self.T, self.NT

        def inp(name, shape, dt=F32):
            return nc.dram_tensor(name, list(shape), dt, kind="ExternalInput").ap()

        self.x = inp("x", [T, D])
        self.meta = inp("meta", [16, D])
        self.g_mix = inp("g_mix", [1, D])
        self.w_in = inp("w_in", [D, D_IN])
        self.ident = inp("ident", [128, 128])
        self.out = nc.dram_tensor("out", [T, D], F32, kind="ExternalOutput").ap()

        def scr(name, shape, dt):
            kind = "ExternalOutput" if name in self.debug else "Internal"
            return nc.dram_tensor(name, list(shape), dt, kind=kind).ap()

        self.PF = {}
        for nm, rows in [("qkv", 3072), ("z", 1024), ("qg", 512), ("kg", 512), ("rg", 1024),
                         ("gd", 1024), ("gg", 1024)]:
            self.PF[nm] = scr("pf_" + nm, [rows, NT], BF16)
        self.PF_LR = scr("pf_lr", [16, NT], F32)
        self.PT_VG = scr("pt_vg", [NT, 1024], BF16)
        self.PT_AB = scr("pt_ab", [NT, 16], F32)

    def load_consts(self):
        fw, nc = self.fw, self.nc
        self.idf, self.idfR = fw.sbuf("idf", [128, 128], F32)
        self.idb, self.idbR = fw.sbuf("idb", [128, 128], BF16)
        fw.sp.dma(self.idf[:], self.ident, self.idfR, "w")
        fw.pool.dma(self.idb[:], self.ident, self.idbR, "w")

    def phase1(self):
        fw, nc = self.fw, self.nc
        NT, NTILES = self.NT, self.NTILES
        self.hnT, self.hnTR = fw.sbuf("hnT", [128, 8, NT], BF16)
        fw.push_scope()
        gb, gbR = fw.sbuf("gb", [128, D], F32)
        fw.sp.dma(gb[:], self.g_mix.partition_broadcast(128), gbR, "w")
        xb = [fw.sbuf(f"xb{i}", [128, D], F32) for i in range(3)]
        junk, junkR = fw.sbuf("junk", [128, D], BF16)
        ssb = [fw.sbuf(f"ss{i}", [128, 1], F32) for i in range(2)]
        hnb = [fw.sbuf(f"hn{i}", [128, D], BF16) for i in range(2)]
        ptb = [fw.psum(f"pt{i}", [128, 8, 128], BF16) for i in range(2)]
        for i in range(NTILES):
            xt, xR = xb[i % 3]
            ss, ssR = ssb[i % 2]
            hn, hnR = hnb[i % 2]
            pt, ptR = ptb[i % 2]
            if i == 0:
                fw.pool.op(lambda: nc.gpsimd.memset(xt[:], 0.0), writes=[xR])
                fw.sp.dma(xt[112:128, :], self.meta, xR, "w")
            else:
                fw.sp.dma(xt[:], self.x[(i - 1) * 128:i * 128, :], xR, "w")
            self.rms_rstd(xt, xR, junk, junkR, ss, ssR)
            fw.dve.op(lambda: nc.vector.scalar_tensor_tensor(
                out=hn[:], in0=xt[:], scalar=ss[:], in1=gb[:], op0=ALU.mult, op1=ALU.mult),
                reads=[xR, ssR, gbR], writes=[hnR])
            for kc in range(8):
                fw.pe.op(lambda: nc.tensor.transpose(out=pt[:, kc, :], in_=hn[:, kc * 128:(kc + 1) * 128],
                                                     identity=self.idb[:]),
                         reads=[hnR, self.idbR], writes=[ptR])
            fw.act.op(lambda: nc.scalar.copy(out=self.hnT[:, :, i * 128:(i + 1) * 128], in_=pt[:]),
                      reads=[ptR], writes=[self.hnTR])
        fw.pop_scope()

    def rms_rstd(self, xt, xR, junk, junkR, ss, ssR, n=D):
        fw, nc = self.fw, self.nc
        fw.act.op(lambda: nc.scalar.activation(out=junk[:], in_=xt[:], func=AF.Square, accum_out=ss[:]),
                  reads=[xR], writes=[junkR, ssR])
        fw.dve.op(lambda: nc.vector.tensor_scalar(out=ss[:], in0=ss[:], scalar1=1.0 / n, scalar2=EPS,
                                                  op0=ALU.mult, op1=ALU.add), reads=[ssR], writes=[ssR])
        fw.act.op(lambda: nc.scalar.sqrt(out=ss[:], in_=ss[:]), reads=[ssR], writes=[ssR])
        fw.dve.op(lambda: nc.vector.reciprocal(out=ss[:], in_=ss[:]), reads=[ssR], writes=[ssR])

    def phase2(self):
        fw, nc = self.fw, self.nc
        NT = self.NT
        fw.push_scope()
        groups = groups_of(self.NSEQ, self.SEQ)
        w_v = self.w_in.rearrange("(kc p) c -> p kc c", p=128)
        wb = [fw.sbuf(f"wb{i}", [128, 8, 512], BF16) for i in range(2)]
        pb = [fw.psum(f"pp{i}", [128, 512], F32) for i in range(4)]
        SG = 2048
        stg = [fw.sbuf(f"stg{i}", [128, SG], BF16) for i in range(3)]
        cnt = {"w": 0, "p": 0, "s": 0, "e": 0}

        def evac(out_ap, outR, ps, psR):
            if cnt["e"] % 2 == 0:
                fw.act.op(lambda: nc.scalar.copy(out=out_ap, in_=ps), reads=[psR], writes=[outR])
            else:
                fw.dve.op(lambda: nc.vector.tensor_copy(out=out_ap, in_=ps), reads=[psR], writes=[outR])
            cnt["e"] += 1

        cwp, cwpR = fw.sbuf("cwp", [128, 24, 4], F32)
        fw.sp.dma(cwp[:], self.convT, cwpR, "w")
        xrb = [fw.sbuf(f"xrb{i}", [128, 516], F32) for i in range(2)]
        accb = [fw.sbuf(f"accb{i}", [128, 512], F32) for i in range(2)]
        mh, mhR = fw.sbuf("mhist", [128, 4], F32)
        cst_ = {"i": 0, "prev": None}

        def conv_evac(st, sR, off, ps, psR, n0, N, ci):
            xr, xrR = xrb[cst_["i"] % 2]
            acc, accR = accb[cst_["i"] % 2]
            cst_["i"] += 1
            is_meta = n0 == 0
            seq_start = (not is_meta) and (n0 - 128) % self.SEQ == 0
            if is_meta:
                fw.pool.op(lambda: nc.gpsimd.memset(xr[:, 0:4], 0.0), writes=[xrR])
            elif seq_start:
                fw.pool.op(lambda: nc.gpsimd.tensor_copy(out=xr[:, 0:3], in_=mh[:, 0:3]), reads=[mhR], writes=[xrR])
            else:
                pxr, pxrR, pN = cst_["prev"]
                fw.pool.op(lambda: nc.gpsimd.tensor_copy(out=xr[:, 0:3], in_=pxr[:, pN:pN + 3]), reads=[pxrR], writes=[xrR])
            fw.act.op(lambda: nc.scalar.copy(out=xr[:, 3:3 + N], in_=ps[:, :N]), reads=[psR], writes=[xrR])
            if is_meta:
                fw.pool.op(lambda: nc.gpsimd.tensor_copy(out=mh[:, 0:3], in_=xr[:, N:N + 3]), reads=[xrR], writes=[mhR])
            fw.act.op(lambda: nc.scalar.mul(out=acc[:, :N], in_=xr[:, 0:N], mul=cwp[:, ci, 0:1]), reads=[xrR, cwpR], writes=[accR])
            for j in (1, 2):
                fw.dve.op(lambda: nc.vector.scalar_tensor_tensor(out=acc[:, :N], in0=xr[:, j:j + N], scalar=cwp[:, ci, j:j + 1], in1=acc[:, :N],
                                                                 op0=ALU.mult, op1=ALU.add), reads=[xrR, cwpR, accR], writes=[accR])
            fw.dve.op(lambda: nc.vector.scalar_tensor_tensor(out=st[:, off:off + N], in0=xr[:, 3:3 + N], scalar=cwp[:, ci, 3:4], in1=acc[:, :N],
                                                             op0=ALU.mult, op1=ALU.add), reads=[xrR, cwpR, accR], writes=[sR])
            cst_["prev"] = (xr, xrR, N)

        segs = [("qkv", 0, 3072), ("z", 3072, 1024), ("qg", 4112, 512), ("kg", 4624, 512),
                ("rg", 6160, 1024), ("gd", 7200, 1024), ("gg", 8224, 1024)]
        for nm, c0, ncols in segs:
            dst = self.PF[nm]
            for b0 in range(0, ncols, 512):
                wt, wR = wb[cnt["w"] % 2]
                cnt["w"] += 1
                fw.pool.dma(wt[:], w_v[:, :, c0 + b0:c0 + b0 + 512], wR, "w")
                for cc in range(4):
                    r0 = b0 + cc * 128
                    gi = 0
                    while gi < len(groups):
                        st, sR = stg[cnt["s"] % 3]
                        cnt["s"] += 1
                        n_start = groups[gi][0]
                        off = 0
                        while gi < len(groups) and off + groups[gi][1] <= SG and groups[gi][0] == n_start + off:
                            n0, N = groups[gi]
                            ps, psR = pb[cnt["p"] % 4]
                            cnt["p"] += 1
                            for kc in range(8):
                                fw.pe.op(lambda: nc.tensor.matmul(
                                    ps[:, :N], lhsT=wt[:, kc, cc * 128:(cc + 1) * 128], rhs=self.hnT[:, kc, n0:n0 + N],
                                    start=(kc == 0), stop=(kc == 7)), reads=[wR, self.hnTR], writes=[psR])
                            if nm == "qkv":
                                conv_evac(st, sR, off, ps, psR, n0, N, r0 // 128)
                            else:
                                evac(st[:, off:off + N], sR, ps[:, :N], psR)
                            off += N
                            gi += 1
                        fw.sp.dma(dst[r0:r0 + 128, n_start:n_start + off], st[:, :off], sR, "r")
        wl, wlR = fw.sbuf("wl", [128, 8, 16], BF16)
        fw.pool.dma(wl[:], w_v[:, :, 7184:7200], wlR, "w")
        lst = [fw.sbuf(f"lst{i}", [16, 512], F32) for i in range(2)]
        for gi, (n0, N) in enumerate(groups):
            ps, psR = pb[cnt["p"] % 4]
            cnt["p"] += 1
            st, sR = lst[gi % 2]
            for kc in range(8):
                fw.pe.op(lambda: nc.tensor.matmul(ps[:16, :N], lhsT=wl[:, kc, :], rhs=self.hnT[:, kc, n0:n0 + N],
                                                  start=(kc == 0), stop=(kc == 7)),
                         reads=[wlR, self.hnTR], writes=[psR])
            evac(st[:, :N], sR, ps[:16, :N], psR)
            fw.sp.dma(self.PF_LR[:, n0:n0 + N], st[:, :N], sR, "r")
        wv = [fw.sbuf(f"wv{i}", [128, 8, 512], BF16) for i in range(2)]
        for hf in range(2):
            fw.pool.dma(wv[hf][0][:], w_v[:, :, 5136 + hf * 512:5136 + (hf + 1) * 512], wv[hf][1], "w")
        wab, wabR = fw.sbuf("wab", [128, 8, 16], BF16)
        fw.pool.dma(wab[:], w_v[:, :, 4096:4112], wabR, "w")
        vst = [fw.sbuf(f"vst{i}", [128, 1024], BF16) for i in range(2)]
        ast = [fw.sbuf(f"ast{i}", [128, 16], F32) for i in range(2)]
        for i in range(self.NTILES):
            st, sR = vst[i % 2]
            for hf in range(2):
                ps, psR = pb[cnt["p"] % 4]
                cnt["p"] += 1
                for kc in range(8):
                    fw.pe.op(lambda: nc.tensor.matmul(ps[:], lhsT=self.hnT[:, kc, i * 128:(i + 1) * 128],
                                                      rhs=wv[hf][0][:, kc, :], start=(kc == 0), stop=(kc == 7)),
                             reads=[wv[hf][1], self.hnTR], writes=[psR])
                evac(st[:, hf * 512:(hf + 1) * 512], sR, ps[:], psR)
            fw.sp.dma(self.PT_VG[i * 128:(i + 1) * 128, :], st[:], sR, "r")
            ps, psR = pb[cnt["p"] % 4]
            cnt["p"] += 1
            at, aR = ast[i % 2]
            for kc in range(8):
                fw.pe.op(lambda: nc.tensor.matmul(ps[:, :16], lhsT=self.hnT[:, kc, i * 128:(i + 1) * 128],
                                                  rhs=wab[:, kc, :], start=(kc == 0), stop=(kc == 7)),
                         reads=[wabR, self.hnTR], writes=[psR])
            evac(at[:], aR, ps[:, :16], psR)
            fw.sp.dma(self.PT_AB[i * 128:(i + 1) * 128, :], at[:], aR, "r")
        fw.pop_scope()

    def finish(self):
        self.fw.flush()
        self.fw.recording = False
        self.fw.drain_all(self.fw.sp)
        self.fw.barrier()
        self.fw.close()
        return self.nc._nc


def run_interleaved(gens):
    gens = list(gens)
    while gens:
        for g in list(gens):
            try:
                next(g)
            except StopIteration:
                gens.remove(g)


class Slots:
    def __init__(self, t, R):
        self.t, self.R = t, R
        self.i = 0

    def next(self):
        i = self.i % 4
        self.i += 1
        return self.t[:, i * 128:(i + 1) * 128], self.R


def bf16_view(ap):
    return ap.bitcast(BF16)[:, 0:128]


def p3_setup(self):
    nc = self.nc
    NT = self.NT
    self.convT = nc.dram_tensor("convT", [128, 24, 4], F32, kind="ExternalInput").ap()
    self.a_log = nc.dram_tensor("a_log", [1, 8], F32, kind="ExternalInput").ap()
    self.dt_bias = nc.dram_tensor("dt_bias", [1, 8], F32, kind="ExternalInput").ap()
    self.nh_dn = nc.dram_tensor("nh_dn", [128, 1], F32, kind="ExternalInput").ap()
    self.consts = nc.dram_tensor("consts", [128, 4 * 128 + 2], F32, kind="ExternalInput").ap()
    kind = "ExternalOutput" if "od" in self.debug else "Internal"
    self.OD = nc.dram_tensor("od", [1024, NT], BF16, kind=kind).ap()


def load_consts2(self):
    fw, nc = self.fw, self.nc
    self.cst, self.cstR = fw.sbuf("cst", [128, 4 * 128 + 2], F32)
    fw.sp.dma(self.cst[:], self.consts, self.cstR, "w")
    c = self.cst
    self.tri, self.trs, self.mlow = c[:, 128:256], c[:, 256:384], c[:, 384:512]
    self.ind = c[:, 512:514]
    self.onesf, self.onesfR = fw.sbuf("onesf", [128, 128], F32)
    self.onesb, self.onesbR = fw.sbuf("onesb", [128, 128], BF16)
    self.mhalf, self.mhalfR = fw.sbuf("mhalf", [128, 16], F32)
    fw.pool.op(lambda: nc.gpsimd.memset(self.onesf[:], 1.0), writes=[self.onesfR])
    fw.pool.op(lambda: nc.gpsimd.memset(self.onesb[:], 1.0), writes=[self.onesbR])
    fw.pool.op(lambda: nc.gpsimd.memset(self.mhalf[:], -0.5), writes=[self.mhalfR])
    self.epsc, self.epsR = fw.sbuf("epsc", [128, 2], F32)
    self.eps1, self.eps4 = self.epsc[:, 0:1], self.epsc[:, 1:2]
    fw.pool.op(lambda: nc.gpsimd.memset(self.epsc[:, 0:1], EPS), writes=[self.epsR])
    fw.pool.op(lambda: nc.gpsimd.memset(self.epsc[:, 1:2], 4 * EPS), writes=[self.epsR])


def phase3a(self):
    fw, nc = self.fw, self.nc
    NTL = self.NTILES
    mk = lambda nm, w: fw.sbuf(nm, [128, NTL, w], F32)
    self.G, self.GR_ = mk("G", 8)
    self.BETA, self.BETAR = mk("BETA", 8)
    self.HB, self.HBR = mk("HB", 8)
    self.KBS, self.KBSR = mk("KBS", 8)
    self.EGR, self.EGRR = mk("EGR", 8)
    self.EGL, self.EGLR = mk("EGL", 16)
    fw.push_scope()
    dtb, dtbR = fw.sbuf("dtb", [128, 8], F32)
    negA, negAR = fw.sbuf("negA", [128, 8], F32)
    fw.sp.dma(dtb[:], self.dt_bias.partition_broadcast(128), dtbR, "w")
    fw.sp.dma(negA[:], self.a_log.partition_broadcast(128), negAR, "w")
    fw.act.op(lambda: nc.scalar.activation(out=negA[:], in_=negA[:], func=AF.Exp), reads=[negAR], writes=[negAR])
    fw.dve.op(lambda: nc.vector.tensor_scalar(out=negA[:], in0=negA[:], scalar1=-1.0, scalar2=None, op0=ALU.mult),
              reads=[negAR], writes=[negAR])
    abb = [fw.sbuf(f"abb{i}", [128, 16], F32) for i in range(2)]
    t8, t8R = fw.sbuf("t8", [128, 16], F32)
    r2, r2R = fw.sbuf("r2", [128, 16], F32)
    ex, exR = fw.sbuf("ex", [128, 32], F32)
    ps, psR = fw.psum("p3a", [128, 32], F32)
    E1, E1R = fw.sbuf("E1", [128, NTL, 16], F32)
    for i in range(NTL):
        ab, abR = abb[i % 2]
        fw.sp.dma(ab[:], self.PT_AB[i * 128:(i + 1) * 128, :], abR, "w")
        fw.dve.op(lambda: nc.vector.tensor_tensor(out=t8[:, 0:8], in0=ab[:, 0:8], in1=dtb[:], op=ALU.add),
                  reads=[abR, dtbR], writes=[t8R])
        fw.dve.op(lambda: nc.vector.tensor_scalar(out=t8[:, 8:16], in0=ab[:, 8:16], scalar1=-1.0, scalar2=None,
                                                  op0=ALU.mult), reads=[abR], writes=[t8R])
        fw.act.op(lambda: nc.scalar.activation(out=E1[:, i, :], in_=t8[:], func=AF.Exp), reads=[t8R], writes=[E1R])
    SP, SPR = fw.sbuf("SP", [128, NTL, 8], F32)
    fw.act.op(lambda: nc.scalar.activation(out=SP[:], in_=E1[:, :, 0:8], func=AF.Ln, bias=1.0),
              reads=[E1R], writes=[SPR])
    fw.dve.op(lambda: nc.vector.tensor_tensor(out=self.G[:], in0=SP[:], in1=negA[:].unsqueeze(1).to_broadcast([128, NTL, 8]),
                                              op=ALU.mult), reads=[SPR, negAR], writes=[self.GR_])
    fw.dve.op(lambda: nc.vector.tensor_scalar(out=E1[:, :, 8:16], in0=E1[:, :, 8:16], scalar1=1.0, scalar2=None,
                                              op0=ALU.add), reads=[E1R], writes=[E1R])
    fw.dve.op(lambda: nc.vector.reciprocal(out=self.BETA[:], in_=E1[:, :, 8:16]), reads=[E1R], writes=[self.BETAR])
    fw.dve.op(lambda: nc.vector.tensor_scalar(out=self.HB[:], in0=self.BETA[:], scalar1=0.5, scalar2=None,
                                              op0=ALU.mult), reads=[self.BETAR], writes=[self.HBR])
    for i in range(NTL):
        for c in range(2):
            fw.dve.op(lambda: nc.vector.tensor_scalar(out=r2[:, c * 8:(c + 1) * 8], in0=self.G[:, i, :],
                                                      scalar1=self.ind[:, c:c + 1], scalar2=None, op0=ALU.mult),
                      reads=[self.GR_, self.cstR], writes=[r2R])
        fw.pe.op(lambda: nc.tensor.matmul(ps[:, 0:8], lhsT=self.tri, rhs=self.G[:, i, :], start=True, stop=True),
                 reads=[self.cstR, self.GR_], writes=[psR])
        fw.pe.op(lambda: nc.tensor.matmul(ps[:, 8:16], lhsT=self.trs, rhs=self.G[:, i, :], start=True, stop=True),
                 reads=[self.cstR, self.GR_], writes=[psR])
        fw.pe.op(lambda: nc.tensor.matmul(ps[:, 16:32], lhsT=self.onesf[:], rhs=r2[:], start=True, stop=True),
                 reads=[self.onesfR, r2R], writes=[psR])
        fw.act.op(lambda: nc.scalar.activation(out=ex[:], in_=ps[:], func=AF.Exp), reads=[psR], writes=[exR])
        fw.dve.op(lambda: nc.vector.tensor_tensor(out=self.KBS[:, i, :], in0=self.BETA[:, i, :], in1=ex[:, 0:8],
                                                  op=ALU.mult), reads=[self.BETAR, exR], writes=[self.KBSR])
        fw.dve.op(lambda: nc.vector.tensor_copy(out=self.EGR[:, i, :], in_=ex[:, 8:16]), reads=[exR], writes=[self.EGRR])
        fw.dve.op(lambda: nc.vector.tensor_copy(out=self.EGL[:, i, :], in_=ex[:, 16:32]), reads=[exR], writes=[self.EGLR])
    fw.pop_scope()


def dn_head(self, h, hb):
    fw, nc = self.fw, self.nc
    SEQ = self.SEQ
    groups = groups_of(self.NSEQ, SEQ)
    pg, pgR = hb["pg"]
    slA = Slots(pg, pgR)
    slD = Slots(*hb["pd"])
    PFq = self.PF["qkv"]
    (S32, S32R), (Sb, SbR), (Sm, SmR) = hb["S32"], hb["Sb"], hb["Sm"]
    B = lambda k: hb[k]
    for seg in range(3):
        for j in range(4):
            dg, dgR = hb["dg"][seg][j]
            fw.pool.op(lambda: nc.gpsimd.tensor_scalar(out=dg[:], in0=self.idf[:], scalar1=self.cw[:, seg * 8 + h, j:j + 1],
                                                        scalar2=None, op0=ALU.mult),
                       reads=[self.idfR, self.cwR], writes=[dgR])
    yield
    for (n0, N) in groups:
        is_meta = n0 == 0
        seq_start = (not is_meta) and (n0 - 128) % SEQ == 0
        if is_meta:
            fw.pool.op(lambda: nc.gpsimd.memset(S32[:], 0.0), writes=[S32R])
            fw.pool.op(lambda: nc.gpsimd.memset(Sb[:], 0.0), writes=[SbR])
        elif seq_start:
            fw.pool.op(lambda: nc.gpsimd.tensor_copy(out=S32[:], in_=Sm[:]), reads=[SmR], writes=[S32R])
            fw.act.op(lambda: nc.scalar.copy(out=Sb[:], in_=Sm[:]), reads=[SmR], writes=[SbR])
        th, thR = B("th")
        for seg in range(3):
            raw, rawR = hb["raw"][seg]
            rows = slice(seg * 1024 + h * 128, seg * 1024 + (h + 1) * 128)
            if is_meta:
                fw.pool.op(lambda: nc.gpsimd.memset(raw[:, 0:4], 0.0), writes=[rawR])
                fw.sp.dma(raw[:, 3:3 + N], PFq[rows, 0:N], rawR, "w")
            elif seq_start:
                fw.sp.dma(raw[:, 0:3], PFq[rows, 125:128], rawR, "w")
                fw.sp.dma(raw[:, 3:3 + N], PFq[rows, n0:n0 + N], rawR, "w", part=True)
            else:
                fw.sp.dma(raw[:, 0:3 + N], PFq[rows, n0 - 3:n0 + N], rawR, "w")
            for j in range(4):
                dg, dgR = hb["dg"][seg][j]
                fw.pe.op(lambda: nc.tensor.matmul(pg[:, :N], lhsT=dg[:], rhs=raw[:, j:j + N], start=(j == 0), stop=(j == 3)),
                         reads=[dgR, rawR], writes=[pgR])
            yield
            fw.act.op(lambda: nc.scalar.activation(out=th[:, :N], in_=pg[:, :N], func=AF.Tanh, scale=0.5),
                      reads=[pgR], writes=[thR])
            c2, c2R = hb["c2"][seg]
            fw.dve.op(lambda: nc.vector.scalar_tensor_tensor(out=c2[:, :N], in0=th[:, :N], scalar=1.0, in1=pg[:, :N],
                                                             op0=ALU.add, op1=ALU.mult), reads=[thR, pgR], writes=[c2R])
            yield
        sq, sqR = B("sq")
        ssb, ssbR = B("ssb")
        rn, rnR = B("rn")
        nT = {}
        for seg in range(2):
            c2, c2R = hb["c2"][seg]
            fw.act.op(lambda: nc.scalar.activation(out=sq[:, :N], in_=c2[:, :N], func=AF.Square), reads=[c2R], writes=[sqR])
            fw.pe.op(lambda: nc.tensor.matmul(pg[:, :N], lhsT=self.onesb[:], rhs=sq[:, :N], start=True, stop=True),
                     reads=[self.onesbR, sqR], writes=[pgR])
            yield
            fw.act.op(lambda: nc.scalar.activation(out=ssb[:, :N], in_=pg[:, :N], func=AF.Sqrt, bias=self.eps4[:, 0:1]),
                      reads=[pgR, self.epsR], writes=[ssbR])
            fw.dve.op(lambda: nc.vector.reciprocal(out=rn[:, :N], in_=ssb[:, :N]), reads=[ssbR], writes=[rnR])
            nt, ntR = hb["nT"][seg]
            sc = 128 ** -0.5 if seg == 0 else 1.0
            fw.dve.op(lambda: nc.vector.scalar_tensor_tensor(out=nt[:, :N], in0=c2[:, :N], scalar=sc, in1=rn[:, :N],
                                                             op0=ALU.mult, op1=ALU.mult), reads=[c2R, rnR], writes=[ntR])
            yield
        (qnT, qnR), (knT, knR) = hb["nT"]
        vcT, vcR = hb["c2"][2]
        oT, oTR = B("oT")
        for tl in range(N // 128):
            ti = n0 // 128 + tl
            cs = slice(tl * 128, (tl + 1) * 128)
            gB, gBR = B("gB")
            ngB, ngBR = B("ngB")
            fw.act.op(lambda: nc.scalar.mul(out=gB[:], in_=self.onesf[:], mul=self.G[:, ti, h:h + 1]),
                      reads=[self.onesfR, self.GR_], writes=[gBR])
            fw.act.op(lambda: nc.scalar.mul(out=ngB[:], in_=self.onesf[:], mul=self.NG[:, ti, h:h + 1]),
                      reads=[self.onesfR, self.NGR], writes=[ngBR])
            s_kk, s_kkR = slD.next()
            fw.pe.op(lambda: nc.tensor.matmul(s_kk, lhsT=knT[:, cs], rhs=knT[:, cs], start=True, stop=True),
                     reads=[knR], writes=[s_kkR])
            s_qk, s_qkR = slD.next()
            fw.pe.op(lambda: nc.tensor.matmul(s_qk, lhsT=qnT[:, cs], rhs=knT[:, cs], start=True, stop=True),
                     reads=[qnR, knR], writes=[s_qkR])
            s_df, s_dfR = slD.next()
            fw.pe.op(lambda: nc.tensor.matmul(s_df, lhsT=self.tri, rhs=gB[:], start=True, stop=False),
                     reads=[self.cstR, gBR], writes=[s_dfR])
            fw.pe.op(lambda: nc.tensor.matmul(s_df, lhsT=ngB[:], rhs=self.tri, start=False, stop=True),
                     reads=[self.cstR, ngBR], writes=[s_dfR])
            s_gc, s_gcR = slA.next()
            fw.pe.op(lambda: nc.tensor.matmul(s_gc, lhsT=gB[:], rhs=self.tri, start=True, stop=True),
                     reads=[self.cstR, gBR], writes=[s_gcR])
            yield
            dmf, dmfR = B("dmf")
            Dm, DmR = B("Dm")
            Ds, DsR = B("Ds")
            egr, egrR = B("egr")
            qdT, qdR = B("qdT")
            fw.dve.op(lambda: nc.vector.tensor_scalar(out=dmf[:], in0=s_df, scalar1=0.0, scalar2=None, op0=ALU.min),
                      reads=[s_dfR], writes=[dmfR])
            fw.act.op(lambda: nc.scalar.activation(out=dmf[:], in_=dmf[:], func=AF.Exp), reads=[dmfR], writes=[dmfR])
            fw.pool.op(lambda: nc.gpsimd.tensor_tensor(out=Dm[:], in0=dmf[:], in1=self.mlow, op=ALU.mult),
                       reads=[dmfR, self.cstR], writes=[DmR])
            fw.pool.op(lambda: nc.gpsimd.tensor_tensor(out=Ds[:], in0=Dm[:], in1=self.idf[:], op=ALU.subtract),
                       reads=[DmR, self.idfR], writes=[DsR])
            fw.act.op(lambda: nc.scalar.activation(out=egr[:], in_=s_gc, func=AF.Exp), reads=[s_gcR], writes=[egrR])
            fw.dve.op(lambda: nc.vector.tensor_tensor(out=qdT[:], in0=qnT[:, cs], in1=egr[:], op=ALU.mult),
                      reads=[qnR, egrR], writes=[qdR])
            P, PR = hb["Pm"][0]
            fw.dve.op(lambda: nc.vector.scalar_tensor_tensor(out=P[:], in0=s_kk, scalar=self.BETA[:, ti, h:h + 1], in1=Ds[:],
                                                             op0=ALU.mult, op1=ALU.mult),
                      reads=[s_kkR, self.BETAR, DsR], writes=[PR])
            QK, QKR = B("QK")
            fw.dve.op(lambda: nc.vector.tensor_tensor(out=QK[:], in0=s_qk, in1=Dm[:], op=ALU.mult),
                      reads=[s_qkR, DmR], writes=[QKR])
            yield
            t_p, t_pR = slA.next()
            fw.pe.op(lambda: nc.tensor.transpose(out=bf16_view(t_p), in_=P[:], identity=self.idb[:]),
                     reads=[PR, self.idbR], writes=[t_pR])
            t_qk, t_qkR = slA.next()
            fw.pe.op(lambda: nc.tensor.transpose(out=bf16_view(t_qk), in_=QK[:], identity=self.idb[:]),
                     reads=[QKR, self.idbR], writes=[t_qkR])
            t_k, t_kR = slA.next()
            fw.pe.op(lambda: nc.tensor.transpose(out=bf16_view(t_k), in_=knT[:, cs], identity=self.idb[:]),
                     reads=[knR, self.idbR], writes=[t_kR])
            t_v, t_vR = slA.next()
            fw.pe.op(lambda: nc.tensor.transpose(out=bf16_view(t_v), in_=vcT[:, cs], identity=self.idb[:]),
                     reads=[vcR, self.idbR], writes=[t_vR])
            yield
            Q, QR = hb["Qm"][0]
            X, XR = hb["Xm"][0]
            QKT, QKTR = B("QKT")
            kb, kbR = B("kb")
            kdec, kdecR = B("kdec")
            vb, vbR = B("vb")
            fw.act.op(lambda: nc.scalar.copy(out=Q[:], in_=bf16_view(t_p)), reads=[t_pR], writes=[QR])
            fw.dve.op(lambda: nc.vector.tensor_tensor(out=X[:], in0=self.idb[:], in1=Q[:], op=ALU.subtract),
                      reads=[self.idbR, QR], writes=[XR])
            fw.act.op(lambda: nc.scalar.copy(out=QKT[:], in_=bf16_view(t_qk)), reads=[t_qkR], writes=[QKTR])
            fw.act.op(lambda: nc.scalar.mul(out=kb[:], in_=bf16_view(t_k), mul=self.KBS[:, ti, h:h + 1]),
                      reads=[t_kR, self.KBSR], writes=[kbR])
            fw.act.op(lambda: nc.scalar.mul(out=kdec[:], in_=bf16_view(t_k), mul=self.EGR[:, ti, h:h + 1]),
                      reads=[t_kR, self.EGRR], writes=[kdecR])
            fw.act.op(lambda: nc.scalar.mul(out=vb[:], in_=bf16_view(t_v), mul=self.HB[:, ti, h:h + 1]),
                      reads=[t_vR, self.HBR], writes=[vbR])
            yield
            for lvl in range(1, 6):
                Pn, PnR = hb["Pm"][lvl % 2]
                Qn, QnR = hb["Qm"][lvl % 2]
                Xn, XnR = hb["Xm"][lvl % 2]
                if lvl < 5:
                    s_q, s_qR = slD.next()
                    fw.pe.op(lambda: nc.tensor.matmul(s_q, lhsT=P[:], rhs=Q[:], start=True, stop=True),
                             reads=[PR, QR], writes=[s_qR])
                s_p, s_pR = slA.next()
                fw.pe.op(lambda: nc.tensor.matmul(s_p, lhsT=Q[:], rhs=P[:], start=True, stop=True),
                         reads=[PR, QR], writes=[s_pR])
                yield
                fw.act.op(lambda: nc.scalar.copy(out=Pn[:], in_=s_p), reads=[s_pR], writes=[PnR])
                if lvl < 5:
                    fw.dve.op(lambda: nc.vector.tensor_copy(out=Qn[:], in_=s_q), reads=[s_qR], writes=[QnR])
                s_x, s_xR = slD.next()
                fw.pe.op(lambda: nc.tensor.matmul(s_x, lhsT=Pn[:], rhs=X[:], start=True, stop=True),
                         reads=[PnR, XR], writes=[s_xR])
                yield
                fw.dve.op(lambda: nc.vector.tensor_tensor(out=Xn[:], in0=X[:], in1=s_x, op=ALU.add),
                          reads=[XR, s_xR], writes=[XnR])
                P, PR, Q, QR, X, XR = Pn, PnR, Qn, QnR, Xn, XnR
            s_u, s_uR = slA.next()
            fw.pe.op(lambda: nc.tensor.matmul(s_u, lhsT=X[:], rhs=vb[:], start=True, stop=True),
                     reads=[XR, vbR], writes=[s_uR])
            s_w, s_wR = slD.next()
            fw.pe.op(lambda: nc.tensor.matmul(s_w, lhsT=kb[:], rhs=X[:], start=True, stop=True),
                     reads=[XR, kbR], writes=[s_wR])
            yield
            usb, usbR = B("usb")
            nwT, nwTR = B("nwT")
            vnew, vnewR = B("vnew")
            fw.act.op(lambda: nc.scalar.copy(out=usb[:], in_=s_u), reads=[s_uR], writes=[usbR])
            fw.dve.op(lambda: nc.vector.tensor_scalar(out=nwT[:], in0=s_w, scalar1=-1.0, scalar2=None, op0=ALU.mult),
                      reads=[s_wR], writes=[nwTR])
            yield
            for c in range(2):
                r = slice(c * 64, (c + 1) * 64)
                s_ws, s_wsR = slD.next()
                fw.pe.op(lambda: nc.tensor.matmul(s_ws[r, :], lhsT=nwT[:, r], rhs=Sb[:], start=True, stop=True),
                         reads=[nwTR, SbR], writes=[s_wsR])
                yield
                fw.dve.op(lambda: nc.vector.tensor_tensor(out=vnew[r, :], in0=usb[r, :], in1=s_ws[r, :], op=ALU.add),
                          reads=[usbR, s_wsR], writes=[vnewR])
                s_o, s_oR = slA.next()
                fw.pe.op(lambda: nc.tensor.matmul(s_o[:, 0:64], lhsT=Sb[:], rhs=qdT[:, r], start=True, stop=False),
                         reads=[SbR, qdR], writes=[s_oR])
                fw.pe.op(lambda: nc.tensor.matmul(s_o[:, 0:64], lhsT=vnew[r, :], rhs=QKT[r, r], start=False, stop=True),
                         reads=[vnewR, QKTR], writes=[s_oR])
                s_s, s_sR = slD.next()
                fw.pe.op(lambda: nc.tensor.matmul(s_s, lhsT=kdec[r, :], rhs=vnew[r, :], start=True, stop=True),
                         reads=[kdecR, vnewR], writes=[s_sR])
                yield
                fw.act.op(lambda: nc.scalar.copy(out=oT[:, tl * 128 + c * 64:tl * 128 + (c + 1) * 64], in_=s_o[:, 0:64]),
                          reads=[s_oR], writes=[oTR])
                fw.dve.op(lambda: nc.vector.scalar_tensor_tensor(out=S32[:], in0=S32[:], scalar=self.EGL[:, ti, c * 8 + h:c * 8 + h + 1],
                                                                 in1=s_s, op0=ALU.mult, op1=ALU.add),
                          reads=[S32R, self.EGLR, s_sR], writes=[S32R])
                fw.act.op(lambda: nc.scalar.copy(out=Sb[:], in_=S32[:]), reads=[S32R], writes=[SbR])
                yield
        if is_meta:
            fw.pool.op(lambda: nc.gpsimd.tensor_copy(out=Sm[:], in_=S32[:]), reads=[S32R], writes=[SmR])
        fw.act.op(lambda: nc.scalar.activation(out=sq[:, :N], in_=oT[:, :N], func=AF.Square), reads=[oTR], writes=[sqR])
        fw.pe.op(lambda: nc.tensor.matmul(pg[:, :N], lhsT=self.onesb[:], rhs=sq[:, :N], start=True, stop=True),
                 reads=[self.onesbR, sqR], writes=[pgR])
        zt, ztR = B("zt")
        fw.sp.dma(zt[:, :N], self.PF["z"][h * 128:(h + 1) * 128, n0:n0 + N], ztR, "w")
        yield
        fw.act.op(lambda: nc.scalar.activation(out=ssb[:, :N], in_=pg[:, :N], func=AF.Sqrt, scale=1.0 / 128, bias=self.eps1[:, 0:1]),
                  reads=[pgR, self.epsR], writes=[ssbR])
        fw.dve.op(lambda: nc.vector.reciprocal(out=rn[:, :N], in_=ssb[:, :N]), reads=[ssbR], writes=[rnR])
        fw.act.op(lambda: nc.scalar.activation(out=th[:, :N], in_=zt[:, :N], func=AF.Tanh, scale=0.5),
                  reads=[ztR], writes=[thR])
        fw.dve.op(lambda: nc.vector.scalar_tensor_tensor(out=th[:, :N], in0=th[:, :N], scalar=1.0, in1=zt[:, :N],
                                                         op0=ALU.add, op1=ALU.mult), reads=[thR, ztR], writes=[thR])
        fw.dve.op(lambda: nc.vector.scalar_tensor_tensor(out=rn[:, :N], in0=oT[:, :N], scalar=self.nhh[:, 0:1], in1=rn[:, :N],
                                                         op0=ALU.mult, op1=ALU.mult), reads=[oTR, self.nhhR, rnR], writes=[rnR])
        od, odR = B("od")
        fw.dve.op(lambda: nc.vector.tensor_tensor(out=od[:, :N], in0=rn[:, :N], in1=th[:, :N], op=ALU.mult),
                  reads=[rnR, thR], writes=[odR])
        fw.sp.dma(self.OD[h * 128:(h + 1) * 128, n0:n0 + N], od[:, :N], odR, "r")
        yield


def phase3(self, HP=4):
    fw, nc = self.fw, self.nc
    fw.push_scope()
    self.cw, self.cwR = fw.sbuf("cw", [128, 24, 4], F32)
    fw.sp.dma(self.cw[:], self.convT, self.cwR, "w")
    self.nhh, self.nhhR = fw.sbuf("nhh", [128, 1], F32)
    fw.sp.dma(self.nhh[:], self.nh_dn, self.nhhR, "w")
    fw.dve.op(lambda: nc.vector.tensor_scalar(out=self.nhh[:], in0=self.nhh[:], scalar1=0.5, scalar2=None, op0=ALU.mult),
              reads=[self.nhhR], writes=[self.nhhR])
    hbs = []
    for p in range(HP):
        hb = {}
        hb["pg"] = fw.psum(f"pg{p}", [128, 512], F32)
        hb["pd"] = fw.psum(f"pd{p}", [128, 512], F32)
        sq = lambda nm, dt=BF16, w=128: fw.sbuf(f"{nm}{p}", [128, w], dt)
        hb["S32"], hb["Sb"], hb["Sm"] = sq("S32", F32), sq("Sb"), sq("Sm", F32)
        hb["dg"] = [[sq(f"dg{s}{j}_") for j in range(4)] for s in range(3)]
        hb["raw"] = [sq(f"raw{s}_", BF16, 516) for s in range(3)]
        hb["c2"] = [sq("c2q", F32, 512), sq("c2k", F32, 512), sq("c2v", BF16, 512)]
        hb["nT"] = [sq("qnT", BF16, 512), sq("knT", BF16, 512)]
        for nm, dt, w in [("th", F32, 512), ("sq", BF16, 512), ("ssb", F32, 512), ("rn", F32, 512), ("oT", F32, 512),
                          ("zt", BF16, 512), ("od", BF16, 512),
                          ("gB", F32, 128), ("ngB", F32, 128), ("dmf", F32, 128), ("Dm", F32, 128), ("Ds", F32, 128),
                          ("egr", F32, 128), ("qdT", BF16, 128), ("QK", BF16, 128), ("QKT", BF16, 128), ("kb", BF16, 128),
                          ("kdec", BF16, 128), ("vb", BF16, 128), ("usb", F32, 128), ("nwT", BF16, 128), ("vnew", BF16, 128)]:
            hb[nm] = sq(nm, dt, w)
        hb["Pm"] = [sq("Pm0"), sq("Pm1")]
        hb["Qm"] = [sq("Qm0"), sq("Qm1")]
        hb["Xm"] = [sq("Xm0"), sq("Xm1")]
        hbs.append(hb)
    for h0 in range(0, 8, HP):
        run_interleaved([dn_head(self, h0 + p, hbs[p]) for p in range(HP)])
    fw.pop_scope()


MK.p3_setup = p3_setup
MK.load_consts2 = load_consts2
MK.phase3a = phase3a
MK.phase3 = phase3


def p4_setup(self):
    nc = self.nc
    self.w_alpha = nc.dram_tensor("w_alpha", [16, 512], F32, kind="ExternalInput").ap()
    self.nb_alpha = nc.dram_tensor("b_alphaT", [128, 4], F32, kind="ExternalInput").ap()
    self.nh_gla = nc.dram_tensor("nh_glaT", [128, 2], F32, kind="ExternalInput").ap()
    kind = "ExternalOutput" if "og" in self.debug else "Internal"
    self.OG = nc.dram_tensor("og", [1024, self.NT], BF16, kind=kind).ap()


def gla_head(self, h, hb):
    fw, nc = self.fw, self.nc
    SEQ = self.SEQ
    groups = groups_of(self.NSEQ, SEQ)
    pg, pgR = hb["pg"]
    pd, pdR = hb["pd"]
    slA = Slots(pg, pgR)
    slD = Slots(pd, pdR)
    (S32, S32R), (Sb, SbR), (Sm, SmR) = hb["S32"], hb["Sb"], hb["Sm"]
    B = lambda k: hb[k]
    sc = 128 ** -0.5
    for (n0, N) in groups:
        is_meta = n0 == 0
        seq_start = (not is_meta) and (n0 - 128) % SEQ == 0
        nch = N // 64
        ntl = N // 128
        if is_meta:
            fw.pool.op(lambda: nc.gpsimd.memset(S32[:], 0.0), writes=[S32R])
            fw.pool.op(lambda: nc.gpsimd.memset(Sb[:], 0.0), writes=[SbR])
        elif seq_start:
            fw.pool.op(lambda: nc.gpsimd.tensor_copy(out=S32[:], in_=Sm[:]), reads=[SmR], writes=[S32R])
            fw.act.op(lambda: nc.scalar.copy(out=Sb[:], in_=Sm[:]), reads=[SmR], writes=[SbR])
        qt, qtR = B("qt")
        kt, ktR = B("kt")
        lr, lrR = B("lr")
        vt, vtR = B("vt")
        fw.sp.dma(qt[:, :N], self.PF["qg"][h * 128:(h + 1) * 128, n0:n0 + N], qtR, "w")
        fw.sp.dma(kt[:, :N], self.PF["kg"][h * 128:(h + 1) * 128, n0:n0 + N], ktR, "w")
        fw.sp.dma(lr[:, :N], self.PF_LR[:, n0:n0 + N], lrR, "w")
        fw.sp.dma(vt[:, :ntl, :], self.PT_VG[n0:n0 + N, h * 256:(h + 1) * 256].rearrange("(t p) c -> p t c", p=128), vtR, "w")
        fw.pe.op(lambda: nc.tensor.matmul(pg[:, :N], lhsT=self.wal[:, h * 128:(h + 1) * 128], rhs=lr[:, :N], start=True, stop=True),
                 reads=[self.walR, lrR], writes=[pgR])
        yield
        e0, e0R = B("e0")
        cc, ccR = B("cc")
        fw.act.op(lambda: nc.scalar.activation(out=e0[:, :N], in_=pg[:, :N], func=AF.Exp, scale=-1.0, bias=self.nba[:, h:h + 1]),
                  reads=[pgR, self.nbaR], writes=[e0R])
        fw.act.op(lambda: nc.scalar.activation(out=e0[:, :N], in_=e0[:, :N], func=AF.Ln, bias=1.0), reads=[e0R], writes=[e0R])
        fw.dve.op(lambda: nc.vector.tensor_tensor_scan(out=cc[:, :N], data0=self.rmask[:, :N], data1=e0[:, :N], initial=0.0,
                                                       op0=ALU.mult, op1=ALU.add), reads=[self.rmaskR, e0R], writes=[ccR])
        yield
        c3 = cc[:, :N].rearrange("p (c k) -> p c k", k=64)
        d1, d1R = B("d1")
        d13 = d1[:, :N].rearrange("p (c k) -> p c k", k=64)
        ea, eaR = B("ea")
        qg, qgR = B("qg")
        kg, kgR = B("kg")
        qd, qdR = B("qd")
        kd, kdR = B("kd")
        e3, e3R = B("e3")
        fw.dve.op(lambda: nc.vector.tensor_tensor(out=d13, in0=c3, in1=c3[:, :, 31:32].to_broadcast([128, nch, 64]), op=ALU.subtract),
                  reads=[ccR], writes=[d1R])
        fw.act.op(lambda: nc.scalar.activation(out=ea[:, :N], in_=d1[:, :N], func=AF.Exp, scale=-1.0 / 16), reads=[d1R], writes=[eaR])
        fw.dve.op(lambda: nc.vector.scalar_tensor_tensor(out=qg[:, :N], in0=qt[:, :N], scalar=sc, in1=ea[:, :N], op0=ALU.mult, op1=ALU.mult),
                  reads=[qtR, eaR], writes=[qgR])
        fw.act.op(lambda: nc.scalar.activation(out=ea[:, :N], in_=d1[:, :N], func=AF.Exp, scale=1.0 / 16), reads=[d1R], writes=[eaR])
        fw.dve.op(lambda: nc.vector.tensor_tensor(out=kg[:, :N], in0=kt[:, :N], in1=ea[:, :N], op=ALU.mult),
                  reads=[ktR, eaR], writes=[kgR])
        yield
        fw.act.op(lambda: nc.scalar.activation(out=e3[:, :N], in_=cc[:, :N], func=AF.Exp, scale=-1.0 / 16), reads=[ccR], writes=[e3R])
        fw.dve.op(lambda: nc.vector.scalar_tensor_tensor(out=qd[:, :N], in0=qt[:, :N], scalar=sc, in1=e3[:, :N], op0=ALU.mult, op1=ALU.mult),
                  reads=[qtR, e3R], writes=[qdR])
        fw.dve.op(lambda: nc.vector.tensor_tensor(out=d13, in0=c3, in1=c3[:, :, 63:64].to_broadcast([128, nch, 64]), op=ALU.subtract),
                  reads=[ccR], writes=[d1R])
        fw.act.op(lambda: nc.scalar.activation(out=ea[:, :N], in_=d1[:, :N], func=AF.Exp, scale=1.0 / 16), reads=[d1R], writes=[eaR])
        fw.dve.op(lambda: nc.vector.tensor_tensor(out=kd[:, :N], in0=kt[:, :N], in1=ea[:, :N], op=ALU.mult),
                  reads=[ktR, eaR], writes=[kdR])
        yield
        oT = hb["oT"]
        for tl in range(ntl):
            cs = slice(tl * 128, (tl + 1) * 128)
            s_at, s_atR = slD.next()
            fw.pe.op(lambda: nc.tensor.matmul(s_at, lhsT=kg[:, cs], rhs=qg[:, cs], start=True, stop=True),
                     reads=[kgR, qgR], writes=[s_atR])
            t_k, t_kR = slA.next()
            fw.pe.op(lambda: nc.tensor.transpose(out=bf16_view(t_k), in_=kd[:, cs], identity=self.idb[:]),
                     reads=[kdR, self.idbR], writes=[t_kR])
            yield
            am, amR = B("am")
            ktk, ktkR = B("ktk")
            fw.dve.op(lambda: nc.vector.tensor_tensor(out=am[:], in0=s_at, in1=self.tri, op=ALU.mult),
                      reads=[s_atR, self.cstR], writes=[amR])
            fw.act.op(lambda: nc.scalar.copy(out=ktk[:], in_=bf16_view(t_k)), reads=[t_kR], writes=[ktkR])
            yield
            for c in range(2):
                r = slice(c * 64, (c + 1) * 64)
                col = tl * 128 + c * 64
                for hf in range(2):
                    s_o, s_oR = slA.next()
                    fw.pe.op(lambda: nc.tensor.matmul(s_o[:, 0:64], lhsT=Sb[:, hf * 128:(hf + 1) * 128], rhs=qd[:, col:col + 64],
                                                      start=True, stop=False), reads=[SbR, qdR], writes=[s_oR])
                    fw.pe.op(lambda: nc.tensor.matmul(s_o[:, 0:64], lhsT=vt[r, tl, hf * 128:(hf + 1) * 128], rhs=am[r, r],
                                                      start=False, stop=True), reads=[vtR, amR], writes=[s_oR])
                    o_t, o_R = oT[hf]
                    fw.act.op(lambda: nc.scalar.copy(out=o_t[:, col:col + 64], in_=s_o[:, 0:64]), reads=[s_oR], writes=[o_R])
                fw.pe.op(lambda: nc.tensor.matmul(pd[:, 0:256], lhsT=ktk[r, :], rhs=vt[r, tl, :], start=True, stop=True),
                         reads=[ktkR, vtR], writes=[pdR])
                yield
                fw.dve.op(lambda: nc.vector.scalar_tensor_tensor(out=S32[:], in0=S32[:], scalar=e3[:, col + 63:col + 64], in1=pd[:, 0:256],
                                                                 op0=ALU.mult, op1=ALU.add), reads=[S32R, e3R, pdR], writes=[S32R])
                fw.act.op(lambda: nc.scalar.copy(out=Sb[:], in_=S32[:]), reads=[S32R], writes=[SbR])
                yield
        if is_meta:
            fw.pool.op(lambda: nc.gpsimd.tensor_copy(out=Sm[:], in_=S32[:]), reads=[S32R], writes=[SmR])
        sq = hb["sq"]
        for hf in range(2):
            o_t, o_R = oT[hf]
            s_t, s_R = sq[hf]
            fw.act.op(lambda: nc.scalar.activation(out=s_t[:, :N], in_=o_t[:, :N], func=AF.Square), reads=[o_R], writes=[s_R])
            fw.pe.op(lambda: nc.tensor.matmul(pg[:, :N], lhsT=self.onesb[:], rhs=s_t[:, :N], start=(hf == 0), stop=(hf == 1)),
                     reads=[self.onesbR, s_R], writes=[pgR])
        yield
        ssb, ssbR = B("ssb")
        rn, rnR = B("rn")
        fw.act.op(lambda: nc.scalar.activation(out=ssb[:, :N], in_=pg[:, :N], func=AF.Sqrt, scale=1.0 / 256, bias=self.eps1[:, 0:1]),
                  reads=[pgR, self.epsR], writes=[ssbR])
        fw.dve.op(lambda: nc.vector.reciprocal(out=rn[:, :N], in_=ssb[:, :N]), reads=[ssbR], writes=[rnR])
        for hf in range(2):
            o_t, o_R = oT[hf]
            rt, rtR = B("rt")
            th, thR = B("th")
            og, ogR = B("og")
            rows = slice(h * 256 + hf * 128, h * 256 + (hf + 1) * 128)
            fw.sp.dma(rt[:, :N], self.PF["rg"][rows, n0:n0 + N], rtR, "w")
            fw.act.op(lambda: nc.scalar.activation(out=th[:, :N], in_=rt[:, :N], func=AF.Tanh, scale=0.5), reads=[rtR], writes=[thR])
            fw.dve.op(lambda: nc.vector.scalar_tensor_tensor(out=th[:, :N], in0=th[:, :N], scalar=1.0, in1=rt[:, :N], op0=ALU.add, op1=ALU.mult),
                      reads=[thR, rtR], writes=[thR])
            fw.dve.op(lambda: nc.vector.scalar_tensor_tensor(out=ssb[:, :N], in0=o_t[:, :N], scalar=self.nhg[:, hf:hf + 1], in1=rn[:, :N],
                                                             op0=ALU.mult, op1=ALU.mult), reads=[o_R, self.nhgR, rnR], writes=[ssbR])
            fw.dve.op(lambda: nc.vector.tensor_tensor(out=og[:, :N], in0=ssb[:, :N], in1=th[:, :N], op=ALU.mult),
                      reads=[ssbR, thR], writes=[ogR])
            fw.sp.dma(self.OG[rows, n0:n0 + N], og[:, :N], ogR, "r")
            yield


def gla_stream(self, h, hb, seqs, bk):
    fw, nc = self.fw, self.nc
    SEQ = self.SEQ
    groups = [(0, 128)] + [g_ for g_ in groups_of(self.NSEQ, SEQ, with_meta=False, gmax=256) if (g_[0] - 128) // SEQ in seqs]
    (S32, S32R), (Sb, SbR), (Sm, SmR) = hb["S32"], hb["Sb"], hb["Sm"]
    B = lambda k: hb[k]
    sc = 128 ** -0.5
    for (n0, N) in groups:
        is_meta = n0 == 0
        seq_start = (not is_meta) and (n0 - 128) % SEQ == 0
        nch = N // 64
        ntl = N // 128
        if is_meta:
            fw.pool.op(lambda: nc.gpsimd.memset(S32[:], 0.0), writes=[S32R])
            fw.pool.op(lambda: nc.gpsimd.memset(Sb[:], 0.0), writes=[SbR])
        elif seq_start:
            fw.pool.op(lambda: nc.gpsimd.tensor_copy(out=S32[:], in_=Sm[:]), reads=[SmR], writes=[S32R])
            fw.act.op(lambda: nc.scalar.copy(out=Sb[:], in_=Sm[:]), reads=[SmR], writes=[SbR])
        qt, qtR = B("qt")
        kt, ktR = B("kt")
        lr, lrR = B("lr")
        vt, vtR = B("vt")
        fw.sp.dma(qt[:, :N], self.PF["qg"][h * 128:(h + 1) * 128, n0:n0 + N], qtR, "w")
        fw.sp.dma(kt[:, :N], self.PF["kg"][h * 128:(h + 1) * 128, n0:n0 + N], ktR, "w")
        fw.sp.dma(lr[:, :N], self.PF_LR[:, n0:n0 + N], lrR, "w")
        fw.sp.dma(vt[:, :ntl, :], self.PT_VG[n0:n0 + N, h * 256:(h + 1) * 256].rearrange("(t p) c -> p t c", p=128), vtR, "w")
        g1 = []
        yield from acq(bk, 1, g1)
        pg, pgR = g1[0]
        fw.pe.op(lambda: nc.tensor.matmul(pg[:, :N], lhsT=self.wal[:, h * 128:(h + 1) * 128], rhs=lr[:, :N], start=True, stop=True),
                 reads=[self.walR, lrR], writes=[pgR])
        yield
        e0, e0R = B("e0")
        cc, ccR = B("cc")
        fw.act.op(lambda: nc.scalar.activation(out=e0[:, :N], in_=pg[:, :N], func=AF.Exp, scale=-1.0, bias=self.nba[:, h:h + 1]),
                  reads=[pgR, self.nbaR], writes=[e0R])
        bk.release(g1)
        fw.act.op(lambda: nc.scalar.activation(out=e0[:, :N], in_=e0[:, :N], func=AF.Ln, bias=1.0), reads=[e0R], writes=[e0R])
        fw.dve.op(lambda: nc.vector.tensor_tensor_scan(out=cc[:, :N], data0=self.rmask[:, :N], data1=e0[:, :N], initial=0.0,
                                                       op0=ALU.mult, op1=ALU.add), reads=[self.rmaskR, e0R], writes=[ccR])
        yield
        c3 = cc[:, :N].rearrange("p (c k) -> p c k", k=64)
        d1, d1R = B("d1")
        d13 = d1[:, :N].rearrange("p (c k) -> p c k", k=64)
        ea, eaR = B("ea")
        qg, qgR = B("qg")
        kg, kgR = B("kg")
        qd, qdR = B("qd")
        kd, kdR = B("kd")
        e3, e3R = B("e3")
        fw.dve.op(lambda: nc.vector.tensor_tensor(out=d13, in0=c3, in1=c3[:, :, 31:32].to_broadcast([128, nch, 64]), op=ALU.subtract),
                  reads=[ccR], writes=[d1R])
        fw.act.op(lambda: nc.scalar.activation(out=ea[:, :N], in_=d1[:, :N], func=AF.Exp, scale=-1.0 / 16), reads=[d1R], writes=[eaR])
        fw.dve.op(lambda: nc.vector.scalar_tensor_tensor(out=qg[:, :N], in0=qt[:, :N], scalar=sc, in1=ea[:, :N], op0=ALU.mult, op1=ALU.mult),
                  reads=[qtR, eaR], writes=[qgR])
        fw.act.op(lambda: nc.scalar.activation(out=ea[:, :N], in_=d1[:, :N], func=AF.Exp, scale=1.0 / 16), reads=[d1R], writes=[eaR])
        fw.dve.op(lambda: nc.vector.tensor_tensor(out=kg[:, :N], in0=kt[:, :N], in1=ea[:, :N], op=ALU.mult),
                  reads=[ktR, eaR], writes=[kgR])
        yield
        fw.act.op(lambda: nc.scalar.activation(out=e3[:, :N], in_=cc[:, :N], func=AF.Exp, scale=-1.0 / 16), reads=[ccR], writes=[e3R])
        fw.dve.op(lambda: nc.vector.scalar_tensor_tensor(out=qd[:, :N], in0=qt[:, :N], scalar=sc, in1=e3[:, :N], op0=ALU.mult, op1=ALU.mult),
                  reads=[qtR, e3R], writes=[qdR])
        fw.dve.op(lambda: nc.vector.tensor_tensor(out=d13, in0=c3, in1=c3[:, :, 63:64].to_broadcast([128, nch, 64]), op=ALU.subtract),
                  reads=[ccR], writes=[d1R])
        fw.act.op(lambda: nc.scalar.activation(out=ea[:, :N], in_=d1[:, :N], func=AF.Exp, scale=1.0 / 16), reads=[d1R], writes=[eaR])
        fw.dve.op(lambda: nc.vector.tensor_tensor(out=kd[:, :N], in0=kt[:, :N], in1=ea[:, :N], op=ALU.mult),
                  reads=[ktR, eaR], writes=[kdR])
        yield
        oT = hb["oT"]
        for tl in range(ntl):
            cs = slice(tl * 128, (tl + 1) * 128)
            g2 = []
            yield from acq(bk, 2, g2)
            (b_at, s_atR), (b_tk, t_kR) = g2
            s_at, t_k = b_at[:, 0:128], b_tk[:, 0:128]
            fw.pe.op(lambda: nc.tensor.matmul(s_at, lhsT=kg[:, cs], rhs=qg[:, cs], start=True, stop=True),
                     reads=[kgR, qgR], writes=[s_atR])
            fw.pe.op(lambda: nc.tensor.transpose(out=bf16_view(t_k), in_=kd[:, cs], identity=self.idb[:]),
                     reads=[kdR, self.idbR], writes=[t_kR])
            yield
            am, amR = B("am")
            ktk, ktkR = B("ktk")
            fw.dve.op(lambda: nc.vector.tensor_tensor(out=am[:], in0=s_at, in1=self.tri, op=ALU.mult),
                      reads=[s_atR, self.cstR], writes=[amR])
            fw.act.op(lambda: nc.scalar.copy(out=ktk[:], in_=bf16_view(t_k)), reads=[t_kR], writes=[ktkR])
            bk.release(g2)
            yield
            for c in range(2):
                r = slice(c * 64, (c + 1) * 64)
                col = tl * 128 + c * 64
                g3 = []
                yield from acq(bk, 2, g3)
                (b_o, s_oR), (pd, pdR) = g3
                for hf in range(2):
                    s_o = b_o[:, hf * 128:(hf + 1) * 128]
                    fw.pe.op(lambda: nc.tensor.matmul(s_o[:, 0:64], lhsT=Sb[:, hf * 128:(hf + 1) * 128], rhs=qd[:, col:col + 64],
                                                      start=True, stop=False), reads=[SbR, qdR], writes=[s_oR])
                    fw.pe.op(lambda: nc.tensor.matmul(s_o[:, 0:64], lhsT=vt[r, tl, hf * 128:(hf + 1) * 128], rhs=am[r, r],
                                                      start=False, stop=True), reads=[vtR, amR], writes=[s_oR])
                    o_t, o_R = oT[hf]
                    fw.act.op(lambda: nc.scalar.copy(out=o_t[:, col:col + 64], in_=s_o[:, 0:64]), reads=[s_oR], writes=[o_R])
                fw.pe.op(lambda: nc.tensor.matmul(pd[:, 0:256], lhsT=ktk[r, :], rhs=vt[r, tl, :], start=True, stop=True),
                         reads=[ktkR, vtR], writes=[pdR])
                yield
                fw.dve.op(lambda: nc.vector.scalar_tensor_tensor(out=S32[:], in0=S32[:], scalar=e3[:, col + 63:col + 64], in1=pd[:, 0:256],
                                                                 op0=ALU.mult, op1=ALU.add), reads=[S32R, e3R, pdR], writes=[S32R])
                fw.act.op(lambda: nc.scalar.copy(out=Sb[:], in_=S32[:]), reads=[S32R], writes=[SbR])
                bk.release(g3)
                yield
        if is_meta:
            fw.pool.op(lambda: nc.gpsimd.tensor_copy(out=Sm[:], in_=S32[:]), reads=[S32R], writes=[SmR])
        sq = hb["sq"]
        g4 = []
        yield from acq(bk, 1, g4)
        pg, pgR = g4[0]
        for hf in range(2):
            o_t, o_R = oT[hf]
            s_t, s_R = sq[hf]
            fw.act.op(lambda: nc.scalar.activation(out=s_t[:, :N], in_=o_t[:, :N], func=AF.Square), reads=[o_R], writes=[s_R])
            fw.pe.op(lambda: nc.tensor.matmul(pg[:, :N], lhsT=self.onesb[:], rhs=s_t[:, :N], start=(hf == 0), stop=(hf == 1)),
                     reads=[self.onesbR, s_R], writes=[pgR])
        yield
        ssb, ssbR = B("ssb")
        rn, rnR = B("rn")
        fw.act.op(lambda: nc.scalar.activation(out=ssb[:, :N], in_=pg[:, :N], func=AF.Sqrt, scale=1.0 / 256, bias=self.eps1[:, 0:1]),
                  reads=[pgR, self.epsR], writes=[ssbR])
        bk.release(g4)
        fw.dve.op(lambda: nc.vector.reciprocal(out=rn[:, :N], in_=ssb[:, :N]), reads=[ssbR], writes=[rnR])
        for hf in range(2):
            o_t, o_R = oT[hf]
            rt, rtR = B("rt")
            th, thR = B("th")
            og, ogR = B("og")
            rows = slice(h * 256 + hf * 128, h * 256 + (hf + 1) * 128)
            fw.sp.dma(rt[:, :N], self.PF["rg"][rows, n0:n0 + N], rtR, "w")
            fw.act.op(lambda: nc.scalar.activation(out=th[:, :N], in_=rt[:, :N], func=AF.Tanh, scale=0.5), reads=[rtR], writes=[thR])
            fw.dve.op(lambda: nc.vector.scalar_tensor_tensor(out=th[:, :N], in0=th[:, :N], scalar=1.0, in1=rt[:, :N], op0=ALU.add, op1=ALU.mult),
                      reads=[thR, rtR], writes=[thR])
            fw.dve.op(lambda: nc.vector.scalar_tensor_tensor(out=ssb[:, :N], in0=o_t[:, :N], scalar=self.nhg[:, hf:hf + 1], in1=rn[:, :N],
                                                             op0=ALU.mult, op1=ALU.mult), reads=[o_R, self.nhgR, rnR], writes=[ssbR])
            fw.dve.op(lambda: nc.vector.tensor_tensor(out=og[:, :N], in0=ssb[:, :N], in1=th[:, :N], op=ALU.mult),
                      reads=[ssbR, thR], writes=[ogR])
            fw.sp.dma(self.OG[rows, n0:n0 + N], og[:, :N], ogR, "r")
            yield


def phase4(self):
    fw, nc = self.fw, self.nc
    fw.push_scope()
    self.wal, self.walR = fw.sbuf("wal", [16, 512], F32)
    fw.sp.dma(self.wal[:], self.w_alpha, self.walR, "w")
    self.nba, self.nbaR = fw.sbuf("nba", [128, 4], F32)
    fw.sp.dma(self.nba[:], self.nb_alpha, self.nbaR, "w")
    fw.dve.op(lambda: nc.vector.tensor_scalar(out=self.nba[:], in0=self.nba[:], scalar1=-1.0, scalar2=None, op0=ALU.mult),
              reads=[self.nbaR], writes=[self.nbaR])
    self.nhg, self.nhgR = fw.sbuf("nhg", [128, 2], F32)
    fw.sp.dma(self.nhg[:], self.nh_gla, self.nhgR, "w")
    fw.dve.op(lambda: nc.vector.tensor_scalar(out=self.nhg[:], in0=self.nhg[:], scalar1=0.5, scalar2=None, op0=ALU.mult),
              reads=[self.nhgR], writes=[self.nhgR])
    self.rmask, self.rmaskR = fw.sbuf("rmask", [128, 512], F32)
    fw.pool.op(lambda: nc.gpsimd.memset(self.rmask[:], 1.0), writes=[self.rmaskR])
    fw.pool.op(lambda: nc.gpsimd.memset(self.rmask[:].rearrange("p (c k) -> p c k", k=64)[:, :, 0:1], 0.0), writes=[self.rmaskR])
    hbs = []
    for p in range(4):
        hb = {}
        hb["pg"] = fw.psum(f"gpg{p}", [128, 512], F32)
        hb["pd"] = fw.psum(f"gpd{p}", [128, 512], F32)
        sq = lambda nm, dt=BF16, w=512: fw.sbuf(f"g{nm}{p}", [128, w], dt)
        hb["S32"], hb["Sb"], hb["Sm"] = sq("S32", F32, 256), sq("Sb", BF16, 256), sq("Sm", F32, 256)
        hb["qt"], hb["kt"] = sq("qt"), sq("kt")
        hb["lr"] = fw.sbuf(f"glr{p}", [16, 512], F32)
        hb["vt"] = fw.sbuf(f"gvt{p}", [128, 4, 256], BF16)
        for nm in ("e0", "cc", "d1", "ea", "e3", "ssb", "rn", "th"):
            hb[nm] = sq(nm, F32)
        for nm in ("qg", "kg", "qd", "kd", "rt", "og"):
            hb[nm] = sq(nm)
        hb["am"], hb["ktk"] = sq("am", BF16, 128), sq("ktk", BF16, 128)
        hb["oT"] = [sq("oT0", F32), sq("oT1", F32)]
        hb["sq"] = [sq("sq0"), sq("sq1")]
        hbs.append(hb)
    run_interleaved([gla_head(self, p, hbs[p]) for p in range(4)])
    fw.pop_scope()


MK.p4_setup = p4_setup
MK.phase4 = phase4


def p5_setup(self):
    nc = self.nc
    T, CAP = self.T, self.CAP
    inp = lambda nm, shp: nc.dram_tensor(nm, list(shp), F32, kind="ExternalInput").ap()
    self.w_pd = inp("w_proj_dn", [D, D])
    self.w_pg = inp("w_proj_gla", [D, D])
    self.w_o = inp("w_out", [D, D])
    self.g_ffn = inp("g_ffn", [1, D])
    self.g_fin = inp("g_fin", [1, D])
    self.w_rt = inp("w_rt", [D, 72])
    self.b_rt = inp("b_rt", [1, 72])
    self.ebase = inp("ebase", [1, 64])
    self.ustr = inp("ustr", [128, 128])
    self.w1 = inp("w1", [N_EXP, D, 512])
    self.w3 = inp("w3", [N_EXP, D, 512])
    self.w2 = inp("w2", [N_EXP, 512, D])
    scr = lambda nm, shp, dt: nc.dram_tensor(nm, list(shp), dt, kind=("ExternalOutput" if nm in self.debug else "Internal")).ap()
    self.H1 = scr("h1", [T, D], F32)
    self.XS = scr("xs", [N_EXP * CAP, D], BF16)
    self.YS = scr("ys", [N_EXP * CAP, D], F32)
    self.DBG = scr("dbg", [T, 8], F32)


def phase5(self):
    fw, nc = self.fw, self.nc
    T, CAP, NT = self.T, self.CAP, self.NT
    TT = T // 128
    self.SLOT, self.SLOTR = fw.sbuf("SLOT", [128, TT, 2], I32)
    self.bc_reg = nc.gpsimd.to_reg(N_EXP * CAP - 1)
    self.GATE, self.GATER = fw.sbuf("GATE", [128, TT, 2], F32)
    fw.push_scope()
    wv = lambda w: w.rearrange("(kc p) c -> p kc c", p=128)
    wpd, wpdR = fw.sbuf("wpd", [128, 8, D], BF16)
    wpg, wpgR = fw.sbuf("wpg", [128, 8, D], BF16)
    wo, woR = fw.sbuf("wo", [128, 8, D], BF16)
    for t_, R_, w_ in ((wpd, wpdR, self.w_pd), (wpg, wpgR, self.w_pg), (wo, woR, self.w_o)):
        for kc in range(8):
            fw.pool.dma(t_[:, kc, :], wv(w_)[:, kc, :], R_, "w", part=True)
    wr, wrR = fw.sbuf("wr", [128, 8, 72], F32)
    fw.sp.dma(wr[:], wv(self.w_rt), wrR, "w")
    br, brR = fw.sbuf("br", [128, 72], F32)
    fw.sp.dma(br[:], self.b_rt.partition_broadcast(128), brR, "w")
    gf, gfR = fw.sbuf("gf", [128, D], F32)
    fw.sp.dma(gf[:], self.g_ffn.partition_broadcast(128), gfR, "w")
    eb, ebR = fw.sbuf("eb", [128, 64], F32)
    fw.sp.dma(eb[:], self.ebase.partition_broadcast(128), ebR, "w")
    us, usR = fw.sbuf("us", [128, 128], BF16)
    fw.pool.dma(us[:], self.ustr, usR, "w")
    zt, ztR = fw.sbuf("zt5", [128, 2048], BF16)
    fw.pool.op(lambda: nc.gpsimd.memset(zt[:], 0.0), writes=[ztR])
    nrows = N_EXP * CAP
    xsR = fw.res("xs_dram")
    zfR = fw.res("xs_zero")
    r0 = 0
    while r0 < nrows:
        nr = min(256, nrows - r0)
        fw.sp.dma(self.XS[r0:r0 + nr, :].rearrange("(p a) c -> p (a c)", p=128), zt[:, :(nr // 128) * D], ztR, "r", part=True,
                  extra_reads=[zfR])
        r0 += nr
    Csum, CsumR = fw.sbuf("Csum", [128, 64], F32)
    Csb, CsbR = fw.sbuf("Csb", [128, 64], BF16)
    fw.pool.op(lambda: nc.gpsimd.memset(Csum[:], 0.0), writes=[CsumR])
    fw.pool.op(lambda: nc.gpsimd.memset(Csb[:], 0.0), writes=[CsbR])
    odb = [fw.sbuf(f"odb{i}", [128, 8, 512], BF16) for i in range(2)]
    ogb = [fw.sbuf(f"ogb{i}", [128, 8, 512], BF16) for i in range(2)]
    mg, mgR = fw.sbuf("mg", [128, 8, 512], BF16)
    gdb = [fw.sbuf(f"gdb{i}", [128, 512], BF16) for i in range(2)]
    ggb = [fw.sbuf(f"ggb{i}", [128, 512], BF16) for i in range(2)]
    sgd = [fw.sbuf(f"sgd{i}", [128, 512], F32) for i in range(1)] * 2
    sgg = [fw.sbuf(f"sgg{i}", [128, 512], F32) for i in range(1)] * 2
    m1b = [fw.sbuf(f"m1b{i}", [128, 512], F32) for i in range(1)] * 2
    py = [fw.psum(f"py{i}", [128, 512], F32) for i in range(4)]
    pm = [fw.psum(f"pm{i}", [128, 512], F32) for i in range(2)]
    ptr, ptrR = fw.psum("ptr", [128, 4, 128], F32)
    pl, plR = fw.psum("pl", [128, 512], F32)
    xb = [fw.sbuf(f"x5b{i}", [128, D], F32) for i in range(4)]
    h1b = [fw.sbuf(f"h1b{i}", [128, D], F32) for i in range(4)]
    hnb = [fw.sbuf(f"hn5b{i}", [128, D], F32) for i in range(4)]
    hbb = [fw.sbuf(f"hb5b{i}", [128, D], BF16) for i in range(4)]
    SB = []
    NP5 = 4
    for par_ in range(NP5):
        small = lambda nm, w, dt=F32: fw.sbuf(f"{nm}_{par_}", [128, w], dt)
        d_ = {}
        for nm, w, dt in [("ss", 1, F32), ("lg", 72, F32), ("m8", 8, F32), ("ng", 1, F32), ("goh", 8, F32), ("eg", 8, F32), ("se", 1, F32),
                          ("msk", 64, F32), ("ein", 8, F32), ("e8", 8, F32), ("oh", 16, F32), ("dv", 4, F32), ("A12", 128, F32),
                          ("Cb", 64, BF16), ("pos", 64, F32), ("val", 64, F32), ("tmp", 64, F32), ("sl", 2, F32)]:
            d_[nm] = small("s5" + nm, w, dt)
        d_["junk"] = SB[0]["junk"] if SB else fw.sbuf("junk5_s", [128, D], BF16)
        d_["hT"] = fw.sbuf(f"hT5_{par_}", [128, 8, 128], F32)
        SB.append(d_)
    cntb = [0]
    prog = {"g1": 0, "g2": [0] * 4}
    cprog = [0]
    grp = groups_of(self.NSEQ, self.SEQ, with_meta=False)
    mgb = [(mg, mgR), fw.sbuf("mg2", [128, 8, 512], BF16)]

    def g1():
        for gi, (n0, N) in enumerate(grp):
            while min(prog["g2"]) < gi - 1:
                yield
            od, odR = odb[gi % 2]
            og, ogR = ogb[gi % 2]
            fw.sp.dma(od[:, :, :N], self.OD.rearrange("(kc p) n -> p kc n", p=128)[:, :, n0:n0 + N], odR, "w")
            fw.sp.dma(og[:, :, :N], self.OG.rearrange("(kc p) n -> p kc n", p=128)[:, :, n0:n0 + N], ogR, "w")
            for cc in range(8):
                cs = slice(cc * 128, (cc + 1) * 128)
                gd, gdR = gdb[cc % 2]
                gg, ggR = ggb[cc % 2]
                sd, sdR = sgd[cc % 2]
                sg, sgR = sgg[cc % 2]
                m1, m1R = m1b[cc % 2]
                fw.sp.dma(gd[:, :N], self.PF["gd"][cs, n0:n0 + N], gdR, "w")
                fw.sp.dma(gg[:, :N], self.PF["gg"][cs, n0:n0 + N], ggR, "w")
                pyd, pydR = py[cntb[0] % 4]
                pyg, pygR = py[(cntb[0] + 1) % 4]
                cntb[0] += 2
                for kc in range(8):
                    fw.pe.op(lambda: nc.tensor.matmul(pyd[:, :N], lhsT=wpd[:, kc, cs], rhs=od[:, kc, :N], start=(kc == 0), stop=(kc == 7)),
                             reads=[wpdR, odR], writes=[pydR])
                for kc in range(8):
                    fw.pe.op(lambda: nc.tensor.matmul(pyg[:, :N], lhsT=wpg[:, kc, cs], rhs=og[:, kc, :N], start=(kc == 0), stop=(kc == 7)),
                             reads=[wpgR, ogR], writes=[pygR])
                fw.act.op(lambda: nc.scalar.activation(out=sd[:, :N], in_=gd[:, :N], func=AF.Tanh, scale=0.5), reads=[gdR], writes=[sdR])
                fw.act.op(lambda: nc.scalar.activation(out=sg[:, :N], in_=gg[:, :N], func=AF.Tanh, scale=0.5), reads=[ggR], writes=[sgR])
                fw.dve.op(lambda: nc.vector.scalar_tensor_tensor(out=m1[:, :N], in0=sd[:, :N], scalar=1.0, in1=pyd[:, :N], op0=ALU.add, op1=ALU.mult),
                          reads=[pydR, sdR], writes=[m1R])
                fw.dve.op(lambda: nc.vector.scalar_tensor_tensor(out=sg[:, :N], in0=sg[:, :N], scalar=1.0, in1=pyg[:, :N], op0=ALU.add, op1=ALU.mult),
                          reads=[pygR, sgR], writes=[sgR])
                fw.dve.op(lambda: nc.vector.tensor_tensor(out=mgb[gi % 2][0][:, cc, :N], in0=m1[:, :N], in1=sg[:, :N], op=ALU.add),
                          reads=[m1R, sgR], writes=[mgb[gi % 2][1]])
                yield

            prog["g1"] = gi + 1
            yield

    def g2(par):
        d_ = SB[par]
        (ss, ssR), (lg, lgR), (m8, m8R), (ng, ngR), (goh, gohR), (eg, egR), (se, seR) = [d_[n] for n in ("ss", "lg", "m8", "ng", "goh", "eg", "se")]
        (msk, mskR), (ein, einR), (e8, e8R), (oh, ohR), (dv, dvR), (A12, A12R) = [d_[n] for n in ("msk", "ein", "e8", "oh", "dv", "A12")]
        (Cb, CbR), (pos, posR), (val, valR), (tmp, tmpR), (sl, slR) = [d_[n] for n in ("Cb", "pos", "val", "tmp", "sl")]
        junk, junkR = d_["junk"]
        hT, hTR = d_["hT"]
        for gi, (n0, N) in enumerate(grp):
            while prog["g1"] < gi + 1:
                yield
            for tl in range(par, N // 128, 4):
                tt = (n0 - 128) // 128 + tl
                ts_ = slice(tl * 128, (tl + 1) * 128)
                xt, xR = xb[tt % 4]
                h1, h1R = h1b[tt % 4]
                hn, hnR = hnb[tt % 4]
                hb_, hbR = hbb[tt % 4]
                fw.sp.dma(xt[:], self.x[tt * 128:(tt + 1) * 128, :], xR, "w")
                for hf in range(2):
                    pmm, pmR = pm[hf]
                    for kc in range(8):
                        fw.pe.op(lambda: nc.tensor.matmul(pmm[:], lhsT=mgb[gi % 2][0][:, kc, ts_], rhs=wo[:, kc, hf * 512:(hf + 1) * 512],
                                                          start=(kc == 0), stop=(kc == 7)), reads=[mgb[gi % 2][1], woR], writes=[pmR])
                    fw.dve.op(lambda: nc.vector.scalar_tensor_tensor(out=h1[:, hf * 512:(hf + 1) * 512], in0=pmm[:], scalar=0.5,
                                                                     in1=xt[:, hf * 512:(hf + 1) * 512], op0=ALU.mult, op1=ALU.add),
                              reads=[pmR, xR], writes=[h1R])
                fw.sp.dma(self.H1[tt * 128:(tt + 1) * 128, :], h1[:], h1R, "r")
                yield
                fw.act.op(lambda: nc.scalar.activation(out=junk[:], in_=h1[:], func=AF.Square, accum_out=ss[:]), reads=[h1R], writes=[junkR, ssR])
                fw.dve.op(lambda: nc.vector.tensor_scalar(out=ss[:], in0=ss[:], scalar1=1.0 / D, scalar2=EPS, op0=ALU.mult, op1=ALU.add),
                          reads=[ssR], writes=[ssR])
                fw.act.op(lambda: nc.scalar.sqrt(out=ss[:], in_=ss[:]), reads=[ssR], writes=[ssR])
                fw.dve.op(lambda: nc.vector.reciprocal(out=ss[:], in_=ss[:]), reads=[ssR], writes=[ssR])
                fw.dve.op(lambda: nc.vector.scalar_tensor_tensor(out=hn[:], in0=h1[:], scalar=ss[:], in1=gf[:], op0=ALU.mult, op1=ALU.mult),
                          reads=[h1R, ssR, gfR], writes=[hnR])
                fw.act.op(lambda: nc.scalar.copy(out=hb_[:], in_=hn[:]), reads=[hnR], writes=[hbR])
                for q4 in range(2):
                    for k4 in range(4):
                        kc = q4 * 4 + k4
                        fw.pe.op(lambda: nc.tensor.transpose(out=ptr[:, k4, :], in_=hn[:, kc * 128:(kc + 1) * 128], identity=self.idf[:]),
                                 reads=[hnR, self.idfR], writes=[ptrR])
                    fw.act.op(lambda: nc.scalar.copy(out=hT[:, q4 * 4:(q4 + 1) * 4, :], in_=ptr[:]), reads=[ptrR], writes=[hTR])
                for kc in range(8):
                    fw.pe.op(lambda: nc.tensor.matmul(pl[:, 0:72], lhsT=hT[:, kc, :], rhs=wr[:, kc, :], start=(kc == 0), stop=(kc == 7)),
                             reads=[hTR, wrR], writes=[plR])
                fw.dve.op(lambda: nc.vector.tensor_tensor(out=lg[:], in0=pl[:, 0:72], in1=br[:], op=ALU.add), reads=[plR, brR], writes=[lgR])
                yield
                fw.dve.op(lambda: nc.vector.max(out=m8[:], in_=lg[:, 0:8]), reads=[lgR], writes=[m8R])
                fw.dve.op(lambda: nc.vector.tensor_scalar(out=goh[:], in0=lg[:, 0:8], scalar1=m8[:, 0:1], scalar2=None, op0=ALU.is_equal),
                          reads=[lgR, m8R], writes=[gohR])
                fw.dve.op(lambda: nc.vector.tensor_scalar(out=ng[:], in0=m8[:, 0:1], scalar1=-1.0, scalar2=None, op0=ALU.mult),
                          reads=[m8R], writes=[ngR])
                fw.act.op(lambda: nc.scalar.activation(out=eg[:], in_=lg[:, 0:8], func=AF.Exp, bias=ng[:], accum_out=se[:]),
                          reads=[lgR, ngR], writes=[egR, seR])
                fw.dve.op(lambda: nc.vector.reciprocal(out=se[:], in_=se[:]), reads=[seR], writes=[seR])
                fw.dve.op(lambda: nc.vector.tensor_tensor(out=msk[:].rearrange("p (g e) -> p g e", e=8),
                                                          in0=lg[:, 8:72].rearrange("p (g e) -> p g e", e=8),
                                                          in1=goh[:].unsqueeze(2).to_broadcast([128, 8, 8]), op=ALU.mult),
                          reads=[lgR, gohR], writes=[mskR])
                fw.dve.op(lambda: nc.vector.tensor_reduce(out=ein[:], in_=msk[:].rearrange("p (g e) -> p e g", e=8), axis=AX.X, op=ALU.add),
                          reads=[mskR], writes=[einR])
                fw.dve.op(lambda: nc.vector.max(out=e8[:], in_=ein[:]), reads=[einR], writes=[e8R])
                yield
                for k in range(2):
                    fw.dve.op(lambda: nc.vector.tensor_scalar(out=oh[:, k * 8:(k + 1) * 8], in0=ein[:], scalar1=e8[:, k:k + 1], scalar2=None,
                                                              op0=ALU.is_equal), reads=[einR, e8R], writes=[ohR])
                fw.dve.op(lambda: nc.vector.tensor_tensor(out=dv[:, 0:1], in0=e8[:, 1:2], in1=e8[:, 0:1], op=ALU.subtract),
                          reads=[e8R], writes=[dvR])
                fw.act.op(lambda: nc.scalar.activation(out=dv[:, 1:2], in_=dv[:, 0:1], func=AF.Exp), reads=[dvR], writes=[dvR])
                fw.dve.op(lambda: nc.vector.tensor_scalar(out=dv[:, 2:3], in0=dv[:, 1:2], scalar1=1.0, scalar2=None, op0=ALU.add),
                          reads=[dvR], writes=[dvR])
                fw.dve.op(lambda: nc.vector.reciprocal(out=dv[:, 2:3], in_=dv[:, 2:3]), reads=[dvR], writes=[dvR])
                fw.dve.op(lambda: nc.vector.tensor_tensor(out=dv[:, 3:4], in0=dv[:, 1:2], in1=dv[:, 2:3], op=ALU.mult),
                          reads=[dvR], writes=[dvR])
                fw.dve.op(lambda: nc.vector.tensor_scalar(out=self.GATE[:, tt, :], in0=dv[:, 2:4], scalar1=se[:, 0:1], scalar2=None, op0=ALU.mult),
                          reads=[dvR, seR], writes=[self.GATER])
                for k in range(2):
                    fw.dve.op(lambda: nc.vector.tensor_tensor(out=A12[:, k * 64:(k + 1) * 64].rearrange("p (g e) -> p g e", e=8),
                                                              in0=goh[:].unsqueeze(2).to_broadcast([128, 8, 8]),
                                                              in1=oh[:, k * 8:(k + 1) * 8].unsqueeze(1).to_broadcast([128, 8, 8]), op=ALU.mult),
                              reads=[gohR, ohR], writes=[A12R])
                fw.dve.op(lambda: nc.vector.tensor_tensor(out=Cb[:], in0=A12[:, 0:64], in1=A12[:, 64:128], op=ALU.add), reads=[A12R], writes=[CbR])
                yield
                while cprog[0] < tt:
                    yield
                fw.pe.op(lambda: nc.tensor.matmul(pl[:, 128:192], lhsT=us[:], rhs=Cb[:], start=True, stop=False), reads=[usR, CbR], writes=[plR])
                fw.pe.op(lambda: nc.tensor.matmul(pl[:, 128:192], lhsT=self.onesb[:], rhs=Csb[:], start=False, stop=True),
                         reads=[self.onesbR, CsbR], writes=[plR])
                fw.dve.op(lambda: nc.vector.tensor_tensor(out=pos[:], in0=pl[:, 128:192], in1=eb[:], op=ALU.add), reads=[plR, ebR], writes=[posR])
                fw.dve.op(lambda: nc.vector.tensor_scalar(out=val[:], in0=pl[:, 128:192], scalar1=float(CAP), scalar2=1.0e7, op0=ALU.is_ge, op1=ALU.mult),
                          reads=[plR], writes=[valR])
                fw.dve.op(lambda: nc.vector.tensor_tensor(out=pos[:], in0=pos[:], in1=val[:], op=ALU.add), reads=[posR, valR], writes=[posR])
                fw.dve.op(lambda: nc.vector.tensor_tensor(out=Csum[:], in0=Csum[:], in1=Cb[:], op=ALU.add), reads=[CsumR, CbR], writes=[CsumR])
                fw.act.op(lambda: nc.scalar.copy(out=Csb[:], in_=Csum[:]), reads=[CsumR], writes=[CsbR])
                cprog[0] = tt + 1
                for k in range(2):
                    fw.dve.op(lambda: nc.vector.tensor_tensor(out=tmp[:], in0=A12[:, k * 64:(k + 1) * 64], in1=pos[:], op=ALU.mult),
                              reads=[A12R, posR], writes=[tmpR])
                    fw.dve.op(lambda: nc.vector.reduce_sum(out=sl[:, k:k + 1], in_=tmp[:], axis=AX.X), reads=[tmpR], writes=[slR])
                fw.dve.op(lambda: nc.vector.tensor_copy(out=self.SLOT[:, tt, :], in_=sl[:]), reads=[slR], writes=[self.SLOTR])
                fw.dve.op(lambda: nc.vector.tensor_scalar(out=sl[:], in0=sl[:], scalar1=float(N_EXP * CAP), scalar2=None, op0=ALU.is_lt),
                          reads=[slR], writes=[slR])
                fw.dve.op(lambda: nc.vector.tensor_tensor(out=self.GATE[:, tt, :], in0=self.GATE[:, tt, :], in1=sl[:], op=ALU.mult),
                          reads=[self.GATER, slR], writes=[self.GATER])
                if tt == 0:
                    fw.pool.wait_dma(ztR, tokens=[zfR, xsR])
                for k in range(2):
                    fw.pool.indirect_dma(hbR, "r", extra_reads=[self.SLOTR, xsR],
                                         out=self.XS[:, :], out_offset=bass.IndirectOffsetOnAxis(ap=self.SLOT[:, tt, k:k + 1], axis=0),
                                         in_=hb_[:, :], in_offset=None, bounds_check=self.bc_reg, oob_is_err=False)
                if "dbg" in self.debug:
                    dbt, dbtR = fw.sbuf(f"dbt{tt}", [128, 8], F32)
                    fw.dve.op(lambda: nc.vector.tensor_copy(out=dbt[:, 0:2], in_=sl[:]), reads=[slR], writes=[dbtR])
                    fw.dve.op(lambda: nc.vector.tensor_copy(out=dbt[:, 2:4], in_=self.GATE[:, tt, :]), reads=[self.GATER], writes=[dbtR])
                    fw.dve.op(lambda: nc.vector.tensor_copy(out=dbt[:, 4:8], in_=lg[:, 0:4]), reads=[lgR], writes=[dbtR])
                    fw.sp.dma(self.DBG[tt * 128:(tt + 1) * 128, :], dbt[:], dbtR, "r")

            prog["g2"][par] = gi + 1
            yield

    run_interleaved([g1()] + [g2(p_) for p_ in range(4)])
    fw.pop_scope()


def phase6(self):
    fw, nc = self.fw, self.nc
    CAP = self.CAP
    fw.push_scope()
    NWB = 3
    w1b = [fw.sbuf(f"w1b{i}", [128, 8, 512], BF16) for i in range(NWB)]
    w3b = [fw.sbuf(f"w3b{i}", [128, 8, 512], BF16) for i in range(NWB)]
    w2b = [fw.sbuf(f"w2b{i}", [128, 4, D], BF16) for i in range(NWB)]
    xsb = [fw.sbuf(f"xsb{i}", [128, D], BF16) for i in range(3)]
    xT, xTR = fw.sbuf("xT6", [128, 8, CAP], BF16)
    hid, hidR = fw.sbuf("hid", [128, 4, CAP], BF16)
    thb = [fw.sbuf(f"th6{i}", [128, CAP], F32) for i in range(2)]
    yb = [fw.sbuf(f"yb{i}", [128, D], F32) for i in range(2)]
    ptb = [fw.psum(f"pt6{i}", [128, 8, 128], BF16) for i in range(2)]
    ph = [fw.psum(f"ph{i}", [128, 512], F32) for i in range(4)]
    pyy = [fw.psum(f"py6{i}", [128, 512], F32) for i in range(2)]
    nb = CAP // 128
    xi = 0
    yi = 0
    si = 0
    for e in range(N_EXP):
        w1t, w1R = w1b[e % NWB]
        w3t, w3R = w3b[e % NWB]
        w2t, w2R = w2b[e % NWB]
        for kc in range(0, 8, 2):
            fw.pool.dma(w1t[:, kc:kc + 2, :], self.w1[e].rearrange("(kc p) f -> p kc f", p=128)[:, kc:kc + 2, :], w1R, "w", part=(kc > 0))
        for kc in range(0, 8, 2):
            fw.pool.dma(w3t[:, kc:kc + 2, :], self.w3[e].rearrange("(kc p) f -> p kc f", p=128)[:, kc:kc + 2, :], w3R, "w", part=(kc > 0))
        for fc in range(4):
            fw.pool.dma(w2t[:, fc, :], self.w2[e].rearrange("(fc p) d -> p fc d", p=128)[:, fc, :], w2R, "w", part=(fc > 0))
        for b in range(nb):
            xs, xsR = xsb[xi % 3]
            pt, ptR = ptb[xi % 2]
            xi += 1
            fw.sp.dma(xs[:], self.XS[e * CAP + b * 128:e * CAP + (b + 1) * 128, :], xsR, "w")
            for kc in range(8):
                fw.pe.op(lambda: nc.tensor.transpose(out=pt[:, kc, :], in_=xs[:, kc * 128:(kc + 1) * 128], identity=self.idb[:]),
                         reads=[xsR, self.idbR], writes=[ptR])
            fw.act.op(lambda: nc.scalar.copy(out=xT[:, :, b * 128:(b + 1) * 128], in_=pt[:]), reads=[ptR], writes=[xTR])
        for fc in range(4):
            fs = slice(fc * 128, (fc + 1) * 128)
            p1, p1R = ph[(2 * fc) % 4]
            p3, p3R = ph[(2 * fc + 1) % 4]
            for kc in range(8):
                fw.pe.op(lambda: nc.tensor.matmul(p1[:, :CAP], lhsT=w1t[:, kc, fs], rhs=xT[:, kc, :], start=(kc == 0), stop=(kc == 7)),
                         reads=[w1R, xTR], writes=[p1R])
            for kc in range(8):
                fw.pe.op(lambda: nc.tensor.matmul(p3[:, :CAP], lhsT=w3t[:, kc, fs], rhs=xT[:, kc, :], start=(kc == 0), stop=(kc == 7)),
                         reads=[w3R, xTR], writes=[p3R])
            th, thR = thb[fc % 2]
            fw.act.op(lambda: nc.scalar.activation(out=th[:], in_=p1[:, :CAP], func=AF.Tanh, scale=0.5), reads=[p1R], writes=[thR])
            fw.dve.op(lambda: nc.vector.scalar_tensor_tensor(out=th[:], in0=th[:], scalar=1.0, in1=p1[:, :CAP], op0=ALU.add, op1=ALU.mult),
                      reads=[thR, p1R], writes=[thR])
            fw.dve.op(lambda: nc.vector.tensor_tensor(out=hid[:, fc, :], in0=th[:], in1=p3[:, :CAP], op=ALU.mult),
                      reads=[thR, p3R], writes=[hidR])
        for b in range(nb):
            y, yR = yb[yi % 2]
            yi += 1
            for hf in range(2):
                pp, ppR = pyy[hf]
                for fc in range(4):
                    fw.pe.op(lambda: nc.tensor.matmul(pp[:], lhsT=hid[:, fc, b * 128:(b + 1) * 128], rhs=w2t[:, fc, hf * 512:(hf + 1) * 512],
                                                      start=(fc == 0), stop=(fc == 3)), reads=[hidR, w2R], writes=[ppR])
                fw.act.op(lambda: nc.scalar.mul(out=y[:, hf * 512:(hf + 1) * 512], in_=pp[:], mul=0.5), reads=[ppR], writes=[yR])
            fw.sp.dma(self.YS[e * CAP + b * 128:e * CAP + (b + 1) * 128, :], y[:], yR, "r")
    fw.pop_scope()


def phase7(self):
    fw, nc = self.fw, self.nc
    T, CAP = self.T, self.CAP
    fw.push_scope()
    gn, gnR = fw.sbuf("gn", [128, D], F32)
    fw.sp.dma(gn[:], self.g_fin.partition_broadcast(128), gnR, "w")
    h1b = [fw.sbuf(f"h7b{i}", [128, D], F32) for i in range(4)]
    y1b = [fw.sbuf(f"y1b{i}", [128, D], F32) for i in range(4)]
    y2b = [fw.sbuf(f"y2b{i}", [128, D], F32) for i in range(4)]
    ob = [fw.sbuf(f"ob{i}", [128, D], F32) for i in range(4)]
    junk, junkR = fw.sbuf("junk7", [128, D], BF16)
    ssb = [fw.sbuf(f"ss7{i}", [128, 1], F32) for i in range(4)]
    for tt in range(T // 128):
        h1, h1R = h1b[tt % 4]
        y1, y1R = y1b[tt % 4]
        y2, y2R = y2b[tt % 4]
        o, oR = ob[tt % 4]
        ss, ssR = ssb[tt % 4]
        fw.sp.dma(h1[:], self.H1[tt * 128:(tt + 1) * 128, :], h1R, "w")
        for k, (yt, yR) in enumerate(((y1, y1R), (y2, y2R))):
            if tt < 4:
                fw.pool.op(lambda: nc.gpsimd.memset(yt[:], 0.0), writes=[yR])
            fw.pool.indirect_dma(yR, "w", extra_reads=[self.SLOTR],
                                 out=yt[:, :], out_offset=None, in_=self.YS[:, :],
                                 in_offset=bass.IndirectOffsetOnAxis(ap=self.SLOT[:, tt, k:k + 1], axis=0),
                                 bounds_check=self.bc_reg, oob_is_err=False)
        fw.dve.op(lambda: nc.vector.scalar_tensor_tensor(out=h1[:], in0=y1[:], scalar=self.GATE[:, tt, 0:1], in1=h1[:], op0=ALU.mult, op1=ALU.add),
                  reads=[y1R, self.GATER, h1R], writes=[h1R])
        fw.dve.op(lambda: nc.vector.scalar_tensor_tensor(out=h1[:], in0=y2[:], scalar=self.GATE[:, tt, 1:2], in1=h1[:], op0=ALU.mult, op1=ALU.add),
                  reads=[y2R, self.GATER, h1R], writes=[h1R])
        fw.act.op(lambda: nc.scalar.activation(out=junk[:], in_=h1[:], func=AF.Square, accum_out=ss[:]), reads=[h1R], writes=[junkR, ssR])
        fw.dve.op(lambda: nc.vector.tensor_scalar(out=ss[:], in0=ss[:], scalar1=1.0 / D, scalar2=EPS, op0=ALU.mult, op1=ALU.add),
                  reads=[ssR], writes=[ssR])
        fw.act.op(lambda: nc.scalar.sqrt(out=ss[:], in_=ss[:]), reads=[ssR], writes=[ssR])
        fw.dve.op(lambda: nc.vector.reciprocal(out=ss[:], in_=ss[:]), reads=[ssR], writes=[ssR])
        fw.dve.op(lambda: nc.vector.scalar_tensor_tensor(out=o[:], in0=h1[:], scalar=ss[:], in1=gn[:], op0=ALU.mult, op1=ALU.mult),
                  reads=[h1R, ssR, gnR], writes=[oR])
        fw.sp.dma(self.out[tt * 128:(tt + 1) * 128, :], o[:], oR, "r")
    fw.pop_scope()


def build_all(self):
    fw = self.fw
    self.p3_setup(); self.p4_setup(); self.p5_setup()
    self.load_consts(); self.load_consts2()
    self.marks = []
    mark = lambda nm: self.marks.append((nm, fw.pe.n, fw.act.n, fw.dve.n, getattr(fw, "sim_time", 0.0)))
    fw.push_scope(); self.phase1(); fw.flush(); mark("p1")
    fw.sched = False; self.phase2(); fw.flush(); fw.sched = True; mark("p2"); fw.pop_scope()
    fw.push_scope(); self.phase3a(); self.phase3a_extra(); mark("p3a"); self.phase3c(); mark("p3"); fw.pop_scope()
    self.phase4(); mark("p4")
    self.phase5(); mark("p5")
    self.phase6(); mark("p6")
    self.phase7(); mark("p7")
    return self.finish()


MK.p5_setup = p5_setup
MK.phase5 = phase5
MK.phase6 = phase6
MK.phase7 = phase7
MK.build_all = build_all


NCORES = 8
NSEQ_CORE = 4
SEQ_LEN = 2048
CAPACITY = 384
_CACHE = {}


def _consts():
    k = np.arange(128)
    same = (k[:, None] // 64) == (k[None, :] // 64)
    ident = np.eye(128, dtype=np.float32)
    tri = (same & (k[:, None] <= k[None, :])).astype(np.float32)
    trs = (same & (k[:, None] > k[None, :])).astype(np.float32)
    mlow = np.ascontiguousarray(tri.T)
    ind = np.stack([(k < 64), (k >= 64)], 1).astype(np.float32)
    return np.ascontiguousarray(np.concatenate([ident, tri, trs, mlow, ind], 1).astype(np.float32))


def kernel(x, meta_tokens, norm_mix, w_in, conv_dn, a_log, dt_bias, norm_head_dn, w_proj_dn,
           w_alpha, b_alpha, norm_head_gla, w_proj_gla, w_out, norm_ffn, w_group, b_group,
           w_router, b_router, w1, w3, w2, norm_final):
    f = lambda a: np.ascontiguousarray(np.asarray(a, dtype=np.float32))
    x = f(x)
    if "nc" not in _CACHE:
        mk = MK(NSEQ_CORE, SEQ_LEN, CAPACITY)
        _CACHE["nc"] = mk.build_all()
    nc = _CACHE["nc"]
    conv = f(conv_dn)[0]
    shared = {
        "meta": f(meta_tokens), "g_mix": f(norm_mix).reshape(1, D), "w_in": f(w_in)[0],
        "ident": np.eye(128, dtype=np.float32),
        "convT": np.ascontiguousarray(conv.T.reshape(24, 128, 4).transpose(1, 0, 2)),
        "a_log": f(a_log).reshape(1, 8), "dt_bias": f(dt_bias).reshape(1, 8),
        "nh_dn": f(norm_head_dn).reshape(128, 1), "consts": _consts(),
        "w_alpha": f(w_alpha)[0], "b_alphaT": np.ascontiguousarray(f(b_alpha)[0].reshape(4, 128).T),
        "nh_glaT": np.ascontiguousarray(f(norm_head_gla)[0].reshape(2, 128).T),
        "w_proj_dn": f(w_proj_dn)[0], "w_proj_gla": f(w_proj_gla)[0], "w_out": f(w_out)[0],
        "g_ffn": f(norm_ffn).reshape(1, D), "g_fin": f(norm_final).reshape(1, D),
        "w_rt": np.ascontiguousarray(np.concatenate([f(w_group)[0], f(w_router)[0]], 1)),
        "b_rt": np.ascontiguousarray(np.concatenate([f(b_group)[0], f(b_router)[0]])[None]),
        "ebase": (np.arange(64, dtype=np.float32) * CAPACITY)[None],
        "ustr": (np.arange(128)[:, None] < np.arange(128)[None, :]).astype(np.float32),
        "w1": f(w1)[0], "w3": f(w3)[0], "w2": f(w2)[0],
    }
    in_maps = []
    for c in range(NCORES):
        m = dict(shared)
        m["x"] = np.ascontiguousarray(x[c * NSEQ_CORE:(c + 1) * NSEQ_CORE].reshape(NSEQ_CORE * SEQ_LEN, D))
        in_maps.append(m)
    res = run_bass_kernel_spmd(nc, in_maps, core_ids=list(range(NCORES)))
    outs = [np.asarray(r["out"], dtype=np.float32).reshape(NSEQ_CORE, SEQ_LEN, D) for r in res.results]
    return np.concatenate(outs, 0)


class Banks:
    def __init__(self, banks):
        self.banks = banks
        self.i = 0

    def next(self):
        t, R = self.banks[self.i % len(self.banks)]
        self.i += 1
        return t, R


class BankPool:
    def __init__(self, banks):
        self.free = list(banks)

    def acquire(self, k):
        if len(self.free) < k:
            return None
        got, self.free = self.free[:k], self.free[k:]
        return got

    def release(self, got):
        self.free.extend(got)


def acq(bk, k, out):
    while True:
        got = bk.acquire(k)
        if got is not None:
            out.extend(got)
            return
        yield


def v4(t):
    return t[:, :].rearrange("p (s c) -> p s c", s=4)


def v4b(t):
    return t[:, :].bitcast(BF16).rearrange("p (s c) -> p s c", s=4)[:, :, 0:128]


def phase3a_extra(self):
    fw, nc = self.fw, self.nc
    NTL = self.NTILES
    kind = "ExternalOutput" if "gct" in self.debug else "Internal"
    self.GCT = nc.dram_tensor("gct", [8, self.NT], F32, kind=kind).ap()
    self.GC, self.GCR = fw.sbuf("GC", [128, NTL, 8], F32)
    fw.push_scope()
    ps, psR = fw.psum("p3x", [128, 8], F32)
    pt, ptR = fw.psum("p3t", [8, 128], F32)
    rb = [fw.sbuf(f"gcrow{i}", [8, 128], F32) for i in range(2)]
    for i in range(NTL):
        fw.pe.op(lambda: nc.tensor.matmul(ps[:, 0:8], lhsT=self.tri, rhs=self.G[:, i, :], start=True, stop=True),
                 reads=[self.cstR, self.GR_], writes=[psR])
        fw.dve.op(lambda: nc.vector.tensor_copy(out=self.GC[:, i, :], in_=ps[:, 0:8]), reads=[psR], writes=[self.GCR])
        fw.pe.op(lambda: nc.tensor.transpose(out=pt[:, :], in_=self.GC[:, i, :], identity=self.idf[:]),
                 reads=[self.GCR, self.idfR], writes=[ptR])
        r, rR = rb[i % 2]
        fw.act.op(lambda: nc.scalar.copy(out=r[:], in_=pt[:]), reads=[ptR], writes=[rR])
        fw.sp.dma(self.GCT[:, i * 128:(i + 1) * 128], r[:], rR, "r")
    fw.pop_scope()


def dn_group(self, g, gb):
    fw, nc = self.fw, self.nc
    SEQ = self.SEQ
    h0 = g * 4
    groups = groups_of(self.NSEQ, SEQ, gmax=256)
    bk = Banks(gb["banks"])
    PFq = self.PF["qkv"]
    B = lambda k: gb[k]
    S32, S32R = B("S32")
    Sb, SbR = B("Sb")
    Sm, SmR = B("Sm")
    bc = lambda ap: ap.unsqueeze(2).to_broadcast([128, 4, 128])
    for b in range(4):
        for seg in range(3):
            for j in range(4):
                dg, dgR = gb["dg"][b][seg][j]
                fw.pool.op(lambda: nc.gpsimd.tensor_scalar(out=dg[:], in0=self.idf[:], scalar1=self.cw[:, seg * 8 + h0 + b, j:j + 1],
                                                            scalar2=None, op0=ALU.mult), reads=[self.idfR, self.cwR], writes=[dgR])
    yield
    qn4, qnR = B("qn4")
    kn4, knR = B("kn4")
    vc4, vcR = B("vc4")
    qd4, qdR = B("qd4")
    oT4, oTR = B("oT4")
    gr4, grR = B("gr4")
    eg4, egR = B("eg4")
    th, thR = B("th")
    sq, sqR = B("sq")
    ssb, ssbR = B("ssb")
    rn, rnR = B("rn")
    for (n0, N) in groups:
        is_meta = n0 == 0
        seq_start = (not is_meta) and (n0 - 128) % SEQ == 0
        if is_meta:
            fw.pool.op(lambda: nc.gpsimd.memset(S32[:], 0.0), writes=[S32R])
            fw.pool.op(lambda: nc.gpsimd.memset(Sb[:], 0.0), writes=[SbR])
        elif seq_start:
            fw.pool.op(lambda: nc.gpsimd.tensor_copy(out=S32[:], in_=Sm[:]), reads=[SmR], writes=[S32R])
            fw.act.op(lambda: nc.scalar.copy(out=Sb[:], in_=Sm[:]), reads=[SmR], writes=[SbR])
        for b in range(4):
            h = h0 + b
            fw.sp.dma(gr4[:, b, :N], self.GCT[h:h + 1, n0:n0 + N].partition_broadcast(128), grR, "w", part=(b > 0))
            for seg in range(3):
                raw, rawR = gb["raw"][seg]
                rows = slice(seg * 1024 + h * 128, seg * 1024 + (h + 1) * 128)
                if is_meta:
                    fw.pool.op(lambda: nc.gpsimd.memset(raw[:, 0:4], 0.0), writes=[rawR])
                    fw.sp.dma(raw[:, 3:3 + N], PFq[rows, 0:N], rawR, "w")
                elif seq_start:
                    fw.sp.dma(raw[:, 0:3], PFq[rows, 125:128], rawR, "w")
                    fw.sp.dma(raw[:, 3:3 + N], PFq[rows, n0:n0 + N], rawR, "w", part=True)
                else:
                    fw.sp.dma(raw[:, 0:3 + N], PFq[rows, n0 - 3:n0 + N], rawR, "w")
                pg, pgR = bk.next()
                for j in range(4):
                    dg, dgR = gb["dg"][b][seg][j]
                    fw.pe.op(lambda: nc.tensor.matmul(pg[:, :N], lhsT=dg[:], rhs=raw[:, j:j + N], start=(j == 0), stop=(j == 3)),
                             reads=[dgR, rawR], writes=[pgR])
                yield
                fw.act.op(lambda: nc.scalar.activation(out=th[:, :N], in_=pg[:, :N], func=AF.Tanh, scale=0.5), reads=[pgR], writes=[thR])
                if seg < 2:
                    c2, c2R = gb["c2"][seg]
                    fw.dve.op(lambda: nc.vector.scalar_tensor_tensor(out=c2[:, :N], in0=th[:, :N], scalar=1.0, in1=pg[:, :N],
                                                                     op0=ALU.add, op1=ALU.mult), reads=[thR, pgR], writes=[c2R])
                else:
                    fw.dve.op(lambda: nc.vector.scalar_tensor_tensor(out=vc4[:, b, :N], in0=th[:, :N], scalar=1.0, in1=pg[:, :N],
                                                                     op0=ALU.add, op1=ALU.mult), reads=[thR, pgR], writes=[vcR])
                yield
            for seg in range(2):
                c2, c2R = gb["c2"][seg]
                fw.act.op(lambda: nc.scalar.activation(out=sq[:, :N], in_=c2[:, :N], func=AF.Square), reads=[c2R], writes=[sqR])
                pg, pgR = bk.next()
                fw.pe.op(lambda: nc.tensor.matmul(pg[:, :N], lhsT=self.onesb[:], rhs=sq[:, :N], start=True, stop=True),
                         reads=[self.onesbR, sqR], writes=[pgR])
                yield
                fw.act.op(lambda: nc.scalar.activation(out=ssb[:, :N], in_=pg[:, :N], func=AF.Sqrt, bias=self.eps4[:, 0:1]),
                          reads=[pgR, self.epsR], writes=[ssbR])
                fw.dve.op(lambda: nc.vector.reciprocal(out=rn[:, :N], in_=ssb[:, :N]), reads=[ssbR], writes=[rnR])
                dst, dstR = (qn4, qnR) if seg == 0 else (kn4, knR)
                sc = 128 ** -0.5 if seg == 0 else 1.0
                fw.pool.op(lambda: nc.gpsimd.tensor_tensor(out=c2[:, :N], in0=c2[:, :N], in1=rn[:, :N], op=ALU.mult),
                           reads=[c2R, rnR], writes=[c2R])
                fw.pool.op(lambda: nc.gpsimd.tensor_scalar(out=dst[:, b, :N], in0=c2[:, :N], scalar1=sc, scalar2=None, op0=ALU.mult),
                           reads=[c2R], writes=[dstR])
                yield
        fw.act.op(lambda: nc.scalar.activation(out=eg4[:, :, :N], in_=gr4[:, :, :N], func=AF.Exp), reads=[grR], writes=[egR])
        fw.dve.op(lambda: nc.vector.tensor_tensor(out=qd4[:, :, :N], in0=qn4[:, :, :N], in1=eg4[:, :, :N], op=ALU.mult),
                  reads=[qnR, egR], writes=[qdR])
        yield
        for tl in range(N // 128):
            ti = n0 // 128 + tl
            cs = slice(tl * 128, (tl + 1) * 128)
            hs = slice(h0, h0 + 4)
            dm, dmR = B("dm")
            Dm, DmR = B("Dm")
            DsB, DsBR = B("DsB")
            fw.pool.op(lambda: nc.gpsimd.tensor_tensor(out=dm[:], in0=gr4[:, :, cs], in1=bc(self.GC[:, ti, hs]), op=ALU.subtract),
                       reads=[grR, self.GCR], writes=[dmR])
            fw.pool.op(lambda: nc.gpsimd.tensor_scalar(out=dm[:], in0=dm[:], scalar1=0.0, scalar2=None, op0=ALU.max),
                       reads=[dmR], writes=[dmR])
            fw.act.op(lambda: nc.scalar.activation(out=dm[:], in_=dm[:], func=AF.Exp, scale=-1.0), reads=[dmR], writes=[dmR])
            fw.pool.op(lambda: nc.gpsimd.tensor_tensor(out=Dm[:], in0=dm[:], in1=self.mlow4[:], op=ALU.mult),
                       reads=[dmR, self.m4R], writes=[DmR])
            fw.pool.op(lambda: nc.gpsimd.tensor_tensor(out=DsB[:], in0=Dm[:], in1=self.id4f[:], op=ALU.subtract),
                       reads=[DmR, self.m4R], writes=[DsBR])
            fw.pool.op(lambda: nc.gpsimd.tensor_tensor(out=DsB[:], in0=DsB[:], in1=bc(self.BETA[:, ti, hs]), op=ALU.mult),
                       reads=[DsBR, self.BETAR], writes=[DsBR])
            p_kk, p_kkR = bk.next()
            for b in range(4):
                fw.pe.op(lambda: nc.tensor.matmul(p_kk[:, b * 128:(b + 1) * 128], lhsT=kn4[:, b, cs], rhs=kn4[:, b, cs], start=True, stop=True),
                         reads=[knR], writes=[p_kkR])
            p_qk, p_qkR = bk.next()
            for b in range(4):
                fw.pe.op(lambda: nc.tensor.matmul(p_qk[:, b * 128:(b + 1) * 128], lhsT=qn4[:, b, cs], rhs=kn4[:, b, cs], start=True, stop=True),
                         reads=[qnR, knR], writes=[p_qkR])
            yield
            P, PR = gb["Pm"][0]
            QK, QKR = B("QK")
            fw.dve.op(lambda: nc.vector.tensor_tensor(out=P[:], in0=v4(p_kk), in1=DsB[:], op=ALU.mult), reads=[p_kkR, DsBR], writes=[PR])
            fw.dve.op(lambda: nc.vector.tensor_tensor(out=QK[:], in0=v4(p_qk), in1=Dm[:], op=ALU.mult), reads=[p_qkR, DmR], writes=[QKR])
            t_p, t_pR = bk.next()
            for b in range(4):
                fw.pe.op(lambda: nc.tensor.transpose(out=v4b(t_p)[:, b, :], in_=P[:, b, :], identity=self.idb[:]),
                         reads=[PR, self.idbR], writes=[t_pR])
            t_qk, t_qkR = bk.next()
            for b in range(4):
                fw.pe.op(lambda: nc.tensor.transpose(out=v4b(t_qk)[:, b, :], in_=QK[:, b, :], identity=self.idb[:]),
                         reads=[QKR, self.idbR], writes=[t_qkR])
            yield
            Q, QR = gb["Qm"][0]
            X, XR = gb["Xm"][0]
            QKT, QKTR = B("QKT")
            fw.act.op(lambda: nc.scalar.copy(out=Q[:], in_=v4b(t_p)), reads=[t_pR], writes=[QR])
            fw.pool.op(lambda: nc.gpsimd.tensor_tensor(out=X[:], in0=self.id4b[:], in1=Q[:], op=ALU.subtract),
                       reads=[self.m4R, QR], writes=[XR])
            fw.act.op(lambda: nc.scalar.copy(out=QKT[:], in_=v4b(t_qk)), reads=[t_qkR], writes=[QKTR])
            t_k, t_kR = bk.next()
            for b in range(4):
                fw.pe.op(lambda: nc.tensor.transpose(out=v4b(t_k)[:, b, :], in_=kn4[:, b, cs], identity=self.idb[:]),
                         reads=[knR, self.idbR], writes=[t_kR])
            t_v, t_vR = bk.next()
            for b in range(4):
                fw.pe.op(lambda: nc.tensor.transpose(out=v4b(t_v)[:, b, :], in_=vc4[:, b, cs], identity=self.idb[:]),
                         reads=[vcR, self.idbR], writes=[t_vR])
            yield
            ktok, ktokR = B("ktok")
            kb, kbR = B("kb")
            kdec, kdecR = B("kdec")
            vb, vbR = B("vb")
            fw.act.op(lambda: nc.scalar.copy(out=ktok[:], in_=v4b(t_k)), reads=[t_kR], writes=[ktokR])
            fw.pool.op(lambda: nc.gpsimd.tensor_tensor(out=kb[:], in0=ktok[:], in1=bc(self.KBS[:, ti, hs]), op=ALU.mult),
                       reads=[ktokR, self.KBSR], writes=[kbR])
            fw.pool.op(lambda: nc.gpsimd.tensor_tensor(out=kdec[:], in0=ktok[:], in1=bc(self.EGR[:, ti, hs]), op=ALU.mult),
                       reads=[ktokR, self.EGRR], writes=[kdecR])
            fw.dve.op(lambda: nc.vector.tensor_tensor(out=vb[:], in0=v4b(t_v), in1=bc(self.HB[:, ti, hs]), op=ALU.mult),
                      reads=[t_vR, self.HBR], writes=[vbR])
            yield
            for lvl in range(1, 6):
                Pn, PnR = gb["Pm"][lvl % 2]
                Qn, QnR = gb["Qm"][lvl % 2]
                Xn, XnR = gb["Xm"][lvl % 2]
                if lvl < 5:
                    s_q, s_qR = bk.next()
                    for b in range(4):
                        fw.pe.op(lambda: nc.tensor.matmul(s_q[:, b * 128:(b + 1) * 128], lhsT=P[:, b, :], rhs=Q[:, b, :], start=True, stop=True),
                                 reads=[PR, QR], writes=[s_qR])
                s_p, s_pR = bk.next()
                for b in range(4):
                    fw.pe.op(lambda: nc.tensor.matmul(s_p[:, b * 128:(b + 1) * 128], lhsT=Q[:, b, :], rhs=P[:, b, :], start=True, stop=True),
                             reads=[PR, QR], writes=[s_pR])
                yield
                fw.act.op(lambda: nc.scalar.copy(out=Pn[:], in_=v4(s_p)), reads=[s_pR], writes=[PnR])
                if lvl < 5:
                    fw.dve.op(lambda: nc.vector.tensor_copy(out=Qn[:], in_=v4(s_q)), reads=[s_qR], writes=[QnR])
                s_x, s_xR = bk.next()
                for b in range(4):
                    fw.pe.op(lambda: nc.tensor.matmul(s_x[:, b * 128:(b + 1) * 128], lhsT=Pn[:, b, :], rhs=X[:, b, :], start=True, stop=True),
                             reads=[PnR, XR], writes=[s_xR])
                yield
                fw.dve.op(lambda: nc.vector.tensor_tensor(out=Xn[:], in0=X[:], in1=v4(s_x), op=ALU.add), reads=[XR, s_xR], writes=[XnR])
                P, PR, Q, QR, X, XR = Pn, PnR, Qn, QnR, Xn, XnR
            s_u, s_uR = bk.next()
            for b in range(4):
                fw.pe.op(lambda: nc.tensor.matmul(s_u[:, b * 128:(b + 1) * 128], lhsT=X[:, b, :], rhs=vb[:, b, :], start=True, stop=True),
                         reads=[XR, vbR], writes=[s_uR])
            s_w, s_wR = bk.next()
            for b in range(4):
                fw.pe.op(lambda: nc.tensor.matmul(s_w[:, b * 128:(b + 1) * 128], lhsT=kb[:, b, :], rhs=X[:, b, :], start=True, stop=True),
                         reads=[XR, kbR], writes=[s_wR])
            yield
            usb, usbR = B("usb")
            wT, wTR = B("wT")
            vnew, vnewR = B("vnew")
            fw.act.op(lambda: nc.scalar.copy(out=usb[:], in_=v4(s_u)), reads=[s_uR], writes=[usbR])
            fw.dve.op(lambda: nc.vector.tensor_copy(out=wT[:], in_=v4(s_w)), reads=[s_wR], writes=[wTR])
            yield
            for c in range(2):
                r = slice(c * 64, (c + 1) * 64)
                col = tl * 128 + c * 64
                s_ws, s_wsR = bk.next()
                for b in range(4):
                    fw.pe.op(lambda: nc.tensor.matmul(s_ws[r, b * 128:(b + 1) * 128], lhsT=wT[:, b, r], rhs=Sb[:, b, :], start=True, stop=True),
                             reads=[wTR, SbR], writes=[s_wsR])
                yield
                fw.dve.op(lambda: nc.vector.tensor_tensor(out=vnew[r, :, :], in0=usb[r, :, :], in1=s_ws[r, :].rearrange("p (s c) -> p s c", s=4),
                                                          op=ALU.subtract), reads=[usbR, s_wsR], writes=[vnewR])
                s_o, s_oR = bk.next()
                for b in range(4):
                    fw.pe.op(lambda: nc.tensor.matmul(s_o[:, b * 128:b * 128 + 64], lhsT=Sb[:, b, :], rhs=qd4[:, b, col:col + 64], start=True, stop=False),
                             reads=[SbR, qdR], writes=[s_oR])
                    fw.pe.op(lambda: nc.tensor.matmul(s_o[:, b * 128:b * 128 + 64], lhsT=vnew[r, b, :], rhs=QKT[r, b, r], start=False, stop=True),
                             reads=[vnewR, QKTR], writes=[s_oR])
                s_s, s_sR = bk.next()
                for b in range(4):
                    fw.pe.op(lambda: nc.tensor.matmul(s_s[:, b * 128:(b + 1) * 128], lhsT=kdec[r, b, :], rhs=vnew[r, b, :], start=True, stop=True),
                             reads=[kdecR, vnewR], writes=[s_sR])
                yield
                fw.act.op(lambda: nc.scalar.copy(out=oT4[:, :, col:col + 64], in_=v4(s_o)[:, :, 0:64]), reads=[s_oR], writes=[oTR])
                fw.dve.op(lambda: nc.vector.tensor_tensor(out=S32[:], in0=S32[:], in1=bc(self.EGL[:, ti, c * 8 + h0:c * 8 + h0 + 4]), op=ALU.mult),
                          reads=[S32R, self.EGLR], writes=[S32R])
                fw.dve.op(lambda: nc.vector.tensor_tensor(out=S32[:], in0=S32[:], in1=v4(s_s), op=ALU.add), reads=[S32R, s_sR], writes=[S32R])
                fw.act.op(lambda: nc.scalar.copy(out=Sb[:], in_=S32[:]), reads=[S32R], writes=[SbR])
                yield
        if is_meta:
            fw.pool.op(lambda: nc.gpsimd.tensor_copy(out=Sm[:], in_=S32[:]), reads=[S32R], writes=[SmR])
        for b in range(4):
            h = h0 + b
            fw.act.op(lambda: nc.scalar.activation(out=sq[:, :N], in_=oT4[:, b, :N], func=AF.Square), reads=[oTR], writes=[sqR])
            pg, pgR = bk.next()
            fw.pe.op(lambda: nc.tensor.matmul(pg[:, :N], lhsT=self.onesb[:], rhs=sq[:, :N], start=True, stop=True),
                     reads=[self.onesbR, sqR], writes=[pgR])
            zt, ztR = B("zt")
            fw.sp.dma(zt[:, :N], self.PF["z"][h * 128:(h + 1) * 128, n0:n0 + N], ztR, "w")
            yield
            fw.act.op(lambda: nc.scalar.activation(out=ssb[:, :N], in_=pg[:, :N], func=AF.Sqrt, scale=1.0 / 128, bias=self.eps1[:, 0:1]),
                      reads=[pgR, self.epsR], writes=[ssbR])
            fw.dve.op(lambda: nc.vector.reciprocal(out=rn[:, :N], in_=ssb[:, :N]), reads=[ssbR], writes=[rnR])
            fw.act.op(lambda: nc.scalar.activation(out=th[:, :N], in_=zt[:, :N], func=AF.Tanh, scale=0.5), reads=[ztR], writes=[thR])
            fw.pool.op(lambda: nc.gpsimd.scalar_tensor_tensor(out=th[:, :N], in0=th[:, :N], scalar=1.0, in1=zt[:, :N], op0=ALU.add, op1=ALU.mult),
                       reads=[thR, ztR], writes=[thR]) if False else \
                fw.dve.op(lambda: nc.vector.scalar_tensor_tensor(out=th[:, :N], in0=th[:, :N], scalar=1.0, in1=zt[:, :N], op0=ALU.add, op1=ALU.mult),
                          reads=[thR, ztR], writes=[thR])
            fw.dve.op(lambda: nc.vector.scalar_tensor_tensor(out=rn[:, :N], in0=oT4[:, b, :N], scalar=self.nhh[:, 0:1], in1=rn[:, :N],
                                                             op0=ALU.mult, op1=ALU.mult), reads=[oTR, self.nhhR, rnR], writes=[rnR])
            od, odR = B("od")
            fw.pool.op(lambda: nc.gpsimd.tensor_tensor(out=od[:, :N], in0=rn[:, :N], in1=th[:, :N], op=ALU.mult),
                       reads=[rnR, thR], writes=[odR])
            fw.sp.dma(self.OD[h * 128:(h + 1) * 128, n0:n0 + N], od[:, :N], odR, "r")
            yield


def phase3b(self):
    fw, nc = self.fw, self.nc
    fw.push_scope()
    self.cw, self.cwR = fw.sbuf("cw", [128, 24, 4], F32)
    fw.sp.dma(self.cw[:], self.convT, self.cwR, "w")
    self.nhh, self.nhhR = fw.sbuf("nhh", [128, 1], F32)
    fw.sp.dma(self.nhh[:], self.nh_dn, self.nhhR, "w")
    fw.dve.op(lambda: nc.vector.tensor_scalar(out=self.nhh[:], in0=self.nhh[:], scalar1=0.5, scalar2=None, op0=ALU.mult),
              reads=[self.nhhR], writes=[self.nhhR])
    self.mlow4, self.m4R = fw.sbuf("mlow4", [128, 4, 128], F32)
    self.id4f, _ = fw.sbuf("id4f", [128, 4, 128], F32)
    self.id4b, _ = fw.sbuf("id4b", [128, 4, 128], BF16)
    for b in range(4):
        fw.pool.op(lambda: nc.gpsimd.tensor_copy(out=self.mlow4[:, b, :], in_=self.mlow), reads=[self.cstR], writes=[self.m4R])
        fw.pool.op(lambda: nc.gpsimd.tensor_copy(out=self.id4f[:, b, :], in_=self.idf[:]), reads=[self.idfR], writes=[self.m4R])
        fw.pool.op(lambda: nc.gpsimd.tensor_copy(out=self.id4b[:, b, :], in_=self.idf[:]), reads=[self.idfR], writes=[self.m4R])
    gbs = []
    NG = 256
    for p in range(2):
        gb = {}
        gb["banks"] = [fw.psum(f"bk{p}_{i}", [128, 512], F32) for i in range(4)]
        t2 = lambda nm, dt=BF16, w=NG: fw.sbuf(f"{nm}{p}", [128, w], dt)
        t4 = lambda nm, dt=BF16, w=128: fw.sbuf(f"{nm}{p}", [128, 4, w], dt)
        gb["S32"], gb["Sb"], gb["Sm"] = t4("S32_", F32), t4("Sb_"), t4("Sm_", F32)
        gb["dg"] = [[[t2(f"dg{b}{s}{j}_", BF16, 128) for j in range(4)] for s in range(3)] for b in range(4)]
        gb["raw"] = [t2(f"raw{s}_", BF16, NG + 4) for s in range(3)]
        gb["c2"] = [t2("c2q_", F32), t2("c2k_", F32)]
        for nm in ("qn4", "kn4", "vc4", "qd4"):
            gb[nm] = t4(nm, BF16, NG)
        for nm in ("oT4", "gr4", "eg4"):
            gb[nm] = t4(nm, F32, NG)
        for nm, dt in [("th", F32), ("sq", BF16), ("ssb", F32), ("rn", F32), ("zt", BF16), ("od", BF16)]:
            gb[nm] = t2(nm + "_", dt)
        for nm, dt in [("dm", F32), ("Dm", F32), ("DsB", F32), ("usb", F32), ("QK", BF16), ("QKT", BF16), ("ktok", BF16),
                       ("kb", BF16), ("kdec", BF16), ("vb", BF16), ("wT", BF16), ("vnew", BF16)]:
            gb[nm] = t4(nm + "_", dt)
        gb["Pm"] = [t4("Pm0_"), t4("Pm1_")]
        gb["Qm"] = [t4("Qm0_"), t4("Qm1_")]
        gb["Xm"] = [t4("Xm0_"), t4("Xm1_")]
        gbs.append(gb)
    run_interleaved([dn_group(self, p, gbs[p]) for p in range(2)])
    fw.pop_scope()


MK.phase3a_extra = phase3a_extra
MK.phase3b = phase3b


def p3_streams(self, g, sh, bk):
    fw, nc = self.fw, self.nc
    SEQ = self.SEQ
    h0 = g * 4
    hs = slice(h0, h0 + 4)
    NTL = self.NTILES
    TPS = SEQ // 128
    PFq = self.PF["qkv"]
    bc = lambda ap, w=128: ap.unsqueeze(2).to_broadcast([128, 4, w])
    prog = sh["prog"]

    def gen_P():
        for t in range(NTL):
            while prog["F"] < t - 1 or prog["B"] < t - 1:
                yield
            par = t % 2
            n0 = t * 128
            qn4, qnR = sh["qn4"][par]
            kn4, knR = sh["kn4"][par]
            vc4, vcR = sh["vc4"][par]
            qd4, qdR = sh["qd4"][par]
            gr4, grR = sh["gr4"][par]
            fw.sp.dma(gr4[:], self.GCT[hs, n0:n0 + 128].partition_broadcast(128), grR, "w")
            for seg in range(3):
                raw, rawR = sh["raw"][seg]
                fw.sp.dma(raw[:, :, 0:128], PFq[seg * 1024 + h0 * 128:seg * 1024 + (h0 + 4) * 128, n0:n0 + 128].rearrange("(b p) n -> p b n", p=128),
                          rawR, "w")
                th, thR = sh["th"][seg % 2]
                fw.act.op(lambda: nc.scalar.activation(out=th[:], in_=raw[:, :, 0:128], func=AF.Tanh, scale=0.5), reads=[rawR], writes=[thR])
                if seg < 2:
                    c2, c2R = sh["c2"][seg]
                    fw.dve.op(lambda: nc.vector.scalar_tensor_tensor(out=c2[:], in0=th[:], scalar=1.0, in1=raw[:, :, 0:128], op0=ALU.add, op1=ALU.mult),
                              reads=[thR, rawR], writes=[c2R])
                else:
                    fw.dve.op(lambda: nc.vector.scalar_tensor_tensor(out=vc4[:], in0=th[:], scalar=1.0, in1=raw[:, :, 0:128], op0=ALU.add, op1=ALU.mult),
                              reads=[thR, rawR], writes=[vcR])
                yield
            gst = []
            yield from acq(bk, 1, gst)
            pst, pstR = gst[0]
            for seg in range(2):
                c2, c2R = sh["c2"][seg]
                sq, sqR = sh["sq"][seg]
                fw.act.op(lambda: nc.scalar.activation(out=sq[:], in_=c2[:], func=AF.Square), reads=[c2R], writes=[sqR])
                for b in range(4):
                    fw.pe.op(lambda: nc.tensor.matmul(pst[:, seg * 4 + b:seg * 4 + b + 1], lhsT=sq[:, b, :], rhs=self.onesb[:, 0:1],
                                                      start=True, stop=True), reads=[self.onesbR, sqR], writes=[pstR])
            yield
            st8, st8R = sh["st8"]
            fw.dve.op(lambda: nc.vector.tensor_scalar(out=st8[:], in0=pst[:, 0:8], scalar1=4 * EPS, scalar2=None, op0=ALU.add),
                      reads=[pstR], writes=[st8R])
            bk.release(gst)
            fw.pool.op(lambda: nc.gpsimd.tensor_tensor(out=st8[:], in0=st8[:], in1=self.mhalf[:, 0:8], op=ALU.pow),
                       reads=[st8R, self.mhalfR], writes=[st8R])
            yield
            gnq = []
            yield from acq(bk, 2, gnq)
            for seg in range(2):
                pn, pnR = gnq[seg]
                dgn, dgnR = sh["dgn"][seg]
                fw.pool.op(lambda: nc.gpsimd.tensor_tensor(out=dgn[:], in0=self.id4f[:], in1=bc(st8[:, seg * 4:(seg + 1) * 4]), op=ALU.mult),
                           reads=[self.m4R, st8R], writes=[dgnR])
                for b in range(4):
                    fw.pe.op(lambda: nc.tensor.matmul(pn[:, b * 128:(b + 1) * 128], lhsT=self.onesb[:], rhs=dgn[:, b, :], start=True, stop=True),
                             reads=[self.onesbR, dgnR], writes=[pnR])
            yield
            for seg in range(2):
                pn, pnR = gnq[seg]
                c2, c2R = sh["c2"][seg]
                dst, dstR = (qn4, qnR) if seg == 0 else (kn4, knR)
                sc = 128 ** -0.5 if seg == 0 else 1.0
                fw.dve.op(lambda: nc.vector.scalar_tensor_tensor(out=dst[:], in0=c2[:], scalar=sc, in1=v4(pn), op0=ALU.mult, op1=ALU.mult),
                          reads=[c2R, pnR], writes=[dstR])
            bk.release(gnq)
            yield
            eg4, egR = sh["eg4"]
            fw.act.op(lambda: nc.scalar.activation(out=eg4[:], in_=gr4[:], func=AF.Exp), reads=[grR], writes=[egR])
            fw.pool.op(lambda: nc.gpsimd.tensor_tensor(out=qd4[:], in0=qn4[:], in1=eg4[:], op=ALU.mult), reads=[qnR, egR], writes=[qdR])
            prog["P"] = t + 1
            yield

    def gen_F():
        for t in range(NTL):
            while prog["P"] < t + 1 or prog["B"] < t - 1:
                yield
            par = t % 2
            ti = t
            qn4, qnR = sh["qn4"][par]
            kn4, knR = sh["kn4"][par]
            vc4, vcR = sh["vc4"][par]
            gr4, grR = sh["gr4"][par]
            QKT, QKTR = sh["QKT"][par]
            kdec, kdecR = sh["kdec"][par]
            usb, usbR = sh["usb"][par]
            wT, wTR = sh["wT"][par]
            dm, dmR = sh["dm"]
            Dm, DmR = sh["Dm"]
            DsB, DsBR = sh["DsB"]
            fw.pool.op(lambda: nc.gpsimd.tensor_tensor(out=DsB[:], in0=self.mstr4[:], in1=bc(self.BETA[:, ti, hs]), op=ALU.mult),
                       reads=[self.m4R, self.BETAR], writes=[DsBR])
            fw.dve.op(lambda: nc.vector.tensor_tensor(out=dm[:], in0=gr4[:], in1=bc(self.GC[:, ti, hs]), op=ALU.subtract),
                      reads=[grR, self.GCR], writes=[dmR])
            fw.pool.op(lambda: nc.gpsimd.tensor_scalar(out=dm[:], in0=dm[:], scalar1=3.0e38, scalar2=0.0, op0=ALU.min, op1=ALU.max),
                       reads=[dmR], writes=[dmR])
            fw.act.op(lambda: nc.scalar.activation(out=dm[:], in_=dm[:], func=AF.Exp, scale=-1.0), reads=[dmR], writes=[dmR])
            fw.pool.op(lambda: nc.gpsimd.tensor_tensor(out=Dm[:], in0=dm[:], in1=self.mlow4[:], op=ALU.mult),
                       reads=[dmR, self.m4R], writes=[DmR])
            fw.dve.op(lambda: nc.vector.tensor_tensor(out=DsB[:], in0=DsB[:], in1=dm[:], op=ALU.mult), reads=[DsBR, dmR], writes=[DsBR])
            gkq = []
            yield from acq(bk, 2, gkq)
            (p_kk, p_kkR), (p_qk, p_qkR) = gkq
            for b in range(4):
                fw.pe.op(lambda: nc.tensor.matmul(p_kk[:, b * 128:(b + 1) * 128], lhsT=kn4[:, b, :], rhs=kn4[:, b, :], start=True, stop=True),
                         reads=[knR], writes=[p_kkR])
            for b in range(4):
                fw.pe.op(lambda: nc.tensor.matmul(p_qk[:, b * 128:(b + 1) * 128], lhsT=qn4[:, b, :], rhs=kn4[:, b, :], start=True, stop=True),
                         reads=[qnR, knR], writes=[p_qkR])
            gkv = []
            yield from acq(bk, 2, gkv)
            (t_k, t_kR), (t_v, t_vR) = gkv
            for b in range(4):
                fw.pe.op(lambda: nc.tensor.transpose(out=v4b(t_k)[:, b, :], in_=kn4[:, b, :], identity=self.idb[:]),
                         reads=[knR, self.idbR], writes=[t_kR])
            for b in range(4):
                fw.pe.op(lambda: nc.tensor.transpose(out=v4b(t_v)[:, b, :], in_=vc4[:, b, :], identity=self.idb[:]),
                         reads=[vcR, self.idbR], writes=[t_vR])
            yield
            ktok, ktokR = sh["ktok"]
            kb, kbR = sh["kb"]
            vb, vbR = sh["vb"]
            fw.act.op(lambda: nc.scalar.copy(out=ktok[:], in_=v4b(t_k)), reads=[t_kR], writes=[ktokR])
            fw.dve.op(lambda: nc.vector.tensor_tensor(out=vb[:], in0=v4b(t_v), in1=bc(self.HB[:, ti, hs]), op=ALU.mult),
                      reads=[t_vR, self.HBR], writes=[vbR])
            bk.release(gkv)
            fw.pool.op(lambda: nc.gpsimd.tensor_tensor(out=kb[:], in0=ktok[:], in1=bc(self.KBS[:, ti, hs]), op=ALU.mult),
                       reads=[ktokR, self.KBSR], writes=[kbR])
            fw.pool.op(lambda: nc.gpsimd.tensor_tensor(out=kdec[:], in0=ktok[:], in1=bc(self.EGR[:, ti, hs]), op=ALU.mult),
                       reads=[ktokR, self.EGRR], writes=[kdecR])
            yield
            P, PR = sh["Pm"][0]
            QK, QKR = sh["QK"]
            fw.dve.op(lambda: nc.vector.tensor_tensor(out=P[:], in0=v4(p_kk), in1=DsB[:], op=ALU.mult), reads=[p_kkR, DsBR], writes=[PR])
            fw.dve.op(lambda: nc.vector.tensor_tensor(out=QK[:], in0=v4(p_qk), in1=Dm[:], op=ALU.mult), reads=[p_qkR, DmR], writes=[QKR])
            bk.release(gkq)
            gtp = []
            yield from acq(bk, 2, gtp)
            (t_p, t_pR), (t_qk, t_qkR) = gtp
            for b in range(4):
                fw.pe.op(lambda: nc.tensor.transpose(out=v4b(t_p)[:, b, :], in_=P[:, b, :], identity=self.idb[:]),
                         reads=[PR, self.idbR], writes=[t_pR])
            for b in range(4):
                fw.pe.op(lambda: nc.tensor.transpose(out=v4b(t_qk)[:, b, :], in_=QK[:, b, :], identity=self.idb[:]),
                         reads=[QKR, self.idbR], writes=[t_qkR])
            yield
            Q, QR = sh["Qm"][0]
            X, XR = sh["Xm"][0]
            fw.act.op(lambda: nc.scalar.copy(out=Q[:], in_=v4b(t_p)), reads=[t_pR], writes=[QR])
            fw.dve.op(lambda: nc.vector.tensor_tensor(out=X[:], in0=self.id4b[:], in1=v4b(t_p), op=ALU.subtract),
                      reads=[self.m4R, t_pR], writes=[XR])
            fw.act.op(lambda: nc.scalar.copy(out=QKT[:], in_=v4b(t_qk)), reads=[t_qkR], writes=[QKTR])
            bk.release(gtp)
            yield
            Pprev = None
            for r in range(1, 7):
                Pn, PnR = sh["Pm"][r % 2]
                Qn, QnR = sh["Qm"][r % 2]
                Xn, XnR = sh["Xm"][r % 2]
                need_sq = r <= 5
                need_q = r <= 4
                need_x = r >= 2
                nb_ = (1 if need_sq else 0) + (1 if need_q else 0) + (1 if need_x else 0)
                gl = []
                yield from acq(bk, nb_, gl)
                gi_ = iter(gl)
                if need_sq:
                    s_p, s_pR = next(gi_)
                    for b in range(4):
                        fw.pe.op(lambda: nc.tensor.matmul(s_p[:, b * 128:(b + 1) * 128], lhsT=Q[:, b, :], rhs=P[:, b, :], start=True, stop=True),
                                 reads=[PR, QR], writes=[s_pR])
                if need_x:
                    s_x, s_xR = next(gi_)
                    for b in range(4):
                        fw.pe.op(lambda: nc.tensor.matmul(s_x[:, b * 128:(b + 1) * 128], lhsT=P[:, b, :], rhs=X[:, b, :], start=True, stop=True),
                                 reads=[PR, XR], writes=[s_xR])
                if need_q:
                    s_q, s_qR = next(gi_)
                    for b in range(4):
                        fw.pe.op(lambda: nc.tensor.matmul(s_q[:, b * 128:(b + 1) * 128], lhsT=P[:, b, :], rhs=Q[:, b, :], start=True, stop=True),
                                 reads=[PR, QR], writes=[s_qR])
                yield
                if need_sq:
                    fw.act.op(lambda: nc.scalar.copy(out=Pn[:], in_=v4(s_p)), reads=[s_pR], writes=[PnR])
                if need_x:
                    fw.dve.op(lambda: nc.vector.tensor_tensor(out=Xn[:], in0=X[:], in1=v4(s_x), op=ALU.add), reads=[XR, s_xR], writes=[XnR])
                    X, XR = Xn, XnR
                if need_q:
                    fw.act.op(lambda: nc.scalar.copy(out=Qn[:], in_=v4(s_q)), reads=[s_qR], writes=[QnR])
                bk.release(gl)
                if need_sq:
                    P, PR = Pn, PnR
                if need_q:
                    Q, QR = Qn, QnR
                yield
            guw = []
            yield from acq(bk, 2, guw)
            (s_u, s_uR), (s_w, s_wR) = guw
            for b in range(4):
                fw.pe.op(lambda: nc.tensor.matmul(s_u[:, b * 128:(b + 1) * 128], lhsT=X[:, b, :], rhs=vb[:, b, :], start=True, stop=True),
                         reads=[XR, vbR], writes=[s_uR])
            for b in range(4):
                fw.pe.op(lambda: nc.tensor.matmul(s_w[:, b * 128:(b + 1) * 128], lhsT=kb[:, b, :], rhs=X[:, b, :], start=True, stop=True),
                         reads=[XR, kbR], writes=[s_wR])
            yield
            fw.act.op(lambda: nc.scalar.copy(out=usb[:], in_=v4(s_u)), reads=[s_uR], writes=[usbR])
            fw.act.op(lambda: nc.scalar.copy(out=wT[:], in_=v4(s_w)), reads=[s_wR], writes=[wTR])
            bk.release(guw)
            prog["F"] = t + 1
            yield

    def gen_B():
        S32, S32R = sh["S32"]
        Sb, SbR = sh["Sb"]
        Sm, SmR = sh["Sm"]
        vnew, vnewR = sh["vnew"]
        for t in range(NTL):
            while prog["F"] < t + 1 or prog["O"] < t - 1:
                yield
            par = t % 2
            ti = t
            is_meta = t == 0
            seq_start = (not is_meta) and (t - 1) % TPS == 0
            qd4, qdR = sh["qd4"][par]
            QKT, QKTR = sh["QKT"][par]
            kdec, kdecR = sh["kdec"][par]
            usb, usbR = sh["usb"][par]
            wT, wTR = sh["wT"][par]
            oT4, oTR = sh["oT4"][par]
            if is_meta:
                fw.pool.op(lambda: nc.gpsimd.memset(S32[:], 0.0), writes=[S32R])
                fw.pool.op(lambda: nc.gpsimd.memset(Sb[:], 0.0), writes=[SbR])
            elif seq_start:
                fw.pool.op(lambda: nc.gpsimd.tensor_copy(out=S32[:], in_=Sm[:]), reads=[SmR], writes=[S32R])
                fw.act.op(lambda: nc.scalar.copy(out=Sb[:], in_=Sm[:]), reads=[SmR], writes=[SbR])
            for c in range(2):
                r = slice(c * 64, (c + 1) * 64)
                gw = []
                yield from acq(bk, 1, gw)
                s_ws, s_wsR = gw[0]
                for b in range(4):
                    fw.pe.op(lambda: nc.tensor.matmul(s_ws[r, b * 128:(b + 1) * 128], lhsT=wT[:, b, r], rhs=Sb[:, b, :], start=True, stop=True),
                             reads=[wTR, SbR], writes=[s_wsR])
                yield
                fw.dve.op(lambda: nc.vector.tensor_tensor(out=vnew[r, :, :], in0=usb[r, :, :], in1=s_ws[r, :].rearrange("p (s c) -> p s c", s=4),
                                                          op=ALU.subtract), reads=[usbR, s_wsR], writes=[vnewR])
                bk.release(gw)
                gso = []
                yield from acq(bk, 2, gso)
                (s_s, s_sR), (s_o, s_oR) = gso
                for b in range(4):
                    fw.pe.op(lambda: nc.tensor.matmul(s_s[:, b * 128:(b + 1) * 128], lhsT=kdec[r, b, :], rhs=vnew[r, b, :], start=True, stop=True),
                             reads=[kdecR, vnewR], writes=[s_sR])
                for b in range(4):
                    fw.pe.op(lambda: nc.tensor.matmul(s_o[:, b * 128:b * 128 + 64], lhsT=Sb[:, b, :], rhs=qd4[:, b, r], start=True, stop=False),
                             reads=[SbR, qdR], writes=[s_oR])
                    fw.pe.op(lambda: nc.tensor.matmul(s_o[:, b * 128:b * 128 + 64], lhsT=vnew[r, b, :], rhs=QKT[r, b, r], start=False, stop=True),
                             reads=[vnewR, QKTR], writes=[s_oR])
                yield
                fw.dve.op(lambda: nc.vector.tensor_tensor(out=S32[:], in0=S32[:], in1=bc(self.EGL[:, ti, c * 8 + h0:c * 8 + h0 + 4]), op=ALU.mult),
                          reads=[S32R, self.EGLR], writes=[S32R])
                fw.dve.op(lambda: nc.vector.tensor_tensor(out=S32[:], in0=S32[:], in1=v4(s_s), op=ALU.add), reads=[S32R, s_sR], writes=[S32R])
                fw.act.op(lambda: nc.scalar.copy(out=Sb[:], in_=S32[:]), reads=[S32R], writes=[SbR])
                fw.act.op(lambda: nc.scalar.copy(out=oT4[:, :, r], in_=v4(s_o)[:, :, 0:64]), reads=[s_oR], writes=[oTR])
                bk.release(gso)
                yield
            if is_meta:
                fw.pool.op(lambda: nc.gpsimd.tensor_copy(out=Sm[:], in_=S32[:]), reads=[S32R], writes=[SmR])
            prog["B"] = t + 1
            yield

    def gen_O():
        for t in range(NTL):
            while prog["B"] < t + 1:
                yield
            par = t % 2
            n0 = t * 128
            oT4, oTR = sh["oT4"][par]
            osq, osqR = sh["osq"]
            ors, orsR = sh["ors"]
            zt, ztR = sh["zt"]
            zth, zthR = sh["zth"]
            od, odR = sh["od"][par]
            rows = lambda ap: ap[h0 * 128:(h0 + 4) * 128, n0:n0 + 128].rearrange("(b p) n -> p b n", p=128)
            fw.sp.dma(zt[:], rows(self.PF["z"]), ztR, "w")
            fw.act.op(lambda: nc.scalar.activation(out=osq[:], in_=oT4[:], func=AF.Square), reads=[oTR], writes=[osqR])
            go = []
            yield from acq(bk, 1, go)
            pg, pgR = go[0]
            for b in range(4):
                fw.pe.op(lambda: nc.tensor.matmul(pg[:, b:b + 1], lhsT=osq[:, b, :], rhs=self.onesb[:, 0:1], start=True, stop=True),
                         reads=[self.onesbR, osqR], writes=[pgR])
            fw.act.op(lambda: nc.scalar.activation(out=zth[:], in_=zt[:], func=AF.Tanh, scale=0.5), reads=[ztR], writes=[zthR])
            yield
            so4, so4R = sh["so4"]
            fw.dve.op(lambda: nc.vector.tensor_scalar(out=so4[:], in0=pg[:, 0:4], scalar1=1.0 / 128, scalar2=EPS, op0=ALU.mult, op1=ALU.add),
                      reads=[pgR], writes=[so4R])
            bk.release(go)
            fw.pool.op(lambda: nc.gpsimd.tensor_tensor(out=so4[:], in0=so4[:], in1=self.mhalf[:, 0:4], op=ALU.pow),
                       reads=[so4R, self.mhalfR], writes=[so4R])
            fw.dve.op(lambda: nc.vector.scalar_tensor_tensor(out=zth[:], in0=zth[:], scalar=1.0, in1=zt[:], op0=ALU.add, op1=ALU.mult),
                      reads=[zthR, ztR], writes=[zthR])
            dgo, dgoR = sh["dgo"]
            fw.pool.op(lambda: nc.gpsimd.tensor_tensor(out=dgo[:], in0=self.id4f[:], in1=bc(so4[:, 0:4]), op=ALU.mult),
                       reads=[self.m4R, so4R], writes=[dgoR])
            go2 = []
            yield from acq(bk, 1, go2)
            pr, prR = go2[0]
            for b in range(4):
                fw.pe.op(lambda: nc.tensor.matmul(pr[:, b * 128:(b + 1) * 128], lhsT=self.onesb[:], rhs=dgo[:, b, :], start=True, stop=True),
                         reads=[self.onesbR, dgoR], writes=[prR])
            yield
            fw.dve.op(lambda: nc.vector.scalar_tensor_tensor(out=ors[:], in0=oT4[:], scalar=self.nhh[:, 0:1], in1=v4(pr), op0=ALU.mult, op1=ALU.mult),
                      reads=[oTR, self.nhhR, prR], writes=[orsR])
            bk.release(go2)
            fw.pool.op(lambda: nc.gpsimd.tensor_tensor(out=od[:], in0=ors[:], in1=zth[:], op=ALU.mult), reads=[orsR, zthR], writes=[odR])
            fw.sp.dma(rows(self.OD), od[:], odR, "r")
            prog["O"] = t + 1
            yield

    return [gen_P(), gen_F(), gen_B(), gen_O()]


def phase3c(self):
    fw, nc = self.fw, self.nc
    fw.push_scope()
    self.cw, self.cwR = fw.sbuf("cw", [128, 24, 4], F32)
    fw.sp.dma(self.cw[:], self.convT, self.cwR, "w")
    self.nhh, self.nhhR = fw.sbuf("nhh", [128, 1], F32)
    fw.sp.dma(self.nhh[:], self.nh_dn, self.nhhR, "w")
    fw.dve.op(lambda: nc.vector.tensor_scalar(out=self.nhh[:], in0=self.nhh[:], scalar1=0.5, scalar2=None, op0=ALU.mult),
              reads=[self.nhhR], writes=[self.nhhR])
    self.mlow4, self.m4R = fw.sbuf("mlow4", [128, 4, 128], F32)
    self.id4f, _ = fw.sbuf("id4f", [128, 4, 128], F32)
    self.id4b, _ = fw.sbuf("id4b", [128, 4, 128], BF16)
    self.mstr4, _ = fw.sbuf("mstr4", [128, 4, 128], F32)
    for b in range(4):
        fw.pool.op(lambda: nc.gpsimd.tensor_copy(out=self.mlow4[:, b, :], in_=self.mlow), reads=[self.cstR], writes=[self.m4R])
        fw.pool.op(lambda: nc.gpsimd.tensor_copy(out=self.id4f[:, b, :], in_=self.idf[:]), reads=[self.idfR], writes=[self.m4R])
        fw.pool.op(lambda: nc.gpsimd.tensor_copy(out=self.id4b[:, b, :], in_=self.idf[:]), reads=[self.idfR], writes=[self.m4R])
    fw.pool.op(lambda: nc.gpsimd.tensor_tensor(out=self.mstr4[:], in0=self.mlow4[:], in1=self.id4f[:], op=ALU.subtract),
               reads=[self.m4R], writes=[self.m4R])
    bk = BankPool([fw.psum(f"bkc{i}", [128, 512], F32) for i in range(8)])
    gens = []
    for p in range(2):
        sh = {"prog": {"P": 0, "F": 0, "B": 0, "O": 0}}
        t4 = lambda nm, dt=BF16, w=128: fw.sbuf(f"c3_{nm}{p}", [128, 4, w], dt)
        for nm, dt in [("qn4", BF16), ("kn4", BF16), ("vc4", BF16), ("qd4", BF16), ("gr4", F32),
                       ("QKT", BF16), ("kdec", BF16), ("usb", F32), ("wT", BF16), ("oT4", F32), ("od", BF16)]:
            sh[nm] = [t4(nm + "a", dt), t4(nm + "b", dt)]
        sh["raw"] = [t4(f"raw{s}_", BF16, 132) for s in range(3)]
        sh["th"] = [t4("tha", F32), t4("thb", F32)]
        sh["c2"] = [t4("c2q", F32), t4("c2k", F32)]
        sh["sq"] = [t4("sqq"), t4("sqk")]
        for nm, dt in [("eg4", F32), ("dm", F32), ("Dm", F32), ("DsB", F32), ("ktok", BF16), ("kb", BF16), ("vb", BF16), ("QK", BF16),
                       ("vnew", BF16), ("S32", F32), ("Sb", BF16), ("Sm", F32), ("osq", BF16), ("ors", F32), ("zt", BF16), ("zth", F32)]:
            sh[nm] = t4(nm + "_", dt)
        sh["Pm"] = [t4("Pm0_"), t4("Pm1_")]
        sh["Qm"] = [t4("Qm0_"), t4("Qm1_")]
        sh["Xm"] = [t4("Xm0_"), t4("Xm1_")]
        sh["st8"] = fw.sbuf(f"c3_st8{p}", [128, 8], F32)
        sh["so4"] = fw.sbuf(f"c3_so4{p}", [128, 4], F32)
        sh["dgn"] = [t4("dgnq", BF16), t4("dgnk", BF16)]
        sh["dgo"] = t4("dgo", BF16)
        gens += p3_streams(self, p, sh, bk)
    run_interleaved(gens)
    fw.pop_scope()


MK.phase3c = phase3c


def phase4b(self):
    fw, nc = self.fw, self.nc
    fw.push_scope()
    self.wal, self.walR = fw.sbuf("wal", [16, 512], F32)
    fw.sp.dma(self.wal[:], self.w_alpha, self.walR, "w")
    self.nba, self.nbaR = fw.sbuf("nba", [128, 4], F32)
    fw.sp.dma(self.nba[:], self.nb_alpha, self.nbaR, "w")
    fw.dve.op(lambda: nc.vector.tensor_scalar(out=self.nba[:], in0=self.nba[:], scalar1=-1.0, scalar2=None, op0=ALU.mult),
              reads=[self.nbaR], writes=[self.nbaR])
    self.nhg, self.nhgR = fw.sbuf("nhg", [128, 2], F32)
    fw.sp.dma(self.nhg[:], self.nh_gla, self.nhgR, "w")
    fw.dve.op(lambda: nc.vector.tensor_scalar(out=self.nhg[:], in0=self.nhg[:], scalar1=0.5, scalar2=None, op0=ALU.mult),
              reads=[self.nhgR], writes=[self.nhgR])
    self.rmask, self.rmaskR = fw.sbuf("rmask", [128, 256], F32)
    fw.pool.op(lambda: nc.gpsimd.memset(self.rmask[:], 1.0), writes=[self.rmaskR])
    fw.pool.op(lambda: nc.gpsimd.memset(self.rmask[:].rearrange("p (c k) -> p c k", k=64)[:, :, 0:1], 0.0), writes=[self.rmaskR])
    bk = BankPool([fw.psum(f"gbk{i}", [128, 512], F32) for i in range(8)])
    halves = [tuple(range(0, (self.NSEQ + 1) // 2)), tuple(range((self.NSEQ + 1) // 2, self.NSEQ))]
    halves = [hv for hv in halves if hv]
    gens = []
    W = 256
    for h in range(4):
        for si, seqs in enumerate(halves):
            p = f"{h}{si}"
            hb = {}
            sq = lambda nm, dt=BF16, w=W: fw.sbuf(f"g4{nm}{p}", [128, w], dt)
            hb["S32"], hb["Sb"], hb["Sm"] = sq("S32", F32, 256), sq("Sb", BF16, 256), sq("Sm", F32, 256)
            hb["qt"], hb["kt"] = sq("qt"), sq("kt")
            hb["lr"] = fw.sbuf(f"g4lr{p}", [16, W], F32)
            hb["vt"] = fw.sbuf(f"g4vt{p}", [128, W // 128, 256], BF16)
            for nm in ("e0", "cc", "d1", "ea", "e3", "ssb", "rn", "th"):
                hb[nm] = sq(nm, F32)
            for nm in ("qg", "kg", "qd", "kd", "rt", "og"):
                hb[nm] = sq(nm)
            hb["am"], hb["ktk"] = sq("am", BF16, 128), sq("ktk", BF16, 128)
            hb["oT"] = [sq("oT0", F32), sq("oT1", F32)]
            hb["sq"] = [sq("sq0"), sq("sq1")]
            gens.append(gla_stream(self, h, hb, seqs, bk))
    run_interleaved(gens)
    fw.pop_scope()


MK.phase4b = phase4b
```
